# Optimizing a Trainium2 kernel written in Bass

```python
import math
import jax, jax.numpy as jnp
from jax import lax
import numpy as np

D_MODEL = 1024
BATCH = 8
SEQ = 8192
DEPTH = 1

CTX_LEN = 256
GRID_W = 64
MIX_W = D_MODEL
F_W = MIX_W // 2
F_GROUPS = 4
F_GROUP_W = F_W // F_GROUPS
DN_W = MIX_W - F_W
DN_HEADS = 4
DK = DN_W // DN_HEADS
DV = DN_W // DN_HEADS
QK_W = DN_HEADS * DK
V_W = DN_HEADS * DV
CONV_CH = 2 * QK_W + V_W
W_IN = F_W + CONV_CH + V_W + 4 * DN_HEADS
CONV_K = 3
CHUNK = 64
N_GROUPS = 4
EXPERTS_PER_GROUP = 8
N_EXPERTS = N_GROUPS * EXPERTS_PER_GROUP
TOP_K = 2
D_EXPERT = D_MODEL // 2
MOE_BLOCK = 128
EPS = 1e-6

kernel_name = "hymba_fnet_gdn_hmoe_dit"


def rmsnorm(x, g):
    xf = x.astype(jnp.float32)
    y = xf * lax.rsqrt(jnp.mean(xf * xf, axis=-1, keepdims=True) + EPS) * g.astype(jnp.float32)
    return y.astype(x.dtype)


def l2norm(a):
    af = a.astype(jnp.float32)
    return af * lax.rsqrt(jnp.sum(af * af, axis=-1, keepdims=True) + EPS)


def modulate(h, shift, scale):
    return h * (1 + scale) + shift


def conv_grid(u, w):
    b, t, ch = u.shape
    rows = t // GRID_W
    y = lax.conv_general_dilated(u.reshape(b, rows, GRID_W, ch), w[:, :, None, :].astype(u.dtype),
                                 (1, 1), 'SAME', dimension_numbers=('NHWC', 'HWIO', 'NHWC'),
                                 feature_group_count=ch)
    return y.reshape(b, t, ch)


def conv_seq(u, w):
    ch = u.shape[-1]
    return lax.conv_general_dilated(u, w[1][:, None, :].astype(u.dtype), (1,), 'SAME',
                                    dimension_numbers=('NWC', 'WIO', 'NWC'), feature_group_count=ch)


def fourier_mix(f):
    b, t, _ = f.shape
    fg = f.astype(jnp.float32).reshape(b, t, F_GROUPS, F_GROUP_W)
    out = jnp.fft.fft2(fg, axes=(1, 3), norm='ortho').real
    return out.reshape(b, t, F_W).astype(f.dtype)


def gated_delta_chunked(q, k, v, g, beta, s0):
    b, t, h, dk = q.shape
    dv = v.shape[-1]
    n = t // CHUNK

    def chunks(a):
        a = jnp.moveaxis(a.astype(jnp.float32), 2, 1)
        return a.reshape(b, h, n, CHUNK, *a.shape[3:])

    q = chunks(q) * dk ** -0.5
    k = chunks(k)
    v = chunks(v)
    beta = chunks(beta)
    gc = jnp.cumsum(chunks(g), axis=-1)
    idx = jnp.arange(CHUNK)
    lower = idx[:, None] >= idx[None, :]
    decay = jnp.exp(jnp.where(lower, gc[..., :, None] - gc[..., None, :], -jnp.inf))
    k_beta = k * beta[..., None]
    m = jnp.where(idx[:, None] > idx[None, :],
                  jnp.einsum('bhnid,bhnjd->bhnij', k_beta, k) * decay, 0.0)
    eye = jnp.eye(CHUNK, dtype=jnp.float32)
    a_mat = eye + m
    t_inv = lax.linalg.triangular_solve(a_mat, jnp.broadcast_to(eye, a_mat.shape), left_side=True,
                                        lower=True, unit_diagonal=True)
    u = t_inv @ (v * beta[..., None])
    w = t_inv @ (k_beta * jnp.exp(gc)[..., None])
    intra = jnp.einsum('bhnid,bhnjd->bhnij', q, k) * decay

    def step(s, blk):
        q_i, k_i, u_i, w_i, a_i, g_i = blk
        v_new = u_i - w_i @ s
        o_i = (q_i * jnp.exp(g_i)[..., None]) @ s + a_i @ v_new
        g_last = g_i[..., -1:]
        s = s * jnp.exp(g_last)[..., None] + jnp.einsum(
            'bhcd,bhce->bhde', k_i * jnp.exp(g_last - g_i)[..., None], v_new)
        return s, o_i

    xs = tuple(jnp.moveaxis(a, 2, 0) for a in (q, k, u, w, intra, gc))
    s_final, o = lax.scan(step, s0.astype(jnp.float32), xs)
    o = jnp.moveaxis(o, 0, 2).reshape(b, h, t, dv)
    return jnp.moveaxis(o, 1, 2), s_final


def bidir_delta(q, k, v, g, beta, s0_fwd, s0_bwd):
    flip = lambda a: jnp.flip(a, axis=1)
    o_f, s_f = gated_delta_chunked(q, k, v, g[:, :, 0], beta[:, :, 0], s0_fwd)
    o_b, s_b = gated_delta_chunked(flip(q), flip(k), flip(v), flip(g[:, :, 1]), flip(beta[:, :, 1]), s0_bwd)
    return o_f + flip(o_b), s_f, s_b


def mixer_features(h, w_in, conv_w, a_log, dt_bias, conv_fn):
    b, t = h.shape[:2]
    p = h @ w_in
    f, qkv, z, gates = jnp.split(p, [F_W, F_W + CONV_CH, F_W + CONV_CH + V_W], axis=-1)
    qkv = jax.nn.silu(conv_fn(qkv, conv_w))
    q, k, v = jnp.split(qkv, [QK_W, 2 * QK_W], axis=-1)
    q = l2norm(q.reshape(b, t, DN_HEADS, DK))
    k = l2norm(k.reshape(b, t, DN_HEADS, DK))
    v = v.reshape(b, t, DN_HEADS, DV)
    gates = gates.astype(jnp.float32).reshape(b, t, 2, 2, DN_HEADS)
    beta = jax.nn.sigmoid(gates[:, :, 0])
    g = -jnp.exp(a_log.astype(jnp.float32)) * jax.nn.softplus(gates[:, :, 1] + dt_bias.astype(jnp.float32))
    return f, q, k, v, z, g, beta


def mixer_output(f, o, z, onorm_g, w_out):
    b, t = f.shape[:2]
    o = (rmsnorm(o, onorm_g) * jax.nn.silu(z.reshape(b, t, DN_HEADS, DV))).astype(f.dtype)
    return jnp.concatenate([fourier_mix(f), o.reshape(b, t, V_W)], axis=-1) @ w_out


def moe_ffn(h, w_group, b_group, w_router, b_router, w_gate, w_up, w_down):
    shp = h.shape
    d = shp[-1]
    xf = h.reshape(-1, d)
    n = xf.shape[0]
    pg = jax.nn.softmax((xf @ w_group).astype(jnp.float32) + b_group.astype(jnp.float32), axis=-1)
    pg_top, g_top = lax.top_k(pg, 1)
    el = ((xf @ w_router).astype(jnp.float32) + b_router.astype(jnp.float32)).reshape(
        n, N_GROUPS, EXPERTS_PER_GROUP)
    el_g = el[jnp.arange(n), g_top[:, 0]]
    pe_top, e_top = lax.top_k(jax.nn.softmax(el_g, axis=-1), TOP_K)
    wts = pg_top * pe_top / jnp.sum(pe_top, axis=-1, keepdims=True)
    eid = (g_top * EXPERTS_PER_GROUP + e_top).reshape(-1).astype(jnp.int32)
    w_f = wts.reshape(-1)
    n_assign = n * TOP_K
    order = jnp.argsort(eid)
    e_s = eid[order]
    tok_s = (order // TOP_K).astype(jnp.int32)
    w_s = w_f[order]
    counts = jnp.zeros((N_EXPERTS,), jnp.int32).at[eid].add(1)
    start = jnp.cumsum(counts) - counts
    pcounts = (counts + MOE_BLOCK - 1) // MOE_BLOCK * MOE_BLOCK
    pend = jnp.cumsum(pcounts)
    pstart = pend - pcounts
    dest = pstart[e_s] + (jnp.arange(n_assign, dtype=jnp.int32) - start[e_s])
    n_slots = -(-n_assign // MOE_BLOCK) * MOE_BLOCK + N_EXPERTS * MOE_BLOCK
    n_blocks = n_slots // MOE_BLOCK
    slot_tok = jnp.full((n_slots,), n, jnp.int32).at[dest].set(tok_s)
    slot_w = jnp.zeros((n_slots,), jnp.float32).at[dest].set(w_s)
    blk_expert = jnp.minimum(
        jnp.searchsorted(pend, jnp.arange(n_blocks, dtype=jnp.int32) * MOE_BLOCK, side='right'),
        N_EXPERTS - 1)
    x_pad = jnp.concatenate([xf, jnp.zeros((1, d), xf.dtype)], axis=0)
    xs = x_pad[slot_tok].reshape(n_blocks, MOE_BLOCK, d)

    def expert_block(args):
        xb, e = args
        hid = jax.nn.silu(xb @ w_gate[e]) * (xb @ w_up[e])
        return hid @ w_down[e]

    ys = lax.map(expert_block, (xs, blk_expert)).reshape(n_slots, d)
    out = jnp.zeros((n + 1, d), ys.dtype).at[slot_tok].add(ys * slot_w[:, None].astype(ys.dtype))
    return out[:n].reshape(shp)


def trunk_layer(x, ctx, c, c_ctx, w_mod, b_mod, norm1_g, w_in, conv_w, a_log, dt_bias, onorm_g, w_out,
                norm2_g, w_group, b_group, w_router, b_router, w_gate, w_up, w_down, update_ctx):
    mod_x = jnp.split((jax.nn.silu(c) @ w_mod + b_mod)[:, None, :], 6, axis=-1)
    mod_c = jnp.split((jax.nn.silu(c_ctx) @ w_mod + b_mod)[None, None, :], 6, axis=-1)

    hc = modulate(rmsnorm(ctx, norm1_g), mod_c[0], mod_c[1])
    fc, qc, kc, vc, zc, gc, bc = mixer_features(hc, w_in, conv_w, a_log, dt_bias, conv_seq)
    zero_state = jnp.zeros((ctx.shape[0], DN_HEADS, DK, DV), jnp.float32)
    oc, s_fwd, s_bwd = bidir_delta(qc, kc, vc, gc, bc, zero_state, zero_state)

    hx = modulate(rmsnorm(x, norm1_g), mod_x[0], mod_x[1])
    fx, qx, kx, vx, zx, gx, bx = mixer_features(hx, w_in, conv_w, a_log, dt_bias, conv_grid)
    ox, _, _ = bidir_delta(qx, kx, vx, gx, bx, s_fwd, s_bwd)
    x = x + mod_x[2] * mixer_output(fx, ox, zx, onorm_g, w_out)
    x = x + mod_x[5] * moe_ffn(modulate(rmsnorm(x, norm2_g), mod_x[3], mod_x[4]),
                               w_group, b_group, w_router, b_router, w_gate, w_up, w_down)
    if update_ctx:
        ctx = ctx + mod_c[2] * mixer_output(fc, oc, zc, onorm_g, w_out)
        ctx = ctx + mod_c[5] * moe_ffn(modulate(rmsnorm(ctx, norm2_g), mod_c[3], mod_c[4]),
                                       w_group, b_group, w_router, b_router, w_gate, w_up, w_down)
    return x, ctx


def setup_inputs(seed: int = 0) -> dict:
    key = jax.random.key(seed)
    ks = jax.random.split(key, 24)
    nrm = lambda k, shape, s: jax.random.normal(k, shape, jnp.float32) * s
    x = nrm(ks[0], (BATCH, SEQ, D_MODEL), 1.0)
    c = nrm(ks[1], (BATCH, D_MODEL), 1.0)
    ctx = nrm(ks[2], (BATCH, CTX_LEN, D_MODEL), 1.0)
    c_ctx = nrm(ks[3], (D_MODEL,), 1.0)
    w_mod = nrm(ks[4], (DEPTH, D_MODEL, 6 * D_MODEL), 0.5 * D_MODEL ** -0.5)
    b_mod = nrm(ks[5], (DEPTH, 6 * D_MODEL), 0.01)
    norm1_g = 1.0 + nrm(ks[6], (DEPTH, D_MODEL), 0.02)
    w_in = nrm(ks[7], (DEPTH, D_MODEL, W_IN), D_MODEL ** -0.5)
    conv_w = nrm(ks[8], (DEPTH, CONV_K, CONV_K, CONV_CH), 1.0 / CONV_K)
    a_log = jnp.log(jax.random.uniform(ks[9], (DEPTH, 2, DN_HEADS), jnp.float32, 1.0, 16.0))
    dt = jnp.exp(jax.random.uniform(ks[10], (DEPTH, 2, DN_HEADS), jnp.float32,
                                    math.log(1e-3), math.log(1e-1)))
    dt_bias = dt + jnp.log(-jnp.expm1(-dt))
    onorm_g = 1.0 + nrm(ks[11], (DEPTH, DV), 0.02)
    w_out = nrm(ks[12], (DEPTH, MIX_W, D_MODEL), MIX_W ** -0.5)
    norm2_g = 1.0 + nrm(ks[13], (DEPTH, D_MODEL), 0.02)
    w_group = nrm(ks[14], (DEPTH, D_MODEL, N_GROUPS), D_MODEL ** -0.5)
    b_group = nrm(ks[15], (DEPTH, N_GROUPS), 0.01)
    w_router = nrm(ks[16], (DEPTH, D_MODEL, N_EXPERTS), D_MODEL ** -0.5)
    b_router = nrm(ks[17], (DEPTH, N_EXPERTS), 0.01)
    w_gate = nrm(ks[18], (DEPTH, N_EXPERTS, D_MODEL, D_EXPERT), D_MODEL ** -0.5)
    w_up = nrm(ks[19], (DEPTH, N_EXPERTS, D_MODEL, D_EXPERT), D_MODEL ** -0.5)
    w_down = nrm(ks[20], (DEPTH, N_EXPERTS, D_EXPERT, D_MODEL), D_EXPERT ** -0.5)
    final_g = 1.0 + nrm(ks[21], (D_MODEL,), 0.02)
    return {"x": x, "c": c, "ctx": ctx, "c_ctx": c_ctx, "w_mod": w_mod, "b_mod": b_mod,
            "norm1_g": norm1_g, "w_in": w_in, "conv_w": conv_w, "a_log": a_log, "dt_bias": dt_bias,
            "onorm_g": onorm_g, "w_out": w_out, "norm2_g": norm2_g, "w_group": w_group,
            "b_group": b_group, "w_router": w_router, "b_router": b_router, "w_gate": w_gate,
            "w_up": w_up, "w_down": w_down, "final_g": final_g}


def reference(x, c, ctx, c_ctx, w_mod, b_mod, norm1_g, w_in, conv_w, a_log, dt_bias, onorm_g, w_out,
              norm2_g, w_group, b_group, w_router, b_router, w_gate, w_up, w_down, final_g):
    for l in range(DEPTH):
        x, ctx = trunk_layer(x, ctx, c, c_ctx, w_mod[l], b_mod[l], norm1_g[l], w_in[l], conv_w[l], a_log[l],
                             dt_bias[l], onorm_g[l], w_out[l], norm2_g[l], w_group[l], b_group[l],
                             w_router[l], b_router[l], w_gate[l], w_up[l], w_down[l],
                             update_ctx=(l < DEPTH - 1))
    return rmsnorm(x, final_g)
```

```python
import contextlib
import numpy as np
import ml_dtypes
import concourse.bass as bass
import concourse.mybir as mybir
from concourse.bass_utils import run_bass_kernel_spmd

F32 = mybir.dt.float32
BF16 = mybir.dt.bfloat16
I32 = mybir.dt.int32
AF = mybir.ActivationFunctionType
ALU = mybir.AluOpType
AX = mybir.AxisListType

T = 8192
D = 1024
TC = 256
NB = 16
EPS = 1e-6


class TB:
    def __init__(self, t, name=""):
        self.t = t
        self.name = name
        self.w = None
        self.r = []
        self.psum = False

    def __getitem__(self, k):
        return self.t[k]


class Sync:
    ENG = ("pe", "dve", "act", "pool", "sp")

    def __init__(self, nc, n_dma_sems=64):
        self.nc = nc
        self.e = {"pe": nc.tensor, "dve": nc.vector, "act": nc.scalar, "pool": nc.gpsimd, "sp": nc.sync}
        self.sem = {k: nc.alloc_semaphore(name="c_" + k) for k in self.ENG}
        self.cnt = {k: 0 for k in self.ENG}
        self.seen = {k: {} for k in self.ENG}
        self.dsems = [nc.alloc_semaphore(name="d_%d" % i) for i in range(n_dma_sems)]
        self.dcnt = [0] * n_dma_sems
        self.dnext = 0
        self.n_hw = n_dma_sems - 8
        self.pnext = 0
        self.n_ins = 0
        self.epoch = 0
        self.limit = None
        self.log = []

    def _wait(self, eng, tok):
        if tok is None:
            return
        kind, key, val = tok[0], tok[1], tok[2]
        if kind == "c":
            if tok[3] < self.epoch:
                return
            if key == "pe" and eng == "pe":
                return
            sem = self.sem[key]
            sk = "c" + key
        else:
            sem = self.dsems[key]
            sk = "d%d" % key
        if self.seen[eng].get(sk, 0) >= val:
            return
        self.e[eng].wait_ge(sem, val)
        self.seen[eng][sk] = val

    def _deps(self, eng, reads, writes):
        for b in reads:
            self._wait(eng, b.w)
            if b.psum:
                for t in b.r:
                    self._wait(eng, t)
        for b in writes:
            self._wait(eng, b.w)
            for t in b.r:
                self._wait(eng, t)

    def _commit(self, tok, reads, writes):
        for b in reads:
            b.r.append(tok)
            if len(b.r) > 48:
                b.r = b.r[-48:]
        for b in writes:
            b.w = tok
            b.r = []

    def op(self, eng, fn, reads=(), writes=()):
        if self.limit is not None and self.n_ins >= self.limit:
            return None
        self._deps(eng, reads, writes)
        self.log.append((self.n_ins, eng, fn.__code__.co_firstlineno))
        ins = fn()
        ins.then_inc(self.sem[eng], 1)
        self.cnt[eng] += 1
        self.n_ins += 1
        tok = ("c", eng, self.cnt[eng], self.epoch)
        self._commit(tok, reads, writes)
        return tok

    def dma(self, eng, fn, reads=(), writes=()):
        if self.limit is not None and self.n_ins >= self.limit:
            return None
        if eng == "pool":
            i = self.n_hw + self.pnext
            self.pnext = (self.pnext + 1) % 8
        else:
            i = self.dnext
            self.dnext = (self.dnext + 1) % self.n_hw
        if self.dcnt[i] > 0:
            self._wait(eng, ("d", i, self.dcnt[i]))
        self._deps(eng, reads, writes)
        self.log.append((self.n_ins, eng + "-dma", fn.__code__.co_firstlineno))
        ins = fn()
        self.dcnt[i] += 16
        ins.then_inc(self.dsems[i], 16)
        self.n_ins += 1
        tok = ("d", i, self.dcnt[i])
        self._commit(tok, reads, writes)
        return tok

    def barrier(self):
        for eng in self.ENG:
            for k in self.ENG:
                if self.cnt[k] > 0 and k != eng:
                    self._wait(eng, ("c", k, self.cnt[k], self.epoch))
            for i in range(len(self.dsems)):
                if self.dcnt[i] > 0:
                    self._wait(eng, ("d", i, self.dcnt[i]))

    def new_epoch(self):
        self.barrier()
        self.epoch += 1
        self.sem = {k: self.nc.alloc_semaphore(name="c%d_%s" % (self.epoch, k)) for k in self.ENG}
        self.cnt = {k: 0 for k in self.ENG}
        for eng in self.ENG:
            self.seen[eng] = {k: v for k, v in self.seen[eng].items() if not k.startswith("c")}


def host_consts():
    c = {}
    c["ident_bf"] = np.eye(128, dtype=np.float32).astype(ml_dtypes.bfloat16)
    c["ident_f"] = np.eye(128, dtype=np.float32)
    n = np.arange(128)
    ang = 2 * np.pi * np.outer(n, n) / 128.0
    c["cs128"] = np.concatenate([np.cos(ang), np.sin(ang)], axis=1).astype(np.float32).astype(ml_dtypes.bfloat16)
    c["ones_bf"] = np.ones((128, 128), np.float32).astype(ml_dtypes.bfloat16)
    c["ones_f"] = np.ones((128, 128), np.float32)
    c["zf"] = np.zeros((128, 512), np.float32)
    c["zb"] = np.zeros((128, 7680), np.float32).astype(ml_dtypes.bfloat16)
    ii = np.arange(128)[:, None]
    jj = np.arange(128)[None, :]
    c["mS0"] = np.tile((ii > jj).astype(np.float32), (1, 4)); c["mI0"] = np.tile((ii >= jj).astype(np.float32), (1, 4))
    c["mS1"] = np.tile((ii < jj).astype(np.float32), (1, 4)); c["mI1"] = np.tile((ii <= jj).astype(np.float32), (1, 4))
    c["Lm0"] = (ii <= jj).astype(np.float32)
    c["Lm1"] = (ii >= jj).astype(np.float32)
    c["Um0"] = (ii > jj).astype(np.float32)
    c["Um1"] = (ii < jj).astype(np.float32)
    bb = np.arange(128)[:, None, None]; aa = np.arange(64)[None, :, None]; bp = np.arange(128)[None, None, :]
    th = 2 * np.pi * ((bp * (64 * bb + aa)) % 8192) / 8192.0
    c["cosA"] = np.cos(th).astype(np.float32).astype(ml_dtypes.bfloat16)
    c["sinA"] = np.sin(th).astype(np.float32).astype(ml_dtypes.bfloat16)
    c["nsinA"] = (-np.sin(th)).astype(np.float32).astype(ml_dtypes.bfloat16)
    a1 = np.arange(64)[:, None]; a2 = np.arange(64)[None, :]
    ps_ = 2 * np.pi * ((a1 * a2) % 64) / 64.0
    c["cs64"] = np.concatenate([np.cos(ps_), -np.sin(ps_)], axis=0).astype(np.float32).astype(ml_dtypes.bfloat16)
    c["I4f"] = np.tile(np.eye(128, dtype=np.float32), (1, 4))
    c["bdm"] = np.tile(((ii // 32) == (jj // 32)).astype(np.float32), (1, 4))
    for i_ in range(4):
        c["cm%d" % i_] = np.tile(((jj // 32) == i_).astype(np.float32) * np.ones((128, 1), np.float32), (1, 4))
    c["I4"] = np.tile(np.eye(128, dtype=np.float32), (1, 4)).astype(ml_dtypes.bfloat16)
    return c


def build(stages=4, dbg=False, nblk=NB, run=(1, 2, 3, 4), feed=(), nch=64, NT4=64, NEXP=32, ulimit=None, limit=None):
    nc = bass.Bass("TRN2", target_bir_lowering=False)
    S = Sync(nc)
    S.limit = limit
    E = S.e

    def din(name, shape, dt=F32):
        return nc.dram_tensor(name, list(shape), dt, kind="ExternalInput").ap()

    def dscr(name, shape, dt, out=False):
        if name in feed:
            return nc.dram_tensor(name, list(shape), dt, kind="ExternalInput").ap()
        return nc.dram_tensor(name, list(shape), dt, kind=("ExternalOutput" if (out or (dbg and (dbg is True or name in dbg))) else "Internal")).ap()

    x_d = din("x", [T, D])
    ctx_d = din("ctx", [TC, D])
    cT_d = din("cT", [128, 16])
    w_mod_d = din("w_mod", [D, 6 * D])
    b_mod_d = din("b_mod", [1, 6 * D])
    n1gT_d = din("n1gT", [128, 8])
    w_in_d = din("w_in", [D, 2576])
    convw_d = din("convw", [128, 12, 9])
    alog_d = din("a_log", [1, 8])
    dtb_d = din("dt_bias", [1, 8])
    ident_bf_d = din("ident_bf", [128, 128], BF16)
    ident_f_d = din("ident_f", [128, 128])
    cs128_d = din("cs128", [128, 256], BF16)
    ones_bf_d = din("ones_bf", [128, 128], BF16)
    ones_f_d = din("ones_f", [128, 128])
    zf_d = din("zf", [128, 512])
    zb_d = din("zb", [128, 7680], BF16)

    qT_d = dscr("qT_s", [4, 128, T], BF16)
    kT_d = dscr("kT_s", [4, 128, T], BF16)
    k_d = dscr("k_s", [T, 512], BF16)
    v_d = dscr("v_s", [T, 512], BF16)
    z_d = dscr("z_s", [T, 512], BF16)
    pq_d = dscr("pq_s", [T, 1024], BF16)
    gb_d = dscr("gb_s", [T, 16], F32)
    cqT_d = dscr("cqT_s", [4, 128, TC], BF16)
    ckT_d = dscr("ckT_s", [4, 128, TC], BF16)
    ck_d = dscr("ck_s", [TC, 512], BF16)
    cv_d = dscr("cv_s", [TC, 512], BF16)
    cgb_d = dscr("cgb_s", [TC, 16], F32)
    modrow_d = dscr("modrow_s", [2, 6 * D], F32)

    of_d = dscr("of_s", [T, 512], F32)
    ob_d = dscr("ob_s", [T, 512], F32)
    s0_d = [dscr("s0_%d" % d, [128, 4, 128], F32) for d in range(2)]
    mS_d = [din("mS%d" % d, [128, 512]) for d in range(2)]
    mI_d = [din("mI%d" % d, [128, 512]) for d in range(2)]
    Lm_d = [din("Lm%d" % d, [128, 128]) for d in range(2)]
    Um_d = [din("Um%d" % d, [128, 128]) for d in range(2)]
    I4_d = din("I4", [128, 512], BF16)
    I4f_d = din("I4f", [128, 512])
    bdm_d = din("bdm", [128, 512])
    cm_d = [din("cm%d" % i, [128, 512]) for i in range(4)]
    Zd = dscr("Z_s", [2, 64, 128, 512], BF16)
    fmT_d = dscr("fmT_s", [4, 128, T], BF16)
    cosA_d = din("cosA", [128, 64, 128], BF16)
    sinA_d = din("sinA", [128, 64, 128], BF16)
    nsinA_d = din("nsinA", [128, 64, 128], BF16)
    cs64_d = din("cs64", [128, 64], BF16)
    x1_d = dscr("x1_s", [T, D], F32)
    h2T_d = dscr("h2T_s", [8, 128, T], BF16)
    comb_d = dscr("comb_s", [T, 32], F32)
    n2g_d = din("n2g", [1, D])
    fing_d = din("fing", [1, D])
    onorm_d = din("onorm", [1, 128])
    bgrp_d = din("b_group", [1, 4])
    brt_d = din("b_router", [1, 32])
    wgrp_d = din("w_group", [D, 4])
    wrt_d = din("w_router", [D, 32])
    w_out_d = din("w_out", [D, D])
    wgate_d = din("w_gate", [32, D, 512])
    wup_d = din("w_up", [32, D, 512])
    wdown_d = din("w_down", [32, 512, D])
    out_d = nc.dram_tensor("out", [T, D], F32, kind="ExternalOutput").ap()

    glob = contextlib.ExitStack()

    def mk(stack, space, name, shape, dt):
        if space == "sb":
            t = stack.enter_context(nc.sbuf_tensor("t_" + name, list(shape), dt))
        else:
            t = stack.enter_context(nc.psum_tensor("t_" + name, list(shape), dt))
            tb = TB(t, name)
            tb.psum = True
            return tb
        return TB(t, name)

    with glob:
        ident_bf = mk(glob, "sb", "ident_bf", [128, 128], BF16)
        ident_f = mk(glob, "sb", "ident_f", [128, 128], F32)
        ones_bf = mk(glob, "sb", "ones_bf", [128, 128], BF16)
        ones_f = mk(glob, "sb", "ones_f", [128, 128], F32)
        modT = mk(glob, "sb", "modT", [128, 48, 2], F32)
        for tb, d in ((ident_bf, ident_bf_d), (ident_f, ident_f_d), (ones_bf, ones_bf_d), (ones_f, ones_f_d)):
            S.dma("sp", lambda tb=tb, d=d: nc.sync.dma_start(out=tb[:], in_=d), writes=[tb])

        def stage1():
            st = contextlib.ExitStack()
            with st:
                sb = lambda name, shape, dt: mk(st, "sb", name, shape, dt)
                ps = lambda name, shape, dt: mk(st, "ps", name, shape, dt)
                n1g = sb("n1g", [128, 8], F32)
                gs1 = sb("gs1", [128, 8, 2], F32)
                w_in = sb("w_in", [128, 8, 2064], BF16)
                cs128 = sb("cs128", [128, 256], BF16)
                wpq = sb("wpq", [128, 8, 1024], BF16)
                convw = sb("convw", [128, 12, 9], F32)
                alog = sb("alog", [128, 8], F32)
                dtb = sb("dtb", [128, 8], F32)
                nea = sb("nea", [128, 8], F32)
                sa = contextlib.ExitStack()
                sa.__enter__()
                sbA = lambda name, shape, dt: mk(sa, "sb", name, shape, dt)
                cT = sbA("cT", [128, 16], F32)
                sc = sbA("sc", [128, 16], F32)
                S.dma("sp", lambda: nc.sync.dma_start(out=cT[:], in_=cT_d), writes=[cT])
                S.op("act", lambda: nc.scalar.activation(out=sc[:], in_=cT[:], func=AF.Silu), reads=[cT], writes=[sc])
                scl = sbA("scl", [128, 8, 2], F32)
                S.op("dve", lambda: nc.vector.tensor_copy(out=scl[:, :, 0], in_=sc[:, 0:8]), reads=[sc], writes=[scl])
                S.op("dve", lambda: nc.vector.tensor_copy(out=scl[:, :, 1], in_=sc[:, 8:16]), reads=[sc], writes=[scl])
                wm = [sbA("wm%d" % i, [128, 8, 512], F32) for i in range(2)]
                bmod = sbA("bmod", [2, 6 * D], F32)
                for r in range(2):
                    S.dma("sp", lambda r=r: nc.sync.dma_start(out=bmod[r:r + 1, :], in_=b_mod_d), writes=[bmod])
                modrow = sbA("modrow", [2, 6 * D], F32)
                pm = [ps("pm%d" % i, [128, 512], F32) for i in range(2)]
                for g in range(12):
                    w = wm[g % 2]
                    S.dma("sp", lambda w=w, g=g: nc.sync.dma_start(
                        out=w[:], in_=w_mod_d[:, g * 512:(g + 1) * 512].rearrange("(k p) n -> p k n", p=128)), writes=[w])
                    p = pm[g % 2]
                    for k in range(8):
                        S.op("pe", lambda p=p, w=w, k=k: nc.tensor.matmul(p[0:2, :], lhsT=scl[:, k, :], rhs=w[:, k, :],
                                                                           start=(k == 0), stop=(k == 7)),
                             reads=[scl, w], writes=[p])
                    S.op("dve", lambda p=p, g=g: nc.vector.tensor_tensor(out=modrow[:, g * 512:(g + 1) * 512], in0=p[0:2, :],
                                                                          in1=bmod[:, g * 512:(g + 1) * 512], op=ALU.add),
                         reads=[p, bmod], writes=[modrow])
                pGb = ps("pGb", [128, 512], F32)
                pmt = TB(pGb.t, "pmt")
                pmtv = pGb[:, 0:96].rearrange("p (a b) -> p a b", b=2)
                for blk in range(48):
                    S.op("pe", lambda blk=blk: nc.tensor.transpose(pmtv[:, blk, :], modrow[0:2, blk * 128:(blk + 1) * 128],
                                                                   ident_f[0:2, 0:2]), reads=[modrow, ident_f], writes=[pGb])
                S.op("dve", lambda: nc.vector.tensor_copy(out=modT[:], in_=pmtv), reads=[pGb], writes=[modT])
                S.dma("pool", lambda: nc.gpsimd.dma_start(out=modrow_d, in_=modrow[:]), reads=[modrow])
                S.dma("sp", lambda: nc.sync.dma_start(out=n1g[:], in_=n1gT_d), writes=[n1g])
                for j in range(2):
                    S.op("dve", lambda j=j: nc.vector.scalar_tensor_tensor(out=gs1[:, :, j], in0=modT[:, 8:16, j], scalar=1.0,
                                                                            in1=n1g[:], op0=ALU.add, op1=ALU.mult),
                         reads=[modT, n1g], writes=[gs1])

                S.dma("pool", lambda: nc.gpsimd.dma_start(out=w_in[:], in_=w_in_d[:, 512:2576].rearrange("(k p) n -> p k n", p=128)),
                      writes=[w_in])
                wf = sbA("wf", [128, 8, 512], BF16)
                S.dma("pool", lambda: nc.gpsimd.dma_start(out=wf[:], in_=w_in_d[:, 0:512].rearrange("(k p) n -> p k n", p=128)),
                      writes=[wf])
                S.dma("sp", lambda: nc.sync.dma_start(out=cs128[:], in_=cs128_d), writes=[cs128])
                wfT = sbA("wfT", [128, 4, 1024], BF16)
                ptr = [ps("ptr%d" % i, [128, 8, 128], BF16) for i in range(1)]
                for g in range(4):
                    for k in range(8):
                        S.op("pe", lambda g=g, k=k: nc.tensor.transpose(ptr[0][:, k, :], wf[:, k, g * 128:(g + 1) * 128], ident_bf[:]),
                             reads=[wf, ident_bf], writes=[ptr[0]])
                    S.op("dve", lambda g=g: nc.vector.tensor_copy(out=wfT[:, g, :], in_=ptr[0][:].rearrange("p a b -> p (a b)")),
                         reads=[ptr[0]], writes=[wfT])
                for k in range(8):
                    p = pm[k % 2]
                    for g in range(4):
                        S.op("pe", lambda p=p, g=g, k=k: nc.tensor.matmul(p[:, (g % 2) * 256:(g % 2) * 256 + 256],
                                                                           lhsT=wfT[:, g, k * 128:(k + 1) * 128], rhs=cs128[:], start=True, stop=True),
                             reads=[wfT, cs128], writes=[p])
                        S.op("act", lambda p=p, g=g, k=k: nc.scalar.copy(out=wpq[:, k, g * 128:(g + 1) * 128],
                                                                          in_=p[:, (g % 2) * 256:(g % 2) * 256 + 128]),
                             reads=[p], writes=[wpq])
                        S.op("act", lambda p=p, g=g, k=k: nc.scalar.copy(out=wpq[:, k, 512 + g * 128:512 + (g + 1) * 128],
                                                                          in_=p[:, (g % 2) * 256 + 128:(g % 2) * 256 + 256]),
                             reads=[p], writes=[wpq])

                S.dma("sp", lambda: nc.sync.dma_start(out=convw[:], in_=convw_d), writes=[convw])
                S.dma("sp", lambda: nc.sync.dma_start(out=alog[:], in_=alog_d.partition_broadcast(128)), writes=[alog])
                S.dma("sp", lambda: nc.sync.dma_start(out=dtb[:], in_=dtb_d.partition_broadcast(128)), writes=[dtb])
                S.op("act", lambda: nc.scalar.activation(out=nea[:], in_=alog[:], func=AF.Exp), reads=[alog], writes=[nea])
                S.op("dve", lambda: nc.vector.tensor_scalar(out=nea[:], in0=nea[:], scalar1=-1.0, scalar2=None, op0=ALU.mult),
                     reads=[nea], writes=[nea])

                S.barrier()
                sa.__exit__(None, None, None)
                if stages <= 0.3:
                    return True
                xt = [sb("xt%d" % i, [128, D], F32) for i in range(2)]
                xn = [sb("xn%d" % i, [128, D], BF16) for i in range(2)]
                junk = sb("junk", [128, D], BF16)
                ssq = [sb("ssq%d" % i, [128, 1], F32) for i in range(4)]
                hT = [sb("hT%d" % i, [128, 8, 512], BF16) for i in range(2)]
                pre = [sb("pre%d" % i, [128, 12, 640], BF16) for i in range(3)]
                cacc = [sb("cacc%d" % i, [128, 512], F32) for i in range(4)]
                cvT = sb("cvT", [128, 12, 512], BF16)
                sq = [sb("sq%d" % i, [128, 512], BF16) for i in range(2)]
                rs = [sb("rs%d" % i, [128, 512], F32) for i in range(2)]
                nT = [sb("nT%d" % i, [128, 8, 512], BF16) for i in range(1)]
                ktok = [sb("ktok%d" % i, [128, 512], BF16) for i in range(2)]
                vtok = [sb("vtok%d" % i, [128, 512], BF16) for i in range(2)]
                pqs = [sb("pqs%d" % i, [128, 1024], BF16) for i in range(2)]
                zs = [sb("zs%d" % i, [128, 512], BF16) for i in range(2)]
                gts = [sb("gts%d" % i, [128, 16], F32) for i in range(2)]
                gbs = [sb("gbs%d" % i, [128, 16], F32) for i in range(2)]
                pT = ps("pT", [128, 8, 128], BF16)
                pA = pm
                pB = [ps("pB%d" % i, [128, 512], F32) for i in range(2)]
                pZ = ps("pZ", [128, 512], F32)
                pG = pGb
                pX = ptr[0]

                cnt = {"x": 0, "n": 0, "s": 0, "c": 0}

                def norm_transpose(src_d, row0, jmod, hTb, col0):
                    xb = xt[cnt["x"] % 2]
                    xnb = xn[cnt["n"] % 2]
                    sq1 = ssq[cnt["s"] % 4]
                    cnt["x"] += 1; cnt["n"] += 1; cnt["s"] += 1
                    S.dma("sp", lambda: nc.sync.dma_start(out=xb[:], in_=src_d[row0:row0 + 128, :]), writes=[xb])
                    S.op("act", lambda: nc.scalar.activation(out=junk[:], in_=xb[:], func=AF.Square, accum_out=sq1[:]),
                         reads=[xb], writes=[junk, sq1])
                    S.op("dve", lambda: nc.vector.tensor_scalar(out=sq1[:], in0=sq1[:], scalar1=1.0 / D, scalar2=EPS, op0=ALU.mult, op1=ALU.add),
                         reads=[sq1], writes=[sq1])
                    S.op("act", lambda: nc.scalar.sqrt(out=sq1[:], in_=sq1[:]), reads=[sq1], writes=[sq1])
                    S.op("dve", lambda: nc.vector.reciprocal(out=sq1[:], in_=sq1[:]), reads=[sq1], writes=[sq1])
                    S.op("act", lambda: nc.scalar.activation(out=xnb[:], in_=xb[:], func=AF.Copy, scale=sq1[:]),
                         reads=[xb, sq1], writes=[xnb])
                    for k in range(8):
                        S.op("pe", lambda k=k: nc.tensor.transpose(pT[:, k, :], xnb[:, k * 128:(k + 1) * 128], ident_bf[:]),
                             reads=[xnb, ident_bf], writes=[pT])
                    for k in range(8):
                        eng = "dve" if k % 2 == 0 else "pool"
                        if eng == "pool":
                            S.op("act", lambda k=k: nc.scalar.activation(out=hTb[:, k, col0:col0 + 128], in_=pT[:, k, :], func=AF.Identity,
                                                                          scale=gs1[:, k, jmod:jmod + 1], bias=modT[:, k, jmod:jmod + 1]),
                                 reads=[pT, gs1, modT], writes=[hTb])
                        else:
                            S.op("dve", lambda k=k: nc.vector.tensor_scalar(out=hTb[:, k, col0:col0 + 128], in0=pT[:, k, :],
                                                                             scalar1=gs1[:, k, jmod:jmod + 1], scalar2=modT[:, k, jmod:jmod + 1],
                                                                             op0=ALU.mult, op1=ALU.add),
                                 reads=[pT, gs1, modT], writes=[hTb])

                def gates_math(gt, gbo):
                    S.op("act", lambda: nc.scalar.activation(out=gbo[:, 0:8], in_=gt[:, 0:8], func=AF.Sigmoid), reads=[gt], writes=[gbo])
                    S.op("dve", lambda: nc.vector.tensor_tensor(out=gt[:, 8:16], in0=gt[:, 8:16], in1=dtb[:], op=ALU.add),
                         reads=[gt, dtb], writes=[gt])
                    S.op("act", lambda: nc.scalar.activation(out=gt[:, 8:16], in_=gt[:, 8:16], func=AF.Exp), reads=[gt], writes=[gt])
                    S.op("act", lambda: nc.scalar.activation(out=gt[:, 8:16], in_=gt[:, 8:16], func=AF.Ln, bias=1.0, scale=1.0),
                         reads=[gt], writes=[gt])
                    S.op("dve", lambda: nc.vector.tensor_tensor(out=gbo[:, 8:16], in0=gt[:, 8:16], in1=nea[:], op=ALU.mult),
                         reads=[gt, nea], writes=[gbo])

                def features(cv, ntok, qTd, kTd, kd, vd, tok0):
                    nTb = nT[0]
                    cnt["c"] += 1
                    for i in range(8):
                        sqb = sq[i % 2]
                        rsb = rs[i % 2]
                        pa = pA[i % 2]
                        S.op("act", lambda: nc.scalar.activation(out=sqb[:, 0:ntok], in_=cv[:, i, 0:ntok], func=AF.Square),
                             reads=[cv], writes=[sqb])
                        S.op("pe", lambda: nc.tensor.matmul(pa[:, 0:ntok], lhsT=ones_bf[:], rhs=sqb[:, 0:ntok], start=True, stop=True),
                             reads=[ones_bf, sqb], writes=[pa])
                        S.op("dve", lambda: nc.vector.tensor_scalar(out=rsb[:, 0:ntok], in0=pa[:, 0:ntok], scalar1=EPS, scalar2=None,
                                                                     op0=ALU.add), reads=[pa], writes=[rsb])
                        S.op("act", lambda: nc.scalar.sqrt(out=rsb[:, 0:ntok], in_=rsb[:, 0:ntok]), reads=[rsb], writes=[rsb])
                        S.op("dve", lambda: nc.vector.reciprocal(out=rsb[:, 0:ntok], in_=rsb[:, 0:ntok]), reads=[rsb], writes=[rsb])
                        S.op("dve", lambda: nc.vector.tensor_tensor(out=nTb[:, i, 0:ntok], in0=cv[:, i, 0:ntok], in1=rsb[:, 0:ntok], op=ALU.mult),
                             reads=[cv, rsb], writes=[nTb])
                    for h in range(4):
                        S.dma("pool", lambda h=h: nc.gpsimd.dma_start(out=qTd[h, :, tok0:tok0 + ntok], in_=nTb[:, h, 0:ntok]), reads=[nTb])
                        S.dma("pool", lambda h=h: nc.gpsimd.dma_start(out=kTd[h, :, tok0:tok0 + ntok], in_=nTb[:, 4 + h, 0:ntok]), reads=[nTb])
                    if stages <= 0.57:
                        return
                    for tt in range(ntok // 128):
                        kb = ktok[tt % 2]
                        vb = vtok[tt % 2]
                        for h in range(4):
                            S.op("pe", lambda h=h: nc.tensor.transpose(pX[:, h, :], nTb[:, 4 + h, tt * 128:(tt + 1) * 128], ident_bf[:]),
                                 reads=[nTb, ident_bf], writes=[pX])
                            S.op("pe", lambda h=h: nc.tensor.transpose(pX[:, 4 + h, :], cv[:, 8 + h, tt * 128:(tt + 1) * 128], ident_bf[:]),
                                 reads=[cv, ident_bf], writes=[pX])
                        S.op("dve", lambda: nc.vector.tensor_copy(out=kb[:], in_=pX[:, 0:4, :].rearrange("p a b -> p (a b)")),
                             reads=[pX], writes=[kb])
                        S.op("dve", lambda: nc.vector.tensor_copy(out=vb[:], in_=pX[:, 4:8, :].rearrange("p a b -> p (a b)")),
                             reads=[pX], writes=[vb])
                        r0 = tok0 + tt * 128
                        S.dma("pool", lambda: nc.gpsimd.dma_start(out=kd[r0:r0 + 128, :], in_=kb[:]), reads=[kb])
                        S.dma("pool", lambda: nc.gpsimd.dma_start(out=vd[r0:r0 + 128, :], in_=vb[:]), reads=[vb])

                hTc = hT[0]
                for tt in range(2):
                    norm_transpose(ctx_d, tt * 128, 1, hTc, tt * 128)
                if stages <= 0.4:
                    S.barrier()
                    return True
                prc = pre[0]
                for ct in range(12):
                    pa = pA[ct % 2]
                    for k in range(8):
                        S.op("pe", lambda k=k: nc.tensor.matmul(pa[:, 0:256], lhsT=w_in[:, k, ct * 128:(ct + 1) * 128], rhs=hTc[:, k, 0:256],
                                                                start=(k == 0), stop=(k == 7)), reads=[w_in, hTc], writes=[pa])
                    S.op("act", lambda: nc.scalar.copy(out=prc[:, ct, 0:256], in_=pa[:, 0:256]), reads=[pa], writes=[prc])
                for tt in range(2):
                    gt = gts[tt % 2]
                    gbo = gbs[tt % 2]
                    for k in range(8):
                        S.op("pe", lambda k=k: nc.tensor.matmul(pG[:, 0:16], lhsT=hTc[:, k, tt * 128:(tt + 1) * 128], rhs=w_in[:, k, 2048:2064],
                                                                start=(k == 0), stop=(k == 7)), reads=[hTc, w_in], writes=[pG])
                    S.op("dve", lambda: nc.vector.tensor_copy(out=gt[:], in_=pG[:, 0:16]), reads=[pG], writes=[gt])
                    gates_math(gt, gbo)
                    S.dma("pool", lambda: nc.gpsimd.dma_start(out=cgb_d[tt * 128:(tt + 1) * 128, :], in_=gbo[:]), reads=[gbo])
                if stages <= 0.5:
                    S.barrier()
                    return True
                for ct in range(12):
                    ca = cacc[ct % 4]
                    eng = "dve"
                    V = E[eng]
                    S.op(eng, lambda: V.tensor_scalar(out=ca[:, 0:256], in0=prc[:, ct, 0:256], scalar1=convw[:, ct, 4:5], scalar2=None, op0=ALU.mult),
                         reads=[prc, convw], writes=[ca])
                    S.op(eng, lambda: V.scalar_tensor_tensor(out=ca[:, 1:256], in0=prc[:, ct, 0:255], scalar=convw[:, ct, 3:4], in1=ca[:, 1:256],
                                                             op0=ALU.mult, op1=ALU.add), reads=[prc, convw, ca], writes=[ca])
                    S.op(eng, lambda: V.scalar_tensor_tensor(out=ca[:, 0:255], in0=prc[:, ct, 1:256], scalar=convw[:, ct, 5:6], in1=ca[:, 0:255],
                                                             op0=ALU.mult, op1=ALU.add), reads=[prc, convw, ca], writes=[ca])
                    S.op("act", lambda: nc.scalar.activation(out=cvT[:, ct, 0:256], in_=ca[:, 0:256], func=AF.Silu), reads=[ca], writes=[cvT])
                if stages <= 0.55:
                    S.barrier()
                    return True
                features(cvT, 256, cqT_d, ckT_d, ck_d, cv_d, 0)
                if stages <= 0.6:
                    S.barrier()
                    return True

                for pb in pre:
                    S.dma("sp", lambda pb=pb: nc.sync.dma_start(out=pb[:].rearrange("p a b -> p (a b)"), in_=zb_d), writes=[pb])

                def inproj(j):
                    hTb = hT[j % 2]
                    for tt in range(4):
                        norm_transpose(x_d, j * 512 + tt * 128, 0, hTb, tt * 128)
                    pj = pre[j % 3]
                    for ct in range(12):
                        pa = pA[ct % 2]
                        for k in range(8):
                            S.op("pe", lambda k=k: nc.tensor.matmul(pa[:], lhsT=w_in[:, k, ct * 128:(ct + 1) * 128], rhs=hTb[:, k, :],
                                                                    start=(k == 0), stop=(k == 7)), reads=[w_in, hTb], writes=[pa])
                        S.op("act", lambda: nc.scalar.copy(out=pj[:, ct, 64:576], in_=pa[:]), reads=[pa], writes=[pj])
                        if j > 0:
                            pp = pre[(j - 1) % 3]
                            S.op("dve", lambda: nc.vector.tensor_copy(out=pp[:, ct, 576:640], in_=pa[:, 0:64]), reads=[pa], writes=[pp])
                        if j < NB - 1:
                            pn = pre[(j + 1) % 3]
                            S.op("dve", lambda: nc.vector.tensor_copy(out=pn[:, ct, 0:64], in_=pa[:, 448:512]), reads=[pa], writes=[pn])
                    if j == NB - 1:
                        S.dma("sp", lambda: nc.sync.dma_start(out=pj[:, :, 576:640], in_=zb_d[:, 0:768].rearrange("p (a b) -> p a b", b=64)), writes=[pj])
                    for tt in range(4):
                        r0 = j * 512 + tt * 128
                        pqb = pqs[tt % 2]
                        zb = zs[tt % 2]
                        gt = gts[tt % 2]
                        gbo = gbs[tt % 2]
                        for half in range(2):
                            for k in range(8):
                                S.op("pe", lambda k=k: nc.tensor.matmul(pB[half][:], lhsT=hTb[:, k, tt * 128:(tt + 1) * 128],
                                                                        rhs=wpq[:, k, half * 512:(half + 1) * 512], start=(k == 0), stop=(k == 7)),
                                     reads=[hTb, wpq], writes=[pB[half]])
                        S.op("dve", lambda: nc.vector.tensor_copy(out=pqb[:, 0:512], in_=pB[0][:]), reads=[pB[0]], writes=[pqb])
                        S.op("act", lambda: nc.scalar.copy(out=pqb[:, 512:1024], in_=pB[1][:]), reads=[pB[1]], writes=[pqb])
                        S.dma("pool", lambda: nc.gpsimd.dma_start(out=pq_d[r0:r0 + 128, :], in_=pqb[:]), reads=[pqb])
                        for k in range(8):
                            S.op("pe", lambda k=k: nc.tensor.matmul(pZ[:], lhsT=hTb[:, k, tt * 128:(tt + 1) * 128], rhs=w_in[:, k, 1536:2048],
                                                                    start=(k == 0), stop=(k == 7)), reads=[hTb, w_in], writes=[pZ])
                        S.op("act", lambda: nc.scalar.activation(out=zb[:], in_=pZ[:], func=AF.Silu), reads=[pZ], writes=[zb])
                        S.dma("pool", lambda: nc.gpsimd.dma_start(out=z_d[r0:r0 + 128, :], in_=zb[:]), reads=[zb])
                        for k in range(8):
                            S.op("pe", lambda k=k: nc.tensor.matmul(pG[:, 0:16], lhsT=hTb[:, k, tt * 128:(tt + 1) * 128], rhs=w_in[:, k, 2048:2064],
                                                                    start=(k == 0), stop=(k == 7)), reads=[hTb, w_in], writes=[pG])
                        S.op("dve", lambda: nc.vector.tensor_copy(out=gt[:], in_=pG[:, 0:16]), reads=[pG], writes=[gt])
                        gates_math(gt, gbo)
                        S.dma("pool", lambda: nc.gpsimd.dma_start(out=gb_d[r0:r0 + 128, :], in_=gbo[:]), reads=[gbo])

                def conv(j):
                    pj = pre[j % 3]
                    pv = pj[:].rearrange("p c (r w) -> p c r w", w=64)
                    for ct in range(12):
                        ca = cacc[ct % 4]
                        cav = ca[:].rearrange("p (r w) -> p r w", w=64)
                        eng = "dve"
                        V = E[eng]
                        S.op(eng, lambda: V.tensor_scalar(out=cav, in0=pv[:, ct, 1:9, :], scalar1=convw[:, ct, 4:5], scalar2=None, op0=ALU.mult),
                             reads=[pj, convw], writes=[ca])
                        for dr in (-1, 0, 1):
                            for dc in (-1, 0, 1):
                                if dr == 0 and dc == 0:
                                    continue
                                tap = (dr + 1) * 3 + (dc + 1)
                                c0 = max(0, -dc)
                                c1 = 64 - max(0, dc)
                                S.op(eng, lambda: V.scalar_tensor_tensor(
                                    out=cav[:, :, c0:c1], in0=pv[:, ct, 1 + dr:9 + dr, c0 + dc:c1 + dc], scalar=convw[:, ct, tap:tap + 1],
                                    in1=cav[:, :, c0:c1], op0=ALU.mult, op1=ALU.add), reads=[pj, convw, ca], writes=[ca])
                        S.op("act", lambda: nc.scalar.activation(out=cvT[:, ct, :], in_=ca[:], func=AF.Silu), reads=[ca], writes=[cvT])
                    features(cvT, 512, qT_d, kT_d, k_d, v_d, j * 512)

                for j in range(nblk + 1):
                    if j < nblk:
                        inproj(j)
                    if j >= 1:
                        conv(j - 1)
                S.new_epoch()
            return False

        if 1 in run:
            if stage1():
                return nc, S

        def stage2():
            st2 = contextlib.ExitStack()
            with st2:
                sb = lambda name, shape, dt: mk(st2, "sb", name, shape, dt)
                ps = lambda name, shape, dt: mk(st2, "ps", name, shape, dt)
                QS = 128.0 ** -0.5
                mS = [sb("mS%d" % d, [128, 512], F32) for d in range(2)]
                mI = [sb("mI%d" % d, [128, 512], F32) for d in range(2)]
                Lm = [sb("Lm%d" % d, [128, 128], F32) for d in range(2)]
                Um = [sb("Um%d" % d, [128, 128], F32) for d in range(2)]
                I4 = sb("I4", [128, 512], BF16)
                for d in range(2):
                    for tb, dd in ((mS[d], mS_d[d]), (mI[d], mI_d[d]), (Lm[d], Lm_d[d]), (Um[d], Um_d[d])):
                        S.dma("sp", lambda tb=tb, dd=dd: nc.sync.dma_start(out=tb[:], in_=dd), writes=[tb])
                S.dma("sp", lambda: nc.sync.dma_start(out=I4[:], in_=I4_d), writes=[I4])
                I4f = sb("I4f", [128, 512], F32)
                bdm = sb("bdm", [128, 512], F32)
                cm = [sb("cm%d" % i, [128, 512], F32) for i in range(4)]
                S.dma("sp", lambda: nc.sync.dma_start(out=I4f[:], in_=I4f_d), writes=[I4f])
                S.dma("sp", lambda: nc.sync.dma_start(out=bdm[:], in_=bdm_d), writes=[bdm])
                for i in range(4):
                    S.dma("sp", lambda i=i: nc.sync.dma_start(out=cm[i][:], in_=cm_d[i]), writes=[cm[i]])
                Sst = [sb("Sst%d" % d, [128, 4, 128], F32) for d in range(2)]
                Sbf = [sb("Sbf%d" % d, [128, 4, 128], BF16) for d in range(2)]
                for d in range(2):
                    S.dma("sp", lambda d=d: nc.sync.dma_start(out=Sst[d][:].rearrange("p a b -> p (a b)"), in_=zf_d), writes=[Sst[d]])
                    S.dma("sp", lambda d=d: nc.sync.dma_start(out=Sbf[d][:].rearrange("p a b -> p (a b)"), in_=zb_d[:, 0:512]), writes=[Sbf[d]])

                class UB:
                    pass
                ubs = []
                for u in range(2):
                    b = UB()
                    n_ = lambda nm: "%s_%d" % (nm, u)
                    b.qT = sb(n_("qT"), [128, 4, 128], BF16); b.kT = sb(n_("kT"), [128, 4, 128], BF16)
                    b.kt = sb(n_("kt"), [128, 512], BF16); b.vt = sb(n_("vt"), [128, 512], BF16)
                    b.gb = sb(n_("gb"), [128, 16], F32)
                    b.Lg = sb(n_("Lg"), [128, 4, 128], F32)
                    b.E1 = sb(n_("E1"), [128, 512], F32); b.E1s = sb(n_("E1s"), [128, 512], F32); b.E1i = sb(n_("E1i"), [128, 512], F32)
                    b.sc = sb(n_("sc"), [128, 40], F32)
                    b.XA = sb(n_("XA"), [128, 8, 128], BF16); b.YA = sb(n_("YA"), [128, 8, 128], BF16)
                    b.XF = [sb(n_("XF%d" % i), [128, 4, 128], F32) for i in range(2)]
                    b.YF = [sb(n_("YF%d" % i), [128, 4, 128], F32) for i in range(2)]
                    b.IYF = sb(n_("IYF"), [128, 4, 128], F32)
                    b.TnF = [sb(n_("TnF%d" % i), [128, 4, 128], F32) for i in range(2)]
                    b.TtF = [sb(n_("TtF%d" % i), [128, 4, 128], F32) for i in range(2)]
                    b.E1sB = sb(n_("E1sB"), [128, 512], F32)
                    b.G = [sb(n_("G%d" % i), [128, 4, 128], BF16) for i in range(4)]
                    b.Bt = sb(n_("Bt"), [128, 4, 256], BF16); b.Xs = sb(n_("Xs"), [128, 4, 256], BF16); b.Vt = sb(n_("Vt"), [128, 4, 256], BF16)
                    b.kd = sb(n_("kd"), [128, 4, 128], BF16)
                    b.wTn = sb(n_("wTn"), [128, 4, 128], BF16); b.vn = sb(n_("vn"), [128, 4, 128], BF16)
                    b.AVs = sb(n_("AVs"), [128, 4, 128], F32); b.o = sb(n_("o"), [128, 4, 128], F32)
                    ubs.append(b)
                F = [ps("F%d" % i, [128, 4, 128], F32) for i in range(7)]
                TPb = ps("TPb", [128, 8, 128], BF16)
                cnt2 = {"u": 0}

                def unit(n, d, srcs, want_o, o_dst):
                    qTd, kTd, kd_, vd_, gbd = srcs
                    b = ubs[cnt2["u"] % 2]
                    cnt2["u"] += 1
                    t0 = n * 128
                    Sd, Sb_ = Sst[d], Sbf[d]
                    S.dma("sp", lambda: nc.sync.dma_start(out=b.qT[:], in_=qTd[:, :, t0:t0 + 128].rearrange("h p t -> p h t")), writes=[b.qT])
                    S.dma("sp", lambda: nc.sync.dma_start(out=b.kT[:], in_=kTd[:, :, t0:t0 + 128].rearrange("h p t -> p h t")), writes=[b.kT])
                    S.dma("sp", lambda: nc.sync.dma_start(out=b.kt[:], in_=kd_[t0:t0 + 128, :]), writes=[b.kt])
                    S.dma("sp", lambda: nc.sync.dma_start(out=b.vt[:], in_=vd_[t0:t0 + 128, :]), writes=[b.vt])
                    S.dma("sp", lambda: nc.sync.dma_start(out=b.gb[:], in_=gbd[t0:t0 + 128, :]), writes=[b.gb])
                    g0 = 8 + 4 * d
                    b0 = 4 * d
                    sc = b.sc
                    for h in range(4):
                        S.op("dve", lambda h=h: nc.vector.tensor_scalar(out=b.Lg[:, h, :], in0=Lm[d][:], scalar1=b.gb[:, g0 + h:g0 + h + 1], scalar2=None, op0=ALU.mult),
                             reads=[Lm[d], b.gb], writes=[b.Lg])
                    for h in range(4):
                        S.op("pe", lambda h=h: nc.tensor.matmul(F[0][:, h, :], lhsT=b.Lg[:, h, :], rhs=Um[d][:], start=True, stop=True),
                             reads=[b.Lg, Um[d]], writes=[F[0]])
                    S.op("pe", lambda: nc.tensor.matmul(F[1][:, 0, 0:4], lhsT=Lm[d][:], rhs=b.gb[:, g0:g0 + 4], start=True, stop=True),
                         reads=[Lm[d], b.gb], writes=[F[1]])
                    S.op("pe", lambda: nc.tensor.matmul(F[1][:, 0, 4:8], lhsT=ones_f[:], rhs=b.gb[:, g0:g0 + 4], start=True, stop=True),
                         reads=[ones_f, b.gb], writes=[F[1]])
                    S.op("act", lambda: nc.scalar.activation(out=b.E1[:], in_=F[0][:].rearrange("p a b -> p (a b)"), func=AF.Exp), reads=[F[0]], writes=[b.E1])
                    S.op("dve", lambda: nc.vector.tensor_copy(out=sc[:, 0:8], in_=F[1][:, 0, 0:8]), reads=[F[1]], writes=[sc])
                    S.op("act", lambda: nc.scalar.activation(out=sc[:, 8:12], in_=sc[:, 0:4], func=AF.Exp), reads=[sc], writes=[sc])
                    S.op("act", lambda: nc.scalar.activation(out=sc[:, 28:32], in_=sc[:, 4:8], func=AF.Exp), reads=[sc], writes=[sc])
                    S.op("dve", lambda: nc.vector.tensor_scalar(out=sc[:, 12:16], in0=sc[:, 8:12], scalar1=QS, scalar2=None, op0=ALU.mult), reads=[sc], writes=[sc])
                    S.op("dve", lambda: nc.vector.tensor_tensor(out=sc[:, 16:20], in0=sc[:, 8:12], in1=b.gb[:, b0:b0 + 4], op=ALU.mult), reads=[sc, b.gb], writes=[sc])
                    S.op("dve", lambda: nc.vector.tensor_scalar(out=sc[:, 20:24], in0=b.gb[:, b0:b0 + 4], scalar1=-1.0, scalar2=None, op0=ALU.mult), reads=[b.gb], writes=[sc])
                    S.op("dve", lambda: nc.vector.tensor_tensor(out=sc[:, 32:36], in0=sc[:, 4:8], in1=sc[:, 0:4], op=ALU.subtract), reads=[sc], writes=[sc])
                    S.op("act", lambda: nc.scalar.activation(out=sc[:, 24:28], in_=sc[:, 32:36], func=AF.Exp), reads=[sc], writes=[sc])
                    S.op("dve", lambda: nc.vector.tensor_tensor(out=b.E1s[:], in0=b.E1[:], in1=mS[d][:], op=ALU.mult), reads=[b.E1, mS[d]], writes=[b.E1s])
                    S.op("dve", lambda: nc.vector.tensor_tensor(out=b.E1i[:], in0=b.E1[:], in1=mI[d][:], op=ALU.mult), reads=[b.E1, mI[d]], writes=[b.E1i])
                    for h in range(4):
                        S.op("pe", lambda h=h: nc.tensor.matmul(F[2][:, h, :], lhsT=b.kT[:, h, :], rhs=b.kT[:, h, :], start=True, stop=True),
                             reads=[b.kT], writes=[F[2]])
                        S.op("pe", lambda h=h: nc.tensor.matmul(F[3][:, h, :], lhsT=b.qT[:, h, :], rhs=b.kT[:, h, :], start=True, stop=True),
                             reads=[b.qT, b.kT], writes=[F[3]])
                    for h in range(4):
                        S.op("dve", lambda h=h: nc.vector.scalar_tensor_tensor(out=b.XA[:, h, :], in0=F[2][:, h, :], scalar=sc[:, 20 + h:21 + h],
                                                                                in1=b.E1s[:, h * 128:(h + 1) * 128], op0=ALU.mult, op1=ALU.mult),
                             reads=[F[2], sc, b.E1s], writes=[b.XA])
                    S.op("dve", lambda: nc.vector.scalar_tensor_tensor(out=b.XA[:, 4:8, :].rearrange("p a b -> p (a b)"), in0=F[3][:].rearrange("p a b -> p (a b)"),
                                                                        scalar=QS, in1=b.E1i[:], op0=ALU.mult, op1=ALU.mult),
                         reads=[F[3], b.E1i], writes=[b.XA])
                    for i in range(8):
                        S.op("pe", lambda i=i: nc.tensor.transpose(TPb[:, i, :], b.XA[:, i, :], ident_bf[:]), reads=[b.XA, ident_bf], writes=[TPb])
                    S.op("dve", lambda: nc.vector.tensor_copy(out=b.YA[:], in_=TPb[:]), reads=[TPb], writes=[b.YA])
                    i4f = I4f[:].rearrange("p (a b) -> p a b", b=128)
                    S.op("dve", lambda: nc.vector.tensor_tensor(out=b.E1sB[:], in0=b.E1s[:], in1=bdm[:], op=ALU.mult), reads=[b.E1s, bdm], writes=[b.E1sB])
                    for h in range(4):
                        S.op("dve", lambda h=h: nc.vector.scalar_tensor_tensor(out=b.XF[0][:, h, :], in0=F[2][:, h, :], scalar=sc[:, 20 + h:21 + h],
                                                                                in1=b.E1sB[:, h * 128:(h + 1) * 128], op0=ALU.mult, op1=ALU.mult),
                             reads=[F[2], sc, b.E1sB], writes=[b.XF[0]])
                    for h in range(4):
                        S.op("pe", lambda h=h: nc.tensor.transpose(F[4][:, h, :], b.XF[0][:, h, :], ident_f[:]), reads=[b.XF[0], ident_f], writes=[F[4]])
                    S.op("dve", lambda: nc.vector.tensor_copy(out=b.YF[0][:], in_=F[4][:]), reads=[F[4]], writes=[b.YF[0]])
                    S.op("dve", lambda: nc.vector.tensor_tensor(out=b.TtF[0][:], in0=b.YF[0][:], in1=i4f, op=ALU.add), reads=[b.YF[0], I4f], writes=[b.TtF[0]])
                    S.op("dve", lambda: nc.vector.tensor_tensor(out=b.TnF[0][:], in0=b.XF[0][:], in1=i4f, op=ALU.add), reads=[b.XF[0], I4f], writes=[b.TnF[0]])
                    Xc, Yc = b.XF[0], b.YF[0]
                    cur = 0
                    for lev in range(1, 5):
                        last = (lev == 4)
                        Xn, Yn = b.XF[lev % 2], b.YF[lev % 2]
                        for h in range(4):
                            S.op("pe", lambda h=h: nc.tensor.matmul(F[2][:, h, :], lhsT=Xc[:, h, :], rhs=Yc[:, h, :], start=True, stop=True),
                                 reads=[Xc, Yc], writes=[F[2]])
                        if not last:
                            for h in range(4):
                                S.op("pe", lambda h=h: nc.tensor.matmul(F[3][:, h, :], lhsT=Yc[:, h, :], rhs=Xc[:, h, :], start=True, stop=True),
                                     reads=[Xc, Yc], writes=[F[3]])
                        S.op("dve", lambda: nc.vector.tensor_tensor(out=b.IYF[:], in0=F[2][:], in1=i4f, op=ALU.add), reads=[F[2], I4f], writes=[b.IYF])
                        if not last:
                            S.op("act", lambda: nc.scalar.copy(out=Yn[:], in_=F[2][:]), reads=[F[2]], writes=[Yn])
                            S.op("act", lambda: nc.scalar.copy(out=Xn[:], in_=F[3][:]), reads=[F[3]], writes=[Xn])
                        Tn_o, Tt_n, Tn_n = b.TnF[cur], b.TtF[1 - cur], b.TnF[1 - cur]
                        for h in range(4):
                            S.op("pe", lambda h=h: nc.tensor.matmul(F[4][:, h, :], lhsT=Tn_o[:, h, :], rhs=b.IYF[:, h, :], start=True, stop=True),
                                 reads=[Tn_o, b.IYF], writes=[F[4]])
                        if not last:
                            for h in range(4):
                                S.op("pe", lambda h=h: nc.tensor.matmul(F[5][:, h, :], lhsT=b.IYF[:, h, :], rhs=Tn_o[:, h, :], start=True, stop=True),
                                     reads=[Tn_o, b.IYF], writes=[F[5]])
                        S.op("dve", lambda: nc.vector.tensor_copy(out=Tt_n[:], in_=F[4][:]), reads=[F[4]], writes=[Tt_n])
                        if not last:
                            S.op("act", lambda: nc.scalar.copy(out=Tn_n[:], in_=F[5][:]), reads=[F[5]], writes=[Tn_n])
                        cur = 1 - cur
                        Xc, Yc = Xn, Yn
                    DgT = b.TtF[cur]
                    for I_ in range(4):
                        S.op("dve", lambda I_=I_: nc.vector.tensor_tensor(out=b.G[I_][:].rearrange("p a b -> p (a b)"), in0=DgT[:].rearrange("p a b -> p (a b)"),
                                                                          in1=cm[I_][:], op=ALU.mult), reads=[DgT, cm[I_]], writes=[b.G[I_]])
                    for h in range(4):
                        S.op("dve", lambda h=h: nc.vector.tensor_scalar(out=b.Bt[:, h, 0:128], in0=b.vt[:, h * 128:(h + 1) * 128], scalar1=b.gb[:, b0 + h:b0 + h + 1], scalar2=None, op0=ALU.mult),
                             reads=[b.vt, b.gb], writes=[b.Bt])
                        S.op("act", lambda h=h: nc.scalar.activation(out=b.Bt[:, h, 128:256], in_=b.kt[:, h * 128:(h + 1) * 128], func=AF.Copy, scale=sc[:, 16 + h:17 + h]),
                             reads=[b.kt, sc], writes=[b.Bt])
                        S.op("act", lambda h=h: nc.scalar.activation(out=b.kd[:, h, :], in_=b.kt[:, h * 128:(h + 1) * 128], func=AF.Copy, scale=sc[:, 24 + h:25 + h]),
                             reads=[b.kt, sc], writes=[b.kd])

                    def pv(h):
                        return F[2 + h // 2][:].rearrange("p a b -> p (a b)")[:, (h % 2) * 256:(h % 2 + 1) * 256]

                    def px(h):
                        return F[4 + h // 2][:].rearrange("p a b -> p (a b)")[:, (h % 2) * 256:(h % 2 + 1) * 256]

                    def fl(t_, lo, hi):
                        return t_[:, lo:hi, :].rearrange("p a b -> p (a b)")

                    for step_, I_ in enumerate((0, 1, 2, 3) if d == 0 else (3, 2, 1, 0)):
                        if step_ == 0:
                            rhs_t = b.Bt
                        else:
                            for h in range(4):
                                S.op("pe", lambda h=h: nc.tensor.matmul(pv(h), lhsT=b.YA[:, h, :], rhs=b.Xs[:, h, :], start=True, stop=True),
                                     reads=[b.YA, b.Xs], writes=[F[2 + h // 2]])
                            for q_ in range(2):
                                S.op("dve", lambda q_=q_: nc.vector.tensor_tensor(out=fl(b.Vt, 2 * q_, 2 * q_ + 2), in0=F[2 + q_][:].rearrange("p a b -> p (a b)"),
                                                                                  in1=fl(b.Bt, 2 * q_, 2 * q_ + 2), op=ALU.add), reads=[F[2 + q_], b.Bt], writes=[b.Vt])
                            rhs_t = b.Vt
                        for h in range(4):
                            S.op("pe", lambda h=h: nc.tensor.matmul(px(h), lhsT=b.G[I_][:, h, :], rhs=rhs_t[:, h, :], start=True, stop=True),
                                 reads=[b.G[I_], rhs_t], writes=[F[4 + h // 2]])
                        for q_ in range(2):
                            if step_ == 0:
                                S.op("dve", lambda q_=q_: nc.vector.tensor_copy(out=fl(b.Xs, 2 * q_, 2 * q_ + 2), in_=F[4 + q_][:].rearrange("p a b -> p (a b)")),
                                     reads=[F[4 + q_]], writes=[b.Xs])
                            else:
                                S.op("dve", lambda q_=q_: nc.vector.tensor_tensor(out=fl(b.Xs, 2 * q_, 2 * q_ + 2), in0=F[4 + q_][:].rearrange("p a b -> p (a b)"),
                                                                                  in1=fl(b.Xs, 2 * q_, 2 * q_ + 2), op=ALU.add), reads=[F[4 + q_], b.Xs], writes=[b.Xs])
                    for h in range(4):
                        S.op("pe", lambda h=h: nc.tensor.transpose(TPb[:, h, :], b.Xs[:, h, 128:256], ident_bf[:]), reads=[b.Xs, ident_bf], writes=[TPb])
                    S.op("dve", lambda: nc.vector.tensor_scalar(out=b.wTn[:], in0=TPb[:, 0:4, :], scalar1=-1.0, scalar2=None, op0=ALU.mult), reads=[TPb], writes=[b.wTn])
                    for h in range(4):
                        S.op("pe", lambda h=h: nc.tensor.matmul(F[6][:, h, :], lhsT=b.wTn[:, h, :], rhs=Sb_[:, h, :], start=True, stop=True),
                             reads=[b.wTn, Sb_], writes=[F[6]])
                    S.op("dve", lambda: nc.vector.tensor_tensor(out=b.vn[:], in0=F[6][:], in1=b.Xs[:, :, 0:128], op=ALU.add), reads=[F[6], b.Xs], writes=[b.vn])
                    if want_o:
                        for h in range(4):
                            S.op("pe", lambda h=h: nc.tensor.matmul(F[4][:, h, :], lhsT=b.qT[:, h, :], rhs=Sb_[:, h, :], start=True, stop=True),
                                 reads=[b.qT, Sb_], writes=[F[4]])
                            S.op("pe", lambda h=h: nc.tensor.matmul(F[5][:, h, :], lhsT=b.YA[:, 4 + h, :], rhs=b.vn[:, h, :], start=True, stop=True),
                                 reads=[b.YA, b.vn], writes=[F[5]])
                        S.op("act", lambda: nc.scalar.copy(out=b.AVs[:], in_=F[5][:]), reads=[F[5]], writes=[b.AVs])
                        for h in range(4):
                            S.op("dve", lambda h=h: nc.vector.scalar_tensor_tensor(out=b.o[:, h, :], in0=F[4][:, h, :], scalar=sc[:, 12 + h:13 + h], in1=b.AVs[:, h, :],
                                                                                    op0=ALU.mult, op1=ALU.add), reads=[F[4], sc, b.AVs], writes=[b.o])
                        S.dma("pool", lambda: nc.gpsimd.dma_start(out=o_dst[t0:t0 + 128, :], in_=b.o[:].rearrange("p a b -> p (a b)")), reads=[b.o])
                    for h in range(4):
                        S.op("pe", lambda h=h: nc.tensor.matmul(F[1][:, h, :], lhsT=b.kd[:, h, :], rhs=b.vn[:, h, :], start=True, stop=True),
                             reads=[b.kd, b.vn], writes=[F[1]])
                    for h in range(4):
                        S.op("dve", lambda h=h: nc.vector.scalar_tensor_tensor(out=Sd[:, h, :], in0=Sd[:, h, :], scalar=sc[:, 28 + h:29 + h], in1=F[1][:, h, :],
                                                                                op0=ALU.mult, op1=ALU.add), reads=[Sd, sc, F[1]], writes=[Sd])
                    S.op("act", lambda: nc.scalar.copy(out=Sb_[:], in_=Sd[:]), reads=[Sd], writes=[Sb_])

                csrc = (cqT_d, ckT_d, ck_d, cv_d, cgb_d)
                msrc = (qT_d, kT_d, k_d, v_d, gb_d)
                if ulimit is not None:
                    for (n_, d_) in ulimit:
                        unit(n_, d_, csrc, False, None)
                    S.barrier()
                    return True
                for n in range(2):
                    unit(n, 0, csrc, False, None)
                    unit(1 - n, 1, csrc, False, None)
                if dbg:
                    for d in range(2):
                        S.dma("pool", lambda d=d: nc.gpsimd.dma_start(out=s0_d[d], in_=Sst[d][:]), reads=[Sst[d]])
                for s_ in range(nch):
                    unit(s_, 0, msrc, True, of_d)
                    unit(nch - 1 - s_, 1, msrc, True, ob_d)
                    if s_ in (21, 43):
                        S.new_epoch()
                S.new_epoch()
            return False

        if 2 in run:
            if stage2():
                return nc, S

        def stage3():
            st3 = contextlib.ExitStack()
            with st3:
                sb = lambda name, shape, dt: mk(st3, "sb", name, shape, dt)
                ps = lambda name, shape, dt: mk(st3, "ps", name, shape, dt)
                cosA = sb("cosA", [128, 64, 128], BF16)
                sinA = sb("sinA", [128, 64, 128], BF16)
                nsinA = sb("nsinA", [128, 64, 128], BF16)
                cs64 = sb("cs64", [128, 64], BF16)
                S.dma("sp", lambda: nc.sync.dma_start(out=cosA[:], in_=cosA_d), writes=[cosA])
                S.dma("sp", lambda: nc.sync.dma_start(out=sinA[:], in_=sinA_d), writes=[sinA])
                S.dma("sp", lambda: nc.sync.dma_start(out=nsinA[:], in_=nsinA_d), writes=[nsinA])
                S.dma("sp", lambda: nc.sync.dma_start(out=cs64[:], in_=cs64_d), writes=[cs64])
                fm = sb("fm", [128, 4, T], BF16)
                pqa = [sb("pqa%d" % i, [128, 1024], BF16) for i in range(2)]
                zs = [sb("zs3_%d" % i, [128, 2, 512], BF16) for i in range(2)]
                zin = [sb("zin%d" % i, [128, 16, 512], BF16) for i in range(2)]
                P = [ps("P3_%d" % i, [128, 512], F32) for i in range(4)]
                pq_v = pq_d.rearrange("(b a) c -> a b c", a=64)
                for a in range(64):
                    pb = pqa[a % 2]
                    zb = zs[a % 2]
                    S.dma("sp", lambda: nc.sync.dma_start(out=pb[:], in_=pq_v[a]), writes=[pb])
                    pr, pi = P[(a % 2) * 2], P[(a % 2) * 2 + 1]
                    S.op("pe", lambda: nc.tensor.matmul(pr[:], lhsT=cosA[:, a, :], rhs=pb[:, 0:512], start=True, stop=False), reads=[cosA, pb], writes=[pr])
                    S.op("pe", lambda: nc.tensor.matmul(pr[:], lhsT=nsinA[:, a, :], rhs=pb[:, 512:1024], start=False, stop=True), reads=[nsinA, pb], writes=[pr])
                    S.op("pe", lambda: nc.tensor.matmul(pi[:], lhsT=cosA[:, a, :], rhs=pb[:, 512:1024], start=True, stop=False), reads=[cosA, pb], writes=[pi])
                    S.op("pe", lambda: nc.tensor.matmul(pi[:], lhsT=sinA[:, a, :], rhs=pb[:, 0:512], start=False, stop=True), reads=[sinA, pb], writes=[pi])
                    S.op("dve", lambda: nc.vector.tensor_copy(out=zb[:, 0, :], in_=pr[:]), reads=[pr], writes=[zb])
                    S.op("act", lambda: nc.scalar.copy(out=zb[:, 1, :], in_=pi[:]), reads=[pi], writes=[zb])
                    S.dma("pool", lambda: nc.gpsimd.dma_start(out=Zd[:, a, :, :].rearrange("r p c -> p r c"), in_=zb[:]), reads=[zb])
                S.barrier()
                zv = Zd.rearrange("r a b c -> (r a) b c")
                for g in range(8):
                    zi = zin[g % 2]
                    S.dma("sp", lambda: nc.sync.dma_start(out=zi[:], in_=zv[:, g * 16:(g + 1) * 16, :]), writes=[zi])
                    for half in range(2):
                        for ct in range(4):
                            pp = P[(half * 4 + ct) % 4]
                            ppv = pp[:].rearrange("p (b a) -> p b a", a=64)
                            for bl in range(8):
                                S.op("pe", lambda bl=bl: nc.tensor.matmul(ppv[:, bl, :], lhsT=zi[:, half * 8 + bl, ct * 128:(ct + 1) * 128], rhs=cs64[:],
                                                                          start=True, stop=True), reads=[zi, cs64], writes=[pp])
                            b0 = g * 16 + half * 8
                            ov = fm[:, ct, :].rearrange("p (a b) -> p b a", b=128)[:, b0:b0 + 8, :]
                            if ct % 2 == 0:
                                S.op("dve", lambda: nc.vector.tensor_scalar(out=ov, in0=ppv, scalar1=1.0 / 1024.0, scalar2=None, op0=ALU.mult), reads=[pp], writes=[fm])
                            else:
                                S.op("act", lambda: nc.scalar.activation(out=ov, in_=ppv, func=AF.Copy, scale=1.0 / 1024.0), reads=[pp], writes=[fm])
                for ct in range(4):
                    S.dma("pool", lambda ct=ct: nc.gpsimd.dma_start(out=fmT_d[ct], in_=fm[:, ct, :]), reads=[fm])
                S.barrier()
            return False

        if 3 in run:
            if stage3():
                return nc, S

        def stage4():
            st4 = contextlib.ExitStack()
            with st4:
                sb = lambda name, shape, dt: mk(st4, "sb", name, shape, dt)
                ps = lambda name, shape, dt: mk(st4, "ps", name, shape, dt)
                comb = sb("comb", [128, NT4, 32], F32)
                gate2b = sb("gate2b", [128, D], F32)
                fingb = sb("fingb", [128, D], F32)
                S.dma("sp", lambda: nc.sync.dma_start(out=gate2b[:], in_=modrow_d[0:1, 5120:6144].partition_broadcast(128)), writes=[gate2b])
                S.dma("sp", lambda: nc.sync.dma_start(out=fingb[:], in_=fing_d.partition_broadcast(128)), writes=[fingb])
                junk4 = sb("junk4", [128, D], BF16)
                PS = [ps("P4_%d" % i, [128, 512], F32) for i in range(7)]
                PTb = ps("PTb4", [128, 8, 128], BF16)
                sa4 = contextlib.ExitStack()
                sa4.__enter__()
                sA = lambda name, shape, dt: mk(sa4, "sb", name, shape, dt)
                gate1b = sA("gate1b", [128, D], F32); gs2b = sA("gs2b", [128, D], F32); sh2b = sA("sh2b", [128, D], F32)
                n2gb = sA("n2gb", [128, D], F32)
                S.dma("sp", lambda: nc.sync.dma_start(out=gate1b[:], in_=modrow_d[0:1, 2048:3072].partition_broadcast(128)), writes=[gate1b])
                S.dma("sp", lambda: nc.sync.dma_start(out=sh2b[:], in_=modrow_d[0:1, 3072:4096].partition_broadcast(128)), writes=[sh2b])
                S.dma("sp", lambda: nc.sync.dma_start(out=gs2b[:], in_=modrow_d[0:1, 4096:5120].partition_broadcast(128)), writes=[gs2b])
                S.dma("sp", lambda: nc.sync.dma_start(out=n2gb[:], in_=n2g_d.partition_broadcast(128)), writes=[n2gb])
                S.op("dve", lambda: nc.vector.scalar_tensor_tensor(out=gs2b[:], in0=gs2b[:], scalar=1.0, in1=n2gb[:], op0=ALU.add, op1=ALU.mult),
                     reads=[gs2b, n2gb], writes=[gs2b])
                og4 = sA("og4", [128, 512], F32)
                for h in range(4):
                    S.dma("sp", lambda h=h: nc.sync.dma_start(out=og4[:, h * 128:(h + 1) * 128], in_=onorm_d.partition_broadcast(128)), writes=[og4])
                brb = sA("brb", [128, 36], F32)
                S.dma("sp", lambda: nc.sync.dma_start(out=brb[:, 0:4], in_=bgrp_d.partition_broadcast(128)), writes=[brb])
                S.dma("sp", lambda: nc.sync.dma_start(out=brb[:, 4:36], in_=brt_d.partition_broadcast(128)), writes=[brb])
                wrg = sA("wrg", [128, 8, 36], F32)
                with nc.allow_non_contiguous_dma(reason="tiny router weights"):
                    S.dma("sp", lambda: nc.sync.dma_start(out=wrg[:, :, 0:4], in_=wgrp_d.rearrange("(k p) n -> p k n", p=128)), writes=[wrg])
                    S.dma("sp", lambda: nc.sync.dma_start(out=wrg[:, :, 4:36], in_=wrt_d.rearrange("(k p) n -> p k n", p=128)), writes=[wrg])
                w_out = sA("w_out", [128, 8, D], BF16)
                S.dma("pool", lambda: nc.gpsimd.dma_start(out=w_out[:], in_=w_out_d.rearrange("(k p) n -> p k n", p=128)), writes=[w_out])
                mixT = [sA("mixT%d" % i, [128, 8, 512], BF16) for i in range(2)]
                oft = [sA("oft%d" % i, [128, 512], F32) for i in range(2)]
                obt = [sA("obt%d" % i, [128, 512], F32) for i in range(2)]
                zt = [sA("zt%d" % i, [128, 512], BF16) for i in range(2)]
                gz = sA("gz", [128, 512], F32)
                ogb = sA("ogb", [128, 512], BF16)
                s4 = [sA("s4_%d" % i, [128, 48], F32) for i in range(2)]
                xt4 = [sA("xt4_%d" % i, [128, D], F32) for i in range(2)]
                x1t = [sA("x1t%d" % i, [128, D], F32) for i in range(2)]
                h2t = [sA("h2t%d" % i, [128, D], F32) for i in range(2)]
                h2T32 = sA("h2T32", [128, 8, 128], F32)
                h2Tb = [sA("h2Tb%d" % i, [128, 8, 128], BF16) for i in range(2)]
                lg = [sA("lg%d" % i, [128, 36], F32) for i in range(2)]
                lm = sA("lm", [128, 32], F32); lm2 = sA("lm2", [128, 32], F32)
                sel1 = sA("sel1", [128, 32], F32); sel2 = sA("sel2", [128, 32], F32)
                for j in range(NT4 // 4):
                    mT = mixT[j % 2]
                    for ct in range(4):
                        S.dma("sp", lambda ct=ct: nc.sync.dma_start(out=mT[:, ct, :], in_=fmT_d[ct, :, j * 512:(j + 1) * 512]), writes=[mT])
                    for tt in range(4):
                        ti = j * 4 + tt
                        r0 = ti * 128
                        a_, b_, z_, sc_ = oft[ti % 2], obt[ti % 2], zt[ti % 2], s4[ti % 2]
                        S.dma("sp", lambda: nc.sync.dma_start(out=a_[:], in_=of_d[r0:r0 + 128, :]), writes=[a_])
                        S.dma("sp", lambda: nc.sync.dma_start(out=b_[:], in_=ob_d[r0:r0 + 128, :]), writes=[b_])
                        S.dma("sp", lambda: nc.sync.dma_start(out=z_[:], in_=z_d[r0:r0 + 128, :]), writes=[z_])
                        S.op("dve", lambda: nc.vector.tensor_tensor(out=a_[:], in0=a_[:], in1=b_[:], op=ALU.add), reads=[a_, b_], writes=[a_])
                        for h in range(4):
                            S.op("act", lambda h=h: nc.scalar.activation(out=junk4[:, 0:128], in_=a_[:, h * 128:(h + 1) * 128], func=AF.Square, accum_out=sc_[:, h:h + 1]),
                                 reads=[a_], writes=[junk4, sc_])
                        S.op("dve", lambda: nc.vector.tensor_scalar(out=sc_[:, 0:4], in0=sc_[:, 0:4], scalar1=1.0 / 128, scalar2=EPS, op0=ALU.mult, op1=ALU.add), reads=[sc_], writes=[sc_])
                        S.op("act", lambda: nc.scalar.sqrt(out=sc_[:, 0:4], in_=sc_[:, 0:4]), reads=[sc_], writes=[sc_])
                        S.op("dve", lambda: nc.vector.reciprocal(out=sc_[:, 0:4], in_=sc_[:, 0:4]), reads=[sc_], writes=[sc_])
                        S.op("dve", lambda: nc.vector.tensor_tensor(out=gz[:], in0=z_[:], in1=og4[:], op=ALU.mult), reads=[z_, og4], writes=[gz])
                        for h in range(4):
                            S.op("dve", lambda h=h: nc.vector.scalar_tensor_tensor(out=ogb[:, h * 128:(h + 1) * 128], in0=a_[:, h * 128:(h + 1) * 128], scalar=sc_[:, h:h + 1],
                                                                                    in1=gz[:, h * 128:(h + 1) * 128], op0=ALU.mult, op1=ALU.mult), reads=[a_, sc_, gz], writes=[ogb])
                        for h in range(4):
                            S.op("pe", lambda h=h: nc.tensor.transpose(PTb[:, h, :], ogb[:, h * 128:(h + 1) * 128], ident_bf[:]), reads=[ogb, ident_bf], writes=[PTb])
                        S.op("dve", lambda: nc.vector.tensor_copy(out=mT[:, 4:8, tt * 128:(tt + 1) * 128], in_=PTb[:, 0:4, :]), reads=[PTb], writes=[mT])
                    for tt in range(4):
                        ti = j * 4 + tt
                        r0 = ti * 128
                        xb, x1b, h2b, sc_ = xt4[ti % 2], x1t[ti % 2], h2t[ti % 2], s4[ti % 2]
                        S.dma("sp", lambda: nc.sync.dma_start(out=xb[:], in_=x_d[r0:r0 + 128, :]), writes=[xb])
                        for half in range(2):
                            pp = PS[half]
                            for k in range(8):
                                S.op("pe", lambda k=k: nc.tensor.matmul(pp[:], lhsT=mT[:, k, tt * 128:(tt + 1) * 128], rhs=w_out[:, k, half * 512:(half + 1) * 512],
                                                                        start=(k == 0), stop=(k == 7)), reads=[mT, w_out], writes=[pp])
                            hs = slice(half * 512, (half + 1) * 512)
                            S.op("dve", lambda: nc.vector.tensor_tensor(out=x1b[:, hs], in0=pp[:], in1=gate1b[:, hs], op=ALU.mult), reads=[pp, gate1b], writes=[x1b])
                        S.op("dve", lambda: nc.vector.tensor_tensor(out=x1b[:], in0=x1b[:], in1=xb[:], op=ALU.add), reads=[x1b, xb], writes=[x1b])
                        S.dma("pool", lambda: nc.gpsimd.dma_start(out=x1_d[r0:r0 + 128, :], in_=x1b[:]), reads=[x1b])
                        S.op("act", lambda: nc.scalar.activation(out=junk4[:], in_=x1b[:], func=AF.Square, accum_out=sc_[:, 8:9]), reads=[x1b], writes=[junk4, sc_])
                        S.op("dve", lambda: nc.vector.tensor_scalar(out=sc_[:, 8:9], in0=sc_[:, 8:9], scalar1=1.0 / D, scalar2=EPS, op0=ALU.mult, op1=ALU.add), reads=[sc_], writes=[sc_])
                        S.op("act", lambda: nc.scalar.sqrt(out=sc_[:, 8:9], in_=sc_[:, 8:9]), reads=[sc_], writes=[sc_])
                        S.op("dve", lambda: nc.vector.reciprocal(out=sc_[:, 8:9], in_=sc_[:, 8:9]), reads=[sc_], writes=[sc_])
                        S.op("dve", lambda: nc.vector.scalar_tensor_tensor(out=h2b[:], in0=x1b[:], scalar=sc_[:, 8:9], in1=gs2b[:], op0=ALU.mult, op1=ALU.mult),
                             reads=[x1b, sc_, gs2b], writes=[h2b])
                        S.op("dve", lambda: nc.vector.tensor_tensor(out=h2b[:], in0=h2b[:], in1=sh2b[:], op=ALU.add), reads=[h2b, sh2b], writes=[h2b])
                        for k in range(8):
                            pp = PS[2 + k // 4]
                            S.op("pe", lambda k=k, pp=pp: nc.tensor.transpose(pp[:, (k % 4) * 128:(k % 4 + 1) * 128], h2b[:, k * 128:(k + 1) * 128], ident_f[:]),
                                 reads=[h2b, ident_f], writes=[pp])
                        hb = h2Tb[ti % 2]
                        for q_ in range(2):
                            pp = PS[2 + q_]
                            S.op("dve", lambda: nc.vector.tensor_copy(out=h2T32[:, q_ * 4:(q_ + 1) * 4, :].rearrange("p a b -> p (a b)"), in_=pp[:]), reads=[pp], writes=[h2T32])
                            S.op("act", lambda: nc.scalar.copy(out=hb[:, q_ * 4:(q_ + 1) * 4, :].rearrange("p a b -> p (a b)"), in_=pp[:]), reads=[pp], writes=[hb])
                        S.dma("pool", lambda: nc.gpsimd.dma_start(out=h2T_d[:, :, r0:r0 + 128].rearrange("k p t -> p k t"), in_=hb[:]), reads=[hb])
                        lt = lg[ti % 2]
                        for k in range(8):
                            S.op("pe", lambda k=k: nc.tensor.matmul(PS[4][:, 0:36], lhsT=h2T32[:, k, :], rhs=wrg[:, k, :], start=(k == 0), stop=(k == 7)),
                                 reads=[h2T32, wrg], writes=[PS[4]])
                        S.op("dve", lambda: nc.vector.tensor_tensor(out=lt[:], in0=PS[4][:, 0:36], in1=brb[:], op=ALU.add), reads=[PS[4], brb], writes=[lt])
                        S.op("dve", lambda: nc.vector.tensor_reduce(out=sc_[:, 16:17], in_=lt[:, 0:4], axis=AX.X, op=ALU.max), reads=[lt], writes=[sc_])
                        S.op("dve", lambda: nc.vector.tensor_scalar(out=sc_[:, 17:18], in0=sc_[:, 16:17], scalar1=-1.0, scalar2=None, op0=ALU.mult), reads=[sc_], writes=[sc_])
                        S.op("act", lambda: nc.scalar.activation(out=sc_[:, 36:40], in_=lt[:, 0:4], func=AF.Exp, bias=sc_[:, 17:18], scale=1.0, accum_out=sc_[:, 18:19]),
                             reads=[lt, sc_], writes=[sc_])
                        S.op("dve", lambda: nc.vector.reciprocal(out=sc_[:, 19:20], in_=sc_[:, 18:19]), reads=[sc_], writes=[sc_])
                        S.op("dve", lambda: nc.vector.tensor_scalar(out=sc_[:, 20:24], in0=lt[:, 0:4], scalar1=sc_[:, 16:17], scalar2=None, op0=ALU.is_equal), reads=[lt, sc_], writes=[sc_])
                        S.op("dve", lambda: nc.vector.tensor_scalar(out=sc_[:, 24:28], in0=sc_[:, 20:24], scalar1=-1.0, scalar2=1e30, op0=ALU.add, op1=ALU.mult), reads=[sc_], writes=[sc_])
                        for g in range(4):
                            S.op("dve", lambda g=g: nc.vector.tensor_scalar(out=lm[:, g * 8:(g + 1) * 8], in0=lt[:, 4 + g * 8:12 + g * 8], scalar1=sc_[:, 20 + g:21 + g],
                                                                             scalar2=sc_[:, 24 + g:25 + g], op0=ALU.mult, op1=ALU.add), reads=[lt, sc_], writes=[lm])
                        S.op("dve", lambda: nc.vector.tensor_reduce(out=sc_[:, 28:29], in_=lm[:], axis=AX.X, op=ALU.max), reads=[lm], writes=[sc_])
                        S.op("dve", lambda: nc.vector.tensor_scalar(out=sel1[:], in0=lm[:], scalar1=sc_[:, 28:29], scalar2=None, op0=ALU.is_equal), reads=[lm, sc_], writes=[sel1])
                        S.op("dve", lambda: nc.vector.scalar_tensor_tensor(out=lm2[:], in0=sel1[:], scalar=-1e30, in1=lm[:], op0=ALU.mult, op1=ALU.add), reads=[sel1, lm], writes=[lm2])
                        S.op("dve", lambda: nc.vector.tensor_reduce(out=sc_[:, 29:30], in_=lm2[:], axis=AX.X, op=ALU.max), reads=[lm2], writes=[sc_])
                        S.op("dve", lambda: nc.vector.tensor_scalar(out=sel2[:], in0=lm2[:], scalar1=sc_[:, 29:30], scalar2=None, op0=ALU.is_equal), reads=[lm2, sc_], writes=[sel2])
                        S.op("dve", lambda: nc.vector.tensor_tensor(out=sc_[:, 30:31], in0=sc_[:, 28:29], in1=sc_[:, 29:30], op=ALU.subtract), reads=[sc_], writes=[sc_])
                        S.op("act", lambda: nc.scalar.activation(out=sc_[:, 31:32], in_=sc_[:, 30:31], func=AF.Sigmoid), reads=[sc_], writes=[sc_])
                        S.op("dve", lambda: nc.vector.tensor_tensor(out=sc_[:, 31:32], in0=sc_[:, 31:32], in1=sc_[:, 19:20], op=ALU.mult), reads=[sc_], writes=[sc_])
                        S.op("dve", lambda: nc.vector.tensor_tensor(out=sc_[:, 32:33], in0=sc_[:, 19:20], in1=sc_[:, 31:32], op=ALU.subtract), reads=[sc_], writes=[sc_])
                        S.op("dve", lambda: nc.vector.tensor_scalar(out=comb[:, ti, :], in0=sel1[:], scalar1=sc_[:, 31:32], scalar2=None, op0=ALU.mult), reads=[sel1, sc_], writes=[comb])
                        S.op("dve", lambda: nc.vector.scalar_tensor_tensor(out=comb[:, ti, :], in0=sel2[:], scalar=sc_[:, 32:33], in1=comb[:, ti, :], op0=ALU.mult, op1=ALU.add),
                             reads=[sel2, sc_, comb], writes=[comb])
                if dbg:
                    S.dma("pool", lambda: nc.gpsimd.dma_start(out=comb_d[0:NT4 * 128, :].rearrange("(t p) e -> p t e", p=128), in_=comb[:]), reads=[comb])
                S.new_epoch()
                sa4.__exit__(None, None, None)
                if stages <= 3.5:
                    return True
                NG = max(1, NT4 // 16)
                TPG = NT4 // NG
                acc = sb("acc", [128, TPG, D], F32)
                wg = [sb("wg%d" % i, [128, 8, 512], BF16) for i in range(2)]
                wu = [sb("wu%d" % i, [128, 8, 512], BF16) for i in range(2)]
                wd = [sb("wd%d" % i, [128, 4, D], BF16) for i in range(2)]
                hblk = [sb("hblk%d" % i, [128, 8, 512], BF16) for i in range(2)]
                sg = [sb("sg%d" % i, [128, 512], F32) for i in range(2)]
                hid = [sb("hid%d" % i, [128, 4, 512], BF16) for i in range(2)]
                x1f = [sb("x1f%d" % i, [128, D], F32) for i in range(2)]
                sf = [sb("sf%d" % i, [128, 4], F32) for i in range(2)]
                cnt4 = {"h": 0}
                for grp in range(NG):
                    for e in range(NEXP):
                        g_, u_, d_ = wg[e % 2], wu[e % 2], wd[e % 2]
                        S.dma("pool", lambda: nc.gpsimd.dma_start(out=g_[:], in_=wgate_d[e].rearrange("(k p) n -> p k n", p=128)), writes=[g_])
                        S.dma("pool", lambda: nc.gpsimd.dma_start(out=u_[:], in_=wup_d[e].rearrange("(k p) n -> p k n", p=128)), writes=[u_])
                        S.dma("pool", lambda: nc.gpsimd.dma_start(out=d_[:], in_=wdown_d[e].rearrange("(k p) n -> p k n", p=128)), writes=[d_])
                        for jb in range(TPG // 4):
                            t0 = (grp * TPG + jb * 4) * 128
                            hb = hblk[cnt4["h"] % 2]
                            hd = hid[cnt4["h"] % 2]
                            cnt4["h"] += 1
                            S.dma("sp", lambda: nc.sync.dma_start(out=hb[:], in_=h2T_d[:, :, t0:t0 + 512].rearrange("k p t -> p k t")), writes=[hb])
                            for hc in range(4):
                                pg_, pu_ = PS[(hc % 2) * 2], PS[(hc % 2) * 2 + 1]
                                for k in range(8):
                                    S.op("pe", lambda k=k: nc.tensor.matmul(pg_[:], lhsT=g_[:, k, hc * 128:(hc + 1) * 128], rhs=hb[:, k, :], start=(k == 0), stop=(k == 7)),
                                         reads=[g_, hb], writes=[pg_])
                                for k in range(8):
                                    S.op("pe", lambda k=k: nc.tensor.matmul(pu_[:], lhsT=u_[:, k, hc * 128:(hc + 1) * 128], rhs=hb[:, k, :], start=(k == 0), stop=(k == 7)),
                                         reads=[u_, hb], writes=[pu_])
                                sgb = sg[hc % 2]
                                S.op("act", lambda: nc.scalar.activation(out=sgb[:], in_=pg_[:], func=AF.Silu), reads=[pg_], writes=[sgb])
                                S.op("dve", lambda: nc.vector.tensor_tensor(out=hd[:, hc, :], in0=pu_[:], in1=sgb[:], op=ALU.mult), reads=[pu_, sgb], writes=[hd])
                            for tt in range(4):
                                tl = jb * 4 + tt
                                tg = grp * TPG + tl
                                for half in range(2):
                                    py = PS[4 + (tt * 2 + half) % 3]
                                    for hc in range(4):
                                        S.op("pe", lambda hc=hc: nc.tensor.matmul(py[:], lhsT=hd[:, hc, tt * 128:(tt + 1) * 128], rhs=d_[:, hc, half * 512:(half + 1) * 512],
                                                                                  start=(hc == 0), stop=(hc == 3)), reads=[hd, d_], writes=[py])
                                    hs = slice(half * 512, (half + 1) * 512)
                                    if e == 0:
                                        S.op("dve", lambda: nc.vector.tensor_scalar(out=acc[:, tl, hs], in0=py[:], scalar1=comb[:, tg, e:e + 1], scalar2=None, op0=ALU.mult),
                                             reads=[py, comb], writes=[acc])
                                    else:
                                        S.op("dve", lambda: nc.vector.scalar_tensor_tensor(out=acc[:, tl, hs], in0=py[:], scalar=comb[:, tg, e:e + 1], in1=acc[:, tl, hs],
                                                                                            op0=ALU.mult, op1=ALU.add), reads=[py, comb, acc], writes=[acc])
                    for tl in range(TPG):
                        tg = grp * TPG + tl
                        r0 = tg * 128
                        xb = x1f[tl % 2]
                        sc_ = sf[tl % 2]
                        S.dma("sp", lambda: nc.sync.dma_start(out=xb[:], in_=x1_d[r0:r0 + 128, :]), writes=[xb])
                        S.op("dve", lambda: nc.vector.tensor_tensor(out=acc[:, tl, :], in0=acc[:, tl, :], in1=gate2b[:], op=ALU.mult), reads=[acc, gate2b], writes=[acc])
                        S.op("dve", lambda: nc.vector.tensor_tensor(out=xb[:], in0=xb[:], in1=acc[:, tl, :], op=ALU.add), reads=[xb, acc], writes=[xb])
                        S.op("act", lambda: nc.scalar.activation(out=junk4[:], in_=xb[:], func=AF.Square, accum_out=sc_[:, 0:1]), reads=[xb], writes=[junk4, sc_])
                        S.op("dve", lambda: nc.vector.tensor_scalar(out=sc_[:, 0:1], in0=sc_[:, 0:1], scalar1=1.0 / D, scalar2=EPS, op0=ALU.mult, op1=ALU.add), reads=[sc_], writes=[sc_])
                        S.op("act", lambda: nc.scalar.sqrt(out=sc_[:, 0:1], in_=sc_[:, 0:1]), reads=[sc_], writes=[sc_])
                        S.op("dve", lambda: nc.vector.reciprocal(out=sc_[:, 0:1], in_=sc_[:, 0:1]), reads=[sc_], writes=[sc_])
                        S.op("dve", lambda: nc.vector.scalar_tensor_tensor(out=xb[:], in0=xb[:], scalar=sc_[:, 0:1], in1=fingb[:], op0=ALU.mult, op1=ALU.mult),
                             reads=[xb, sc_, fingb], writes=[xb])
                        S.dma("pool", lambda: nc.gpsimd.dma_start(out=out_d[r0:r0 + 128, :], in_=xb[:]), reads=[xb])
                    if grp == 1 and NG > 2:
                        S.new_epoch()
                S.barrier()
            return False

        if 4 in run:
            if stage4():
                return nc, S
        if stages <= 1:
            return nc, S

    return nc, S


def make_inputs(core, inp):
    b = core
    m = {}
    m["x"] = np.ascontiguousarray(inp["x"][b])
    m["ctx"] = np.ascontiguousarray(inp["ctx"][b])
    cT = np.zeros((128, 16), np.float32)
    cT[:, 0:8] = inp["c"][b].reshape(8, 128).T
    cT[:, 8:16] = inp["c_ctx"].reshape(8, 128).T
    m["cT"] = cT
    m["w_mod"] = np.ascontiguousarray(inp["w_mod"][0])
    m["b_mod"] = np.ascontiguousarray(inp["b_mod"][0].reshape(1, -1))
    m["n1gT"] = np.ascontiguousarray(inp["norm1_g"][0].reshape(8, 128).T)
    m["w_in"] = np.ascontiguousarray(inp["w_in"][0])
    cw = inp["conv_w"][0].reshape(9, 12, 128)
    m["convw"] = np.ascontiguousarray(cw.transpose(2, 1, 0))
    m["a_log"] = np.ascontiguousarray(inp["a_log"][0].reshape(1, 8))
    m["dt_bias"] = np.ascontiguousarray(inp["dt_bias"][0].reshape(1, 8))
    m["n2g"] = np.ascontiguousarray(inp["norm2_g"][0].reshape(1, -1))
    m["fing"] = np.ascontiguousarray(inp["final_g"].reshape(1, -1))
    m["onorm"] = np.ascontiguousarray(inp["onorm_g"][0].reshape(1, -1))
    m["b_group"] = np.ascontiguousarray(inp["b_group"][0].reshape(1, -1))
    m["b_router"] = np.ascontiguousarray(inp["b_router"][0].reshape(1, -1))
    m["w_group"] = np.ascontiguousarray(inp["w_group"][0])
    m["w_router"] = np.ascontiguousarray(inp["w_router"][0])
    m["w_out"] = np.ascontiguousarray(inp["w_out"][0])
    m["w_gate"] = np.ascontiguousarray(inp["w_gate"][0])
    m["w_up"] = np.ascontiguousarray(inp["w_up"][0])
    m["w_down"] = np.ascontiguousarray(inp["w_down"][0])
    m.update(host_consts())
    return m


def kernel(**inp):
    inp = {k: np.asarray(v) for k, v in inp.items()}
    nc, S = build()
    in_maps = [make_inputs(c, inp) for c in range(8)]
    res = run_bass_kernel_spmd(nc, in_maps, core_ids=list(range(8)))
    return np.stack([r["out"] for r in res.results], axis=0).astype(np.float32)
```

```python
import contextlib
import numpy as np
import ml_dtypes
import concourse.bass as bass
import concourse.mybir as mybir
from concourse.bass_utils import run_bass_kernel_spmd

F32 = mybir.dt.float32
BF16 = mybir.dt.bfloat16
I32 = mybir.dt.int32
AF = mybir.ActivationFunctionType
ALU = mybir.AluOpType
AX = mybir.AxisListType

T = 8192
D = 1024
TC = 256
NB = 16
EPS = 1e-6


class TB:
    def __init__(self, t, name=""):
        self.t = t
        self.name = name
        self.w = None
        self.r = []
        self.psum = False

    def __getitem__(self, k):
        return self.t[k]


class Sync:
    ENG = ("pe", "dve", "act", "pool", "sp")

    def __init__(self, nc, n_dma_sems=64):
        self.nc = nc
        self.e = {"pe": nc.tensor, "dve": nc.vector, "act": nc.scalar, "pool": nc.gpsimd, "sp": nc.sync}
        self.sem = {k: nc.alloc_semaphore(name="c_" + k) for k in self.ENG}
        self.cnt = {k: 0 for k in self.ENG}
        self.seen = {k: {} for k in self.ENG}
        self.dsems = [nc.alloc_semaphore(name="d_%d" % i) for i in range(n_dma_sems)]
        self.dcnt = [0] * n_dma_sems
        self.dnext = 0
        self.n_hw = n_dma_sems - 8
        self.pnext = 0
        self.n_ins = 0
        self.epoch = 0
        self.limit = None
        self.log = []

    def _wait(self, eng, tok):
        if tok is None:
            return
        kind, key, val = tok[0], tok[1], tok[2]
        if kind == "c":
            if tok[3] < self.epoch:
                return
            if key == "pe" and eng == "pe":
                return
            sem = self.sem[key]
            sk = "c" + key
        else:
            sem = self.dsems[key]
            sk = "d%d" % key
        if self.seen[eng].get(sk, 0) >= val:
            return
        self.e[eng].wait_ge(sem, val)
        self.seen[eng][sk] = val

    def _deps(self, eng, reads, writes):
        for b in reads:
            self._wait(eng, b.w)
            if b.psum:
                for t in b.r:
                    self._wait(eng, t)
        for b in writes:
            self._wait(eng, b.w)
            for t in b.r:
                self._wait(eng, t)

    def _commit(self, tok, reads, writes):
        for b in reads:
            b.r.append(tok)
            if len(b.r) > 48:
                b.r = b.r[-48:]
        for b in writes:
            b.w = tok
            b.r = []

    def op(self, eng, fn, reads=(), writes=()):
        if self.limit is not None and self.n_ins >= self.limit:
            return None
        self._deps(eng, reads, writes)
        self.log.append((self.n_ins, eng, fn.__code__.co_firstlineno))
        ins = fn()
        ins.then_inc(self.sem[eng], 1)
        self.cnt[eng] += 1
        self.n_ins += 1
        tok = ("c", eng, self.cnt[eng], self.epoch)
        self._commit(tok, reads, writes)
        return tok

    def dma(self, eng, fn, reads=(), writes=()):
        if self.limit is not None and self.n_ins >= self.limit:
            return None
        if eng == "pool":
            i = self.n_hw + self.pnext
            self.pnext = (self.pnext + 1) % 8
        else:
            i = self.dnext
            self.dnext = (self.dnext + 1) % self.n_hw
        if self.dcnt[i] > 0:
            self._wait(eng, ("d", i, self.dcnt[i]))
        self._deps(eng, reads, writes)
        self.log.append((self.n_ins, eng + "-dma", fn.__code__.co_firstlineno))
        ins = fn()
        self.dcnt[i] += 16
        ins.then_inc(self.dsems[i], 16)
        self.n_ins += 1
        tok = ("d", i, self.dcnt[i])
        self._commit(tok, reads, writes)
        return tok

    def barrier(self):
        for eng in self.ENG:
            for k in self.ENG:
                if self.cnt[k] > 0 and k != eng:
                    self._wait(eng, ("c", k, self.cnt[k], self.epoch))
            for i in range(len(self.dsems)):
                if self.dcnt[i] > 0:
                    self._wait(eng, ("d", i, self.dcnt[i]))

    def new_epoch(self):
        self.barrier()
        self.epoch += 1
        self.sem = {k: self.nc.alloc_semaphore(name="c%d_%s" % (self.epoch, k)) for k in self.ENG}
        self.cnt = {k: 0 for k in self.ENG}
        for eng in self.ENG:
            self.seen[eng] = {k: v for k, v in self.seen[eng].items() if not k.startswith("c")}


def host_consts():
    c = {}
    c["ident_bf"] = np.eye(128, dtype=np.float32).astype(ml_dtypes.bfloat16)
    c["ident_f"] = np.eye(128, dtype=np.float32)
    n = np.arange(128)
    ang = 2 * np.pi * np.outer(n, n) / 128.0
    c["cs128"] = np.concatenate([np.cos(ang), np.sin(ang)], axis=1).astype(np.float32).astype(ml_dtypes.bfloat16)
    c["ones_bf"] = np.ones((128, 128), np.float32).astype(ml_dtypes.bfloat16)
    c["ones_f"] = np.ones((128, 128), np.float32)
    c["zf"] = np.zeros((128, 512), np.float32)
    c["zb"] = np.zeros((128, 7680), np.float32).astype(ml_dtypes.bfloat16)
    ii = np.arange(128)[:, None]
    jj = np.arange(128)[None, :]
    c["mS0"] = np.tile((ii > jj).astype(np.float32), (1, 4)); c["mI0"] = np.tile((ii >= jj).astype(np.float32), (1, 4))
    c["mS1"] = np.tile((ii < jj).astype(np.float32), (1, 4)); c["mI1"] = np.tile((ii <= jj).astype(np.float32), (1, 4))
    c["Lm0"] = (ii <= jj).astype(np.float32)
    c["Lm1"] = (ii >= jj).astype(np.float32)
    c["Um0"] = (ii > jj).astype(np.float32)
    c["Um1"] = (ii < jj).astype(np.float32)
    bb = np.arange(128)[:, None, None]; aa = np.arange(64)[None, :, None]; bp = np.arange(128)[None, None, :]
    th = 2 * np.pi * ((bp * (64 * bb + aa)) % 8192) / 8192.0
    c["cosA"] = np.cos(th).astype(np.float32).astype(ml_dtypes.bfloat16)
    c["sinA"] = np.sin(th).astype(np.float32).astype(ml_dtypes.bfloat16)
    c["nsinA"] = (-np.sin(th)).astype(np.float32).astype(ml_dtypes.bfloat16)
    a1 = np.arange(64)[:, None]; a2 = np.arange(64)[None, :]
    ps_ = 2 * np.pi * ((a1 * a2) % 64) / 64.0
    c["cs64"] = np.concatenate([np.cos(ps_), -np.sin(ps_)], axis=0).astype(np.float32).astype(ml_dtypes.bfloat16)
    c["I4f"] = np.tile(np.eye(128, dtype=np.float32), (1, 4))
    c["bdm"] = np.tile(((ii // 32) == (jj // 32)).astype(np.float32), (1, 4))
    for i_ in range(4):
        c["cm%d" % i_] = np.tile(((jj // 32) == i_).astype(np.float32) * np.ones((128, 1), np.float32), (1, 4))
    c["I4"] = np.tile(np.eye(128, dtype=np.float32), (1, 4)).astype(ml_dtypes.bfloat16)
    return c


def build(stages=4, dbg=False, nblk=NB, run=(1, 2, 3, 4), feed=(), nch=64, NT4=64, NEXP=32, ulimit=None, limit=None):
    nc = bass.Bass("TRN2", target_bir_lowering=False)
    S = Sync(nc)
    S.limit = limit
    E = S.e

    def din(name, shape, dt=F32):
        return nc.dram_tensor(name, list(shape), dt, kind="ExternalInput").ap()

    def dscr(name, shape, dt, out=False):
        if name in feed:
            return nc.dram_tensor(name, list(shape), dt, kind="ExternalInput").ap()
        return nc.dram_tensor(name, list(shape), dt, kind=("ExternalOutput" if (out or (dbg and (dbg is True or name in dbg))) else "Internal")).ap()

    x_d = din("x", [T, D])
    ctx_d = din("ctx", [TC, D])
    cT_d = din("cT", [128, 16])
    w_mod_d = din("w_mod", [D, 6 * D])
    b_mod_d = din("b_mod", [1, 6 * D])
    n1gT_d = din("n1gT", [128, 8])
    w_in_d = din("w_in", [D, 2576])
    convw_d = din("convw", [128, 12, 9])
    alog_d = din("a_log", [1, 8])
    dtb_d = din("dt_bias", [1, 8])
    ident_bf_d = din("ident_bf", [128, 128], BF16)
    ident_f_d = din("ident_f", [128, 128])
    cs128_d = din("cs128", [128, 256], BF16)
    ones_bf_d = din("ones_bf", [128, 128], BF16)
    ones_f_d = din("ones_f", [128, 128])
    zf_d = din("zf", [128, 512])
    zb_d = din("zb", [128, 7680], BF16)

    qT_d = dscr("qT_s", [4, 128, T], BF16)
    kT_d = dscr("kT_s", [4, 128, T], BF16)
    k_d = dscr("k_s", [T, 512], BF16)
    v_d = dscr("v_s", [T, 512], BF16)
    z_d = dscr("z_s", [T, 512], BF16)
    pq_d = dscr("pq_s", [T, 1024], BF16)
    gb_d = dscr("gb_s", [T, 16], F32)
    cqT_d = dscr("cqT_s", [4, 128, TC], BF16)
    ckT_d = dscr("ckT_s", [4, 128, TC], BF16)
    ck_d = dscr("ck_s", [TC, 512], BF16)
    cv_d = dscr("cv_s", [TC, 512], BF16)
    cgb_d = dscr("cgb_s", [TC, 16], F32)
    modrow_d = dscr("modrow_s", [2, 6 * D], F32)

    of_d = dscr("of_s", [T, 512], F32)
    ob_d = dscr("ob_s", [T, 512], F32)
    s0_d = [dscr("s0_%d" % d, [128, 4, 128], F32) for d in range(2)]
    mS_d = [din("mS%d" % d, [128, 512]) for d in range(2)]
    mI_d = [din("mI%d" % d, [128, 512]) for d in range(2)]
    Lm_d = [din("Lm%d" % d, [128, 128]) for d in range(2)]
    Um_d = [din("Um%d" % d, [128, 128]) for d in range(2)]
    I4_d = din("I4", [128, 512], BF16)
    I4f_d = din("I4f", [128, 512])
    bdm_d = din("bdm", [128, 512])
    cm_d = [din("cm%d" % i, [128, 512]) for i in range(4)]
    Zd = dscr("Z_s", [2, 64, 128, 512], BF16)
    fmT_d = dscr("fmT_s", [4, 128, T], BF16)
    cosA_d = din("cosA", [128, 64, 128], BF16)
    sinA_d = din("sinA", [128, 64, 128], BF16)
    nsinA_d = din("nsinA", [128, 64, 128], BF16)
    cs64_d = din("cs64", [128, 64], BF16)
    x1_d = dscr("x1_s", [T, D], F32)
    h2T_d = dscr("h2T_s", [8, 128, T], BF16)
    comb_d = dscr("comb_s", [T, 32], F32)
    n2g_d = din("n2g", [1, D])
    fing_d = din("fing", [1, D])
    onorm_d = din("onorm", [1, 128])
    bgrp_d = din("b_group", [1, 4])
    brt_d = din("b_router", [1, 32])
    wgrp_d = din("w_group", [D, 4])
    wrt_d = din("w_router", [D, 32])
    w_out_d = din("w_out", [D, D])
    wgate_d = din("w_gate", [32, D, 512])
    wup_d = din("w_up", [32, D, 512])
    wdown_d = din("w_down", [32, 512, D])
    out_d = nc.dram_tensor("out", [T, D], F32, kind="ExternalOutput").ap()

    glob = contextlib.ExitStack()

    def mk(stack, space, name, shape, dt):
        if space == "sb":
            t = stack.enter_context(nc.sbuf_tensor("t_" + name, list(shape), dt))
        else:
            t = stack.enter_context(nc.psum_tensor("t_" + name, list(shape), dt))
            tb = TB(t, name)
            tb.psum = True
            return tb
        return TB(t, name)

    with glob:
        ident_bf = mk(glob, "sb", "ident_bf", [128, 128], BF16)
        ident_f = mk(glob, "sb", "ident_f", [128, 128], F32)
        ones_bf = mk(glob, "sb", "ones_bf", [128, 128], BF16)
        ones_f = mk(glob, "sb", "ones_f", [128, 128], F32)
        modT = mk(glob, "sb", "modT", [128, 48, 2], F32)
        for tb, d in ((ident_bf, ident_bf_d), (ident_f, ident_f_d), (ones_bf, ones_bf_d), (ones_f, ones_f_d)):
            S.dma("sp", lambda tb=tb, d=d: nc.sync.dma_start(out=tb[:], in_=d), writes=[tb])

        def stage1():
            st = contextlib.ExitStack()
            with st:
                sb = lambda name, shape, dt: mk(st, "sb", name, shape, dt)
                ps = lambda name, shape, dt: mk(st, "ps", name, shape, dt)
                n1g = sb("n1g", [128, 8], F32)
                gs1 = sb("gs1", [128, 8, 2], F32)
                w_in = sb("w_in", [128, 8, 2064], BF16)
                cs128 = sb("cs128", [128, 256], BF16)
                wpq = sb("wpq", [128, 8, 1024], BF16)
                convw = sb("convw", [128, 12, 9], F32)
                alog = sb("alog", [128, 8], F32)
                dtb = sb("dtb", [128, 8], F32)
                nea = sb("nea", [128, 8], F32)
                sa = contextlib.ExitStack()
                sa.__enter__()
                sbA = lambda name, shape, dt: mk(sa, "sb", name, shape, dt)
                cT = sbA("cT", [128, 16], F32)
                sc = sbA("sc", [128, 16], F32)
                S.dma("sp", lambda: nc.sync.dma_start(out=cT[:], in_=cT_d), writes=[cT])
                S.op("act", lambda: nc.scalar.activation(out=sc[:], in_=cT[:], func=AF.Silu), reads=[cT], writes=[sc])
                scl = sbA("scl", [128, 8, 2], F32)
                S.op("dve", lambda: nc.vector.tensor_copy(out=scl[:, :, 0], in_=sc[:, 0:8]), reads=[sc], writes=[scl])
                S.op("dve", lambda: nc.vector.tensor_copy(out=scl[:, :, 1], in_=sc[:, 8:16]), reads=[sc], writes=[scl])
                wm = [sbA("wm%d" % i, [128, 8, 512], F32) for i in range(2)]
                bmod = sbA("bmod", [2, 6 * D], F32)
                for r in range(2):
                    S.dma("sp", lambda r=r: nc.sync.dma_start(out=bmod[r:r + 1, :], in_=b_mod_d), writes=[bmod])
                modrow = sbA("modrow", [2, 6 * D], F32)
                pm = [ps("pm%d" % i, [128, 512], F32) for i in range(2)]
                for g in range(12):
                    w = wm[g % 2]
                    S.dma("sp", lambda w=w, g=g: nc.sync.dma_start(
                        out=w[:], in_=w_mod_d[:, g * 512:(g + 1) * 512].rearrange("(k p) n -> p k n", p=128)), writes=[w])
                    p = pm[g % 2]
                    for k in range(8):
                        S.op("pe", lambda p=p, w=w, k=k: nc.tensor.matmul(p[0:2, :], lhsT=scl[:, k, :], rhs=w[:, k, :],
                                                                           start=(k == 0), stop=(k == 7)),
                             reads=[scl, w], writes=[p])
                    S.op("dve", lambda p=p, g=g: nc.vector.tensor_tensor(out=modrow[:, g * 512:(g + 1) * 512], in0=p[0:2, :],
                                                                          in1=bmod[:, g * 512:(g + 1) * 512], op=ALU.add),
                         reads=[p, bmod], writes=[modrow])
                pGb = ps("pGb", [128, 512], F32)
                pmt = TB(pGb.t, "pmt")
                pmtv = pGb[:, 0:96].rearrange("p (a b) -> p a b", b=2)
                for blk in range(48):
                    S.op("pe", lambda blk=blk: nc.tensor.transpose(pmtv[:, blk, :], modrow[0:2, blk * 128:(blk + 1) * 128],
                                                                   ident_f[0:2, 0:2]), reads=[modrow, ident_f], writes=[pGb])
                S.op("dve", lambda: nc.vector.tensor_copy(out=modT[:], in_=pmtv), reads=[pGb], writes=[modT])
                S.dma("pool", lambda: nc.gpsimd.dma_start(out=modrow_d, in_=modrow[:]), reads=[modrow])
                S.dma("sp", lambda: nc.sync.dma_start(out=n1g[:], in_=n1gT_d), writes=[n1g])
                for j in range(2):
                    S.op("dve", lambda j=j: nc.vector.scalar_tensor_tensor(out=gs1[:, :, j], in0=modT[:, 8:16, j], scalar=1.0,
                                                                            in1=n1g[:], op0=ALU.add, op1=ALU.mult),
                         reads=[modT, n1g], writes=[gs1])

                S.dma("pool", lambda: nc.gpsimd.dma_start(out=w_in[:], in_=w_in_d[:, 512:2576].rearrange("(k p) n -> p k n", p=128)),
                      writes=[w_in])
                wf = sbA("wf", [128, 8, 512], BF16)
                S.dma("pool", lambda: nc.gpsimd.dma_start(out=wf[:], in_=w_in_d[:, 0:512].rearrange("(k p) n -> p k n", p=128)),
                      writes=[wf])
                S.dma("sp", lambda: nc.sync.dma_start(out=cs128[:], in_=cs128_d), writes=[cs128])
                wfT = sbA("wfT", [128, 4, 1024], BF16)
                ptr = [ps("ptr%d" % i, [128, 8, 128], BF16) for i in range(1)]
                for g in range(4):
                    for k in range(8):
                        S.op("pe", lambda g=g, k=k: nc.tensor.transpose(ptr[0][:, k, :], wf[:, k, g * 128:(g + 1) * 128], ident_bf[:]),
                             reads=[wf, ident_bf], writes=[ptr[0]])
                    S.op("dve", lambda g=g: nc.vector.tensor_copy(out=wfT[:, g, :], in_=ptr[0][:].rearrange("p a b -> p (a b)")),
                         reads=[ptr[0]], writes=[wfT])
                for k in range(8):
                    p = pm[k % 2]
                    for g in range(4):
                        S.op("pe", lambda p=p, g=g, k=k: nc.tensor.matmul(p[:, (g % 2) * 256:(g % 2) * 256 + 256],
                                                                           lhsT=wfT[:, g, k * 128:(k + 1) * 128], rhs=cs128[:], start=True, stop=True),
                             reads=[wfT, cs128], writes=[p])
                        S.op("act", lambda p=p, g=g, k=k: nc.scalar.copy(out=wpq[:, k, g * 128:(g + 1) * 128],
                                                                          in_=p[:, (g % 2) * 256:(g % 2) * 256 + 128]),
                             reads=[p], writes=[wpq])
                        S.op("act", lambda p=p, g=g, k=k: nc.scalar.copy(out=wpq[:, k, 512 + g * 128:512 + (g + 1) * 128],
                                                                          in_=p[:, (g % 2) * 256 + 128:(g % 2) * 256 + 256]),
                             reads=[p], writes=[wpq])

                S.dma("sp", lambda: nc.sync.dma_start(out=convw[:], in_=convw_d), writes=[convw])
                S.dma("sp", lambda: nc.sync.dma_start(out=alog[:], in_=alog_d.partition_broadcast(128)), writes=[alog])
                S.dma("sp", lambda: nc.sync.dma_start(out=dtb[:], in_=dtb_d.partition_broadcast(128)), writes=[dtb])
                S.op("act", lambda: nc.scalar.activation(out=nea[:], in_=alog[:], func=AF.Exp), reads=[alog], writes=[nea])
                S.op("dve", lambda: nc.vector.tensor_scalar(out=nea[:], in0=nea[:], scalar1=-1.0, scalar2=None, op0=ALU.mult),
                     reads=[nea], writes=[nea])

                S.barrier()
                sa.__exit__(None, None, None)
                if stages <= 0.3:
                    return True
                xt = [sb("xt%d" % i, [128, D], F32) for i in range(2)]
                xn = [sb("xn%d" % i, [128, D], BF16) for i in range(2)]
                junk = sb("junk", [128, D], BF16)
                ssq = [sb("ssq%d" % i, [128, 1], F32) for i in range(4)]
                hT = [sb("hT%d" % i, [128, 8, 512], BF16) for i in range(2)]
                pre = [sb("pre%d" % i, [128, 12, 640], BF16) for i in range(3)]
                cacc = [sb("cacc%d" % i, [128, 512], F32) for i in range(4)]
                cvT = sb("cvT", [128, 12, 512], BF16)
                sq = [sb("sq%d" % i, [128, 512], BF16) for i in range(2)]
                rs = [sb("rs%d" % i, [128, 512], F32) for i in range(2)]
                nT = [sb("nT%d" % i, [128, 8, 512], BF16) for i in range(1)]
                ktok = [sb("ktok%d" % i, [128, 512], BF16) for i in range(2)]
                vtok = [sb("vtok%d" % i, [128, 512], BF16) for i in range(2)]
                pqs = [sb("pqs%d" % i, [128, 1024], BF16) for i in range(2)]
                zs = [sb("zs%d" % i, [128, 512], BF16) for i in range(2)]
                gts = [sb("gts%d" % i, [128, 16], F32) for i in range(2)]
                gbs = [sb("gbs%d" % i, [128, 16], F32) for i in range(2)]
                pT = ps("pT", [128, 8, 128], BF16)
                pA = pm
                pB = [ps("pB%d" % i, [128, 512], F32) for i in range(2)]
                pZ = ps("pZ", [128, 512], F32)
                pG = pGb
                pX = ptr[0]

                cnt = {"x": 0, "n": 0, "s": 0, "c": 0}

                def norm_transpose(src_d, row0, jmod, hTb, col0):
                    xb = xt[cnt["x"] % 2]
                    xnb = xn[cnt["n"] % 2]
                    sq1 = ssq[cnt["s"] % 4]
                    cnt["x"] += 1; cnt["n"] += 1; cnt["s"] += 1
                    S.dma("sp", lambda: nc.sync.dma_start(out=xb[:], in_=src_d[row0:row0 + 128, :]), writes=[xb])
                    S.op("act", lambda: nc.scalar.activation(out=junk[:], in_=xb[:], func=AF.Square, accum_out=sq1[:]),
                         reads=[xb], writes=[junk, sq1])
                    S.op("dve", lambda: nc.vector.tensor_scalar(out=sq1[:], in0=sq1[:], scalar1=1.0 / D, scalar2=EPS, op0=ALU.mult, op1=ALU.add),
                         reads=[sq1], writes=[sq1])
                    S.op("act", lambda: nc.scalar.sqrt(out=sq1[:], in_=sq1[:]), reads=[sq1], writes=[sq1])
                    S.op("dve", lambda: nc.vector.reciprocal(out=sq1[:], in_=sq1[:]), reads=[sq1], writes=[sq1])
                    S.op("act", lambda: nc.scalar.activation(out=xnb[:], in_=xb[:], func=AF.Copy, scale=sq1[:]),
                         reads=[xb, sq1], writes=[xnb])
                    for k in range(8):
                        S.op("pe", lambda k=k: nc.tensor.transpose(pT[:, k, :], xnb[:, k * 128:(k + 1) * 128], ident_bf[:]),
                             reads=[xnb, ident_bf], writes=[pT])
                    for k in range(8):
                        eng = "dve" if k >= 4 else "pool"
                        if eng == "pool":
                            S.op("act", lambda k=k: nc.scalar.activation(out=hTb[:, k, col0:col0 + 128], in_=pT[:, k, :], func=AF.Identity,
                                                                          scale=gs1[:, k, jmod:jmod + 1], bias=modT[:, k, jmod:jmod + 1]),
                                 reads=[pT, gs1, modT], writes=[hTb])
                        else:
                            S.op("dve", lambda k=k: nc.vector.tensor_scalar(out=hTb[:, k, col0:col0 + 128], in0=pT[:, k, :],
                                                                             scalar1=gs1[:, k, jmod:jmod + 1], scalar2=modT[:, k, jmod:jmod + 1],
                                                                             op0=ALU.mult, op1=ALU.add),
                                 reads=[pT, gs1, modT], writes=[hTb])

                def gates_math(gt, gbo):
                    S.op("act", lambda: nc.scalar.activation(out=gbo[:, 0:8], in_=gt[:, 0:8], func=AF.Sigmoid), reads=[gt], writes=[gbo])
                    S.op("dve", lambda: nc.vector.tensor_tensor(out=gt[:, 8:16], in0=gt[:, 8:16], in1=dtb[:], op=ALU.add),
                         reads=[gt, dtb], writes=[gt])
                    S.op("act", lambda: nc.scalar.activation(out=gt[:, 8:16], in_=gt[:, 8:16], func=AF.Exp), reads=[gt], writes=[gt])
                    S.op("act", lambda: nc.scalar.activation(out=gt[:, 8:16], in_=gt[:, 8:16], func=AF.Ln, bias=1.0, scale=1.0),
                         reads=[gt], writes=[gt])
                    S.op("dve", lambda: nc.vector.tensor_tensor(out=gbo[:, 8:16], in0=gt[:, 8:16], in1=nea[:], op=ALU.mult),
                         reads=[gt, nea], writes=[gbo])

                def features(cv, ntok, qTd, kTd, kd, vd, tok0):
                    nTb = nT[0]
                    cnt["c"] += 1
                    for i in range(8):
                        sqb = sq[i % 2]
                        rsb = rs[i % 2]
                        pa = pA[i % 2]
                        S.op("act", lambda: nc.scalar.activation(out=sqb[:, 0:ntok], in_=cv[:, i, 0:ntok], func=AF.Square),
                             reads=[cv], writes=[sqb])
                        S.op("pe", lambda: nc.tensor.matmul(pa[:, 0:ntok], lhsT=ones_bf[:], rhs=sqb[:, 0:ntok], start=True, stop=True),
                             reads=[ones_bf, sqb], writes=[pa])
                        S.op("dve", lambda: nc.vector.tensor_scalar(out=rsb[:, 0:ntok], in0=pa[:, 0:ntok], scalar1=EPS, scalar2=None,
                                                                     op0=ALU.add), reads=[pa], writes=[rsb])
                        S.op("act", lambda: nc.scalar.sqrt(out=rsb[:, 0:ntok], in_=rsb[:, 0:ntok]), reads=[rsb], writes=[rsb])
                        S.op("dve", lambda: nc.vector.reciprocal(out=rsb[:, 0:ntok], in_=rsb[:, 0:ntok]), reads=[rsb], writes=[rsb])
                        S.op("dve", lambda: nc.vector.tensor_tensor(out=nTb[:, i, 0:ntok], in0=cv[:, i, 0:ntok], in1=rsb[:, 0:ntok], op=ALU.mult),
                             reads=[cv, rsb], writes=[nTb])
                    for h in range(4):
                        S.dma("pool", lambda h=h: nc.gpsimd.dma_start(out=qTd[h, :, tok0:tok0 + ntok], in_=nTb[:, h, 0:ntok]), reads=[nTb])
                        S.dma("pool", lambda h=h: nc.gpsimd.dma_start(out=kTd[h, :, tok0:tok0 + ntok], in_=nTb[:, 4 + h, 0:ntok]), reads=[nTb])
                    if stages <= 0.57:
                        return
                    for tt in range(ntok // 128):
                        kb = ktok[tt % 2]
                        vb = vtok[tt % 2]
                        for h in range(4):
                            S.op("pe", lambda h=h: nc.tensor.transpose(pX[:, h, :], nTb[:, 4 + h, tt * 128:(tt + 1) * 128], ident_bf[:]),
                                 reads=[nTb, ident_bf], writes=[pX])
                            S.op("pe", lambda h=h: nc.tensor.transpose(pX[:, 4 + h, :], cv[:, 8 + h, tt * 128:(tt + 1) * 128], ident_bf[:]),
                                 reads=[cv, ident_bf], writes=[pX])
                        S.op("dve", lambda: nc.vector.tensor_copy(out=kb[:], in_=pX[:, 0:4, :].rearrange("p a b -> p (a b)")),
                             reads=[pX], writes=[kb])
                        S.op("dve", lambda: nc.vector.tensor_copy(out=vb[:], in_=pX[:, 4:8, :].rearrange("p a b -> p (a b)")),
                             reads=[pX], writes=[vb])
                        r0 = tok0 + tt * 128
                        S.dma("pool", lambda: nc.gpsimd.dma_start(out=kd[r0:r0 + 128, :], in_=kb[:]), reads=[kb])
                        S.dma("pool", lambda: nc.gpsimd.dma_start(out=vd[r0:r0 + 128, :], in_=vb[:]), reads=[vb])

                hTc = hT[0]
                for tt in range(2):
                    norm_transpose(ctx_d, tt * 128, 1, hTc, tt * 128)
                if stages <= 0.4:
                    S.barrier()
                    return True
                prc = pre[0]
                for ct in range(12):
                    pa = pA[ct % 2]
                    for k in range(8):
                        S.op("pe", lambda k=k: nc.tensor.matmul(pa[:, 0:256], lhsT=w_in[:, k, ct * 128:(ct + 1) * 128], rhs=hTc[:, k, 0:256],
                                                                start=(k == 0), stop=(k == 7)), reads=[w_in, hTc], writes=[pa])
                    S.op("act", lambda: nc.scalar.copy(out=prc[:, ct, 0:256], in_=pa[:, 0:256]), reads=[pa], writes=[prc])
                for tt in range(2):
                    gt = gts[tt % 2]
                    gbo = gbs[tt % 2]
                    for k in range(8):
                        S.op("pe", lambda k=k: nc.tensor.matmul(pG[:, 0:16], lhsT=hTc[:, k, tt * 128:(tt + 1) * 128], rhs=w_in[:, k, 2048:2064],
                                                                start=(k == 0), stop=(k == 7)), reads=[hTc, w_in], writes=[pG])
                    S.op("dve", lambda: nc.vector.tensor_copy(out=gt[:], in_=pG[:, 0:16]), reads=[pG], writes=[gt])
                    gates_math(gt, gbo)
                    S.dma("pool", lambda: nc.gpsimd.dma_start(out=cgb_d[tt * 128:(tt + 1) * 128, :], in_=gbo[:]), reads=[gbo])
                if stages <= 0.5:
                    S.barrier()
                    return True
                for ct in range(12):
                    ca = cacc[ct % 4]
                    eng = "dve"
                    V = E[eng]
                    S.op(eng, lambda: V.tensor_scalar(out=ca[:, 0:256], in0=prc[:, ct, 0:256], scalar1=convw[:, ct, 4:5], scalar2=None, op0=ALU.mult),
                         reads=[prc, convw], writes=[ca])
                    S.op(eng, lambda: V.scalar_tensor_tensor(out=ca[:, 1:256], in0=prc[:, ct, 0:255], scalar=convw[:, ct, 3:4], in1=ca[:, 1:256],
                                                             op0=ALU.mult, op1=ALU.add), reads=[prc, convw, ca], writes=[ca])
                    S.op(eng, lambda: V.scalar_tensor_tensor(out=ca[:, 0:255], in0=prc[:, ct, 1:256], scalar=convw[:, ct, 5:6], in1=ca[:, 0:255],
                                                             op0=ALU.mult, op1=ALU.add), reads=[prc, convw, ca], writes=[ca])
                    S.op("act", lambda: nc.scalar.activation(out=cvT[:, ct, 0:256], in_=ca[:, 0:256], func=AF.Silu), reads=[ca], writes=[cvT])
                if stages <= 0.55:
                    S.barrier()
                    return True
                features(cvT, 256, cqT_d, ckT_d, ck_d, cv_d, 0)
                if stages <= 0.6:
                    S.barrier()
                    return True

                for pb in pre:
                    S.dma("sp", lambda pb=pb: nc.sync.dma_start(out=pb[:].rearrange("p a b -> p (a b)"), in_=zb_d), writes=[pb])

                def inproj(j):
                    hTb = hT[j % 2]
                    for tt in range(4):
                        norm_transpose(x_d, j * 512 + tt * 128, 0, hTb, tt * 128)
                    pj = pre[j % 3]
                    for ct in range(12):
                        pa = pA[ct % 2]
                        for k in range(8):
                            S.op("pe", lambda k=k: nc.tensor.matmul(pa[:], lhsT=w_in[:, k, ct * 128:(ct + 1) * 128], rhs=hTb[:, k, :],
                                                                    start=(k == 0), stop=(k == 7)), reads=[w_in, hTb], writes=[pa])
                        S.op("act", lambda: nc.scalar.copy(out=pj[:, ct, 64:576], in_=pa[:]), reads=[pa], writes=[pj])
                        if j > 0:
                            pp = pre[(j - 1) % 3]
                            S.op("dve", lambda: nc.vector.tensor_copy(out=pp[:, ct, 576:640], in_=pa[:, 0:64]), reads=[pa], writes=[pp])
                        if j < NB - 1:
                            pn = pre[(j + 1) % 3]
                            S.op("dve", lambda: nc.vector.tensor_copy(out=pn[:, ct, 0:64], in_=pa[:, 448:512]), reads=[pa], writes=[pn])
                    if j == NB - 1:
                        S.dma("sp", lambda: nc.sync.dma_start(out=pj[:, :, 576:640], in_=zb_d[:, 0:768].rearrange("p (a b) -> p a b", b=64)), writes=[pj])
                    for tt in range(4):
                        r0 = j * 512 + tt * 128
                        pqb = pqs[tt % 2]
                        zb = zs[tt % 2]
                        gt = gts[tt % 2]
                        gbo = gbs[tt % 2]
                        for half in range(2):
                            for k in range(8):
                                S.op("pe", lambda k=k: nc.tensor.matmul(pB[half][:], lhsT=hTb[:, k, tt * 128:(tt + 1) * 128],
                                                                        rhs=wpq[:, k, half * 512:(half + 1) * 512], start=(k == 0), stop=(k == 7)),
                                     reads=[hTb, wpq], writes=[pB[half]])
                        S.op("dve", lambda: nc.vector.tensor_copy(out=pqb[:, 0:512], in_=pB[0][:]), reads=[pB[0]], writes=[pqb])
                        S.op("act", lambda: nc.scalar.copy(out=pqb[:, 512:1024], in_=pB[1][:]), reads=[pB[1]], writes=[pqb])
                        S.dma("pool", lambda: nc.gpsimd.dma_start(out=pq_d[r0:r0 + 128, :], in_=pqb[:]), reads=[pqb])
                        for k in range(8):
                            S.op("pe", lambda k=k: nc.tensor.matmul(pZ[:], lhsT=hTb[:, k, tt * 128:(tt + 1) * 128], rhs=w_in[:, k, 1536:2048],
                                                                    start=(k == 0), stop=(k == 7)), reads=[hTb, w_in], writes=[pZ])
                        S.op("act", lambda: nc.scalar.activation(out=zb[:], in_=pZ[:], func=AF.Silu), reads=[pZ], writes=[zb])
                        S.dma("pool", lambda: nc.gpsimd.dma_start(out=z_d[r0:r0 + 128, :], in_=zb[:]), reads=[zb])
                        for k in range(8):
                            S.op("pe", lambda k=k: nc.tensor.matmul(pG[:, 0:16], lhsT=hTb[:, k, tt * 128:(tt + 1) * 128], rhs=w_in[:, k, 2048:2064],
                                                                    start=(k == 0), stop=(k == 7)), reads=[hTb, w_in], writes=[pG])
                        S.op("dve", lambda: nc.vector.tensor_copy(out=gt[:], in_=pG[:, 0:16]), reads=[pG], writes=[gt])
                        gates_math(gt, gbo)
                        S.dma("pool", lambda: nc.gpsimd.dma_start(out=gb_d[r0:r0 + 128, :], in_=gbo[:]), reads=[gbo])

                def conv(j):
                    pj = pre[j % 3]
                    pv = pj[:].rearrange("p c (r w) -> p c r w", w=64)
                    for ct in range(12):
                        ca = cacc[ct % 4]
                        cav = ca[:].rearrange("p (r w) -> p r w", w=64)
                        eng = "dve"
                        V = E[eng]
                        S.op(eng, lambda: V.tensor_scalar(out=cav, in0=pv[:, ct, 1:9, :], scalar1=convw[:, ct, 4:5], scalar2=None, op0=ALU.mult),
                             reads=[pj, convw], writes=[ca])
                        for dr in (-1, 0, 1):
                            for dc in (-1, 0, 1):
                                if dr == 0 and dc == 0:
                                    continue
                                tap = (dr + 1) * 3 + (dc + 1)
                                c0 = max(0, -dc)
                                c1 = 64 - max(0, dc)
                                S.op(eng, lambda: V.scalar_tensor_tensor(
                                    out=cav[:, :, c0:c1], in0=pv[:, ct, 1 + dr:9 + dr, c0 + dc:c1 + dc], scalar=convw[:, ct, tap:tap + 1],
                                    in1=cav[:, :, c0:c1], op0=ALU.mult, op1=ALU.add), reads=[pj, convw, ca], writes=[ca])
                        S.op("act", lambda: nc.scalar.activation(out=cvT[:, ct, :], in_=ca[:], func=AF.Silu), reads=[ca], writes=[cvT])
                    features(cvT, 512, qT_d, kT_d, k_d, v_d, j * 512)

                for j in range(nblk + 1):
                    if j < nblk:
                        inproj(j)
                    if j >= 1:
                        conv(j - 1)
                S.new_epoch()
            return False

        if 1 in run:
            if stage1():
                return nc, S

        def stage2():
            st2 = contextlib.ExitStack()
            with st2:
                sb = lambda name, shape, dt: mk(st2, "sb", name, shape, dt)
                ps = lambda name, shape, dt: mk(st2, "ps", name, shape, dt)
                QS = 128.0 ** -0.5
                mS = [sb("mS%d" % d, [128, 512], F32) for d in range(2)]
                mI = [sb("mI%d" % d, [128, 512], F32) for d in range(2)]
                Lm = [sb("Lm%d" % d, [128, 128], F32) for d in range(2)]
                Um = [sb("Um%d" % d, [128, 128], F32) for d in range(2)]
                I4 = sb("I4", [128, 512], BF16)
                for d in range(2):
                    for tb, dd in ((mS[d], mS_d[d]), (mI[d], mI_d[d]), (Lm[d], Lm_d[d]), (Um[d], Um_d[d])):
                        S.dma("sp", lambda tb=tb, dd=dd: nc.sync.dma_start(out=tb[:], in_=dd), writes=[tb])
                S.dma("sp", lambda: nc.sync.dma_start(out=I4[:], in_=I4_d), writes=[I4])
                I4f = sb("I4f", [128, 512], F32)
                bdm = sb("bdm", [128, 512], F32)
                cm = [sb("cm%d" % i, [128, 512], F32) for i in range(4)]
                S.dma("sp", lambda: nc.sync.dma_start(out=I4f[:], in_=I4f_d), writes=[I4f])
                S.dma("sp", lambda: nc.sync.dma_start(out=bdm[:], in_=bdm_d), writes=[bdm])
                for i in range(4):
                    S.dma("sp", lambda i=i: nc.sync.dma_start(out=cm[i][:], in_=cm_d[i]), writes=[cm[i]])
                Sst = [sb("Sst%d" % d, [128, 4, 128], F32) for d in range(2)]
                Sbf = [sb("Sbf%d" % d, [128, 4, 128], BF16) for d in range(2)]
                for d in range(2):
                    S.dma("sp", lambda d=d: nc.sync.dma_start(out=Sst[d][:].rearrange("p a b -> p (a b)"), in_=zf_d), writes=[Sst[d]])
                    S.dma("sp", lambda d=d: nc.sync.dma_start(out=Sbf[d][:].rearrange("p a b -> p (a b)"), in_=zb_d[:, 0:512]), writes=[Sbf[d]])

                class UB:
                    pass
                ubs = []
                for u in range(2):
                    b = UB()
                    n_ = lambda nm: "%s_%d" % (nm, u)
                    b.qT = sb(n_("qT"), [128, 4, 128], BF16); b.kT = sb(n_("kT"), [128, 4, 128], BF16)
                    b.kt = sb(n_("kt"), [128, 512], BF16); b.vt = sb(n_("vt"), [128, 512], BF16)
                    b.gb = sb(n_("gb"), [128, 16], F32)
                    b.Lg = sb(n_("Lg"), [128, 4, 128], F32)
                    b.E1 = sb(n_("E1"), [128, 512], F32); b.E1s = sb(n_("E1s"), [128, 512], F32); b.E1i = sb(n_("E1i"), [128, 512], F32)
                    b.sc = sb(n_("sc"), [128, 40], F32)
                    b.XA = sb(n_("XA"), [128, 8, 128], BF16); b.YA = sb(n_("YA"), [128, 8, 128], BF16)
                    b.XF = [sb(n_("XF%d" % i), [128, 4, 128], F32) for i in range(2)]
                    b.YF = [sb(n_("YF%d" % i), [128, 4, 128], F32) for i in range(2)]
                    b.IYF = sb(n_("IYF"), [128, 4, 128], F32)
                    b.TnF = [sb(n_("TnF%d" % i), [128, 4, 128], F32) for i in range(2)]
                    b.TtF = [sb(n_("TtF%d" % i), [128, 4, 128], F32) for i in range(2)]
                    b.E1sB = sb(n_("E1sB"), [128, 512], F32)
                    b.G = [sb(n_("G%d" % i), [128, 4, 128], BF16) for i in range(4)]
                    b.Bt = sb(n_("Bt"), [128, 4, 256], BF16); b.Xs = sb(n_("Xs"), [128, 4, 256], BF16); b.Vt = sb(n_("Vt"), [128, 4, 256], BF16)
                    b.kd = sb(n_("kd"), [128, 4, 128], BF16)
                    b.wTn = sb(n_("wTn"), [128, 4, 128], BF16); b.vn = sb(n_("vn"), [128, 4, 128], BF16)
                    b.AVs = sb(n_("AVs"), [128, 4, 128], F32); b.o = sb(n_("o"), [128, 4, 128], F32)
                    ubs.append(b)
                F = [ps("F%d" % i, [128, 4, 128], F32) for i in range(7)]
                TPb = ps("TPb", [128, 8, 128], BF16)
                cnt2 = {"u": 0}

                def unit(n, d, srcs, want_o, o_dst):
                    qTd, kTd, kd_, vd_, gbd = srcs
                    b = ubs[cnt2["u"] % 2]
                    cnt2["u"] += 1
                    t0 = n * 128
                    Sd, Sb_ = Sst[d], Sbf[d]
                    S.dma("sp", lambda: nc.sync.dma_start(out=b.qT[:], in_=qTd[:, :, t0:t0 + 128].rearrange("h p t -> p h t")), writes=[b.qT])
                    S.dma("sp", lambda: nc.sync.dma_start(out=b.kT[:], in_=kTd[:, :, t0:t0 + 128].rearrange("h p t -> p h t")), writes=[b.kT])
                    S.dma("sp", lambda: nc.sync.dma_start(out=b.kt[:], in_=kd_[t0:t0 + 128, :]), writes=[b.kt])
                    S.dma("sp", lambda: nc.sync.dma_start(out=b.vt[:], in_=vd_[t0:t0 + 128, :]), writes=[b.vt])
                    S.dma("sp", lambda: nc.sync.dma_start(out=b.gb[:], in_=gbd[t0:t0 + 128, :]), writes=[b.gb])
                    g0 = 8 + 4 * d
                    b0 = 4 * d
                    sc = b.sc
                    for h in range(4):
                        S.op("dve", lambda h=h: nc.vector.tensor_scalar(out=b.Lg[:, h, :], in0=Lm[d][:], scalar1=b.gb[:, g0 + h:g0 + h + 1], scalar2=None, op0=ALU.mult),
                             reads=[Lm[d], b.gb], writes=[b.Lg])
                    for h in range(4):
                        S.op("pe", lambda h=h: nc.tensor.matmul(F[0][:, h, :], lhsT=b.Lg[:, h, :], rhs=Um[d][:], start=True, stop=True),
                             reads=[b.Lg, Um[d]], writes=[F[0]])
                    S.op("pe", lambda: nc.tensor.matmul(F[1][:, 0, 0:4], lhsT=Lm[d][:], rhs=b.gb[:, g0:g0 + 4], start=True, stop=True),
                         reads=[Lm[d], b.gb], writes=[F[1]])
                    S.op("pe", lambda: nc.tensor.matmul(F[1][:, 0, 4:8], lhsT=ones_f[:], rhs=b.gb[:, g0:g0 + 4], start=True, stop=True),
                         reads=[ones_f, b.gb], writes=[F[1]])
                    S.op("act", lambda: nc.scalar.activation(out=b.E1[:], in_=F[0][:].rearrange("p a b -> p (a b)"), func=AF.Exp), reads=[F[0]], writes=[b.E1])
                    S.op("dve", lambda: nc.vector.tensor_copy(out=sc[:, 0:8], in_=F[1][:, 0, 0:8]), reads=[F[1]], writes=[sc])
                    S.op("act", lambda: nc.scalar.activation(out=sc[:, 8:12], in_=sc[:, 0:4], func=AF.Exp), reads=[sc], writes=[sc])
                    S.op("act", lambda: nc.scalar.activation(out=sc[:, 28:32], in_=sc[:, 4:8], func=AF.Exp), reads=[sc], writes=[sc])
                    S.op("dve", lambda: nc.vector.tensor_scalar(out=sc[:, 12:16], in0=sc[:, 8:12], scalar1=QS, scalar2=None, op0=ALU.mult), reads=[sc], writes=[sc])
                    S.op("dve", lambda: nc.vector.tensor_tensor(out=sc[:, 16:20], in0=sc[:, 8:12], in1=b.gb[:, b0:b0 + 4], op=ALU.mult), reads=[sc, b.gb], writes=[sc])
                    S.op("dve", lambda: nc.vector.tensor_scalar(out=sc[:, 20:24], in0=b.gb[:, b0:b0 + 4], scalar1=-1.0, scalar2=None, op0=ALU.mult), reads=[b.gb], writes=[sc])
                    S.op("dve", lambda: nc.vector.tensor_tensor(out=sc[:, 32:36], in0=sc[:, 4:8], in1=sc[:, 0:4], op=ALU.subtract), reads=[sc], writes=[sc])
                    S.op("act", lambda: nc.scalar.activation(out=sc[:, 24:28], in_=sc[:, 32:36], func=AF.Exp), reads=[sc], writes=[sc])
                    S.op("dve", lambda: nc.vector.tensor_tensor(out=b.E1s[:], in0=b.E1[:], in1=mS[d][:], op=ALU.mult), reads=[b.E1, mS[d]], writes=[b.E1s])
                    S.op("dve", lambda: nc.vector.tensor_tensor(out=b.E1i[:], in0=b.E1[:], in1=mI[d][:], op=ALU.mult), reads=[b.E1, mI[d]], writes=[b.E1i])
                    for h in range(4):
                        S.op("pe", lambda h=h: nc.tensor.matmul(F[2][:, h, :], lhsT=b.kT[:, h, :], rhs=b.kT[:, h, :], start=True, stop=True),
                             reads=[b.kT], writes=[F[2]])
                        S.op("pe", lambda h=h: nc.tensor.matmul(F[3][:, h, :], lhsT=b.qT[:, h, :], rhs=b.kT[:, h, :], start=True, stop=True),
                             reads=[b.qT, b.kT], writes=[F[3]])
                    for h in range(4):
                        S.op("dve", lambda h=h: nc.vector.scalar_tensor_tensor(out=b.XA[:, h, :], in0=F[2][:, h, :], scalar=sc[:, 20 + h:21 + h],
                                                                                in1=b.E1s[:, h * 128:(h + 1) * 128], op0=ALU.mult, op1=ALU.mult),
                             reads=[F[2], sc, b.E1s], writes=[b.XA])
                    S.op("dve", lambda: nc.vector.scalar_tensor_tensor(out=b.XA[:, 4:8, :].rearrange("p a b -> p (a b)"), in0=F[3][:].rearrange("p a b -> p (a b)"),
                                                                        scalar=QS, in1=b.E1i[:], op0=ALU.mult, op1=ALU.mult),
                         reads=[F[3], b.E1i], writes=[b.XA])
                    for i in range(8):
                        S.op("pe", lambda i=i: nc.tensor.transpose(TPb[:, i, :], b.XA[:, i, :], ident_bf[:]), reads=[b.XA, ident_bf], writes=[TPb])
                    S.op("dve", lambda: nc.vector.tensor_copy(out=b.YA[:], in_=TPb[:]), reads=[TPb], writes=[b.YA])
                    i4f = I4f[:].rearrange("p (a b) -> p a b", b=128)
                    S.op("dve", lambda: nc.vector.tensor_tensor(out=b.E1sB[:], in0=b.E1s[:], in1=bdm[:], op=ALU.mult), reads=[b.E1s, bdm], writes=[b.E1sB])
                    for h in range(4):
                        S.op("dve", lambda h=h: nc.vector.scalar_tensor_tensor(out=b.XF[0][:, h, :], in0=F[2][:, h, :], scalar=sc[:, 20 + h:21 + h],
                                                                                in1=b.E1sB[:, h * 128:(h + 1) * 128], op0=ALU.mult, op1=ALU.mult),
                             reads=[F[2], sc, b.E1sB], writes=[b.XF[0]])
                    for h in range(4):
                        S.op("pe", lambda h=h: nc.tensor.transpose(F[4][:, h, :], b.XF[0][:, h, :], ident_f[:]), reads=[b.XF[0], ident_f], writes=[F[4]])
                    S.op("dve", lambda: nc.vector.tensor_copy(out=b.YF[0][:], in_=F[4][:]), reads=[F[4]], writes=[b.YF[0]])
                    S.op("dve", lambda: nc.vector.tensor_tensor(out=b.TtF[0][:], in0=b.YF[0][:], in1=i4f, op=ALU.add), reads=[b.YF[0], I4f], writes=[b.TtF[0]])
                    S.op("dve", lambda: nc.vector.tensor_tensor(out=b.TnF[0][:], in0=b.XF[0][:], in1=i4f, op=ALU.add), reads=[b.XF[0], I4f], writes=[b.TnF[0]])
                    Xc, Yc = b.XF[0], b.YF[0]
                    cur = 0
                    for lev in range(1, 5):
                        last = (lev == 4)
                        Xn, Yn = b.XF[lev % 2], b.YF[lev % 2]
                        for h in range(4):
                            S.op("pe", lambda h=h: nc.tensor.matmul(F[2][:, h, :], lhsT=Xc[:, h, :], rhs=Yc[:, h, :], start=True, stop=True),
                                 reads=[Xc, Yc], writes=[F[2]])
                        if not last:
                            for h in range(4):
                                S.op("pe", lambda h=h: nc.tensor.matmul(F[3][:, h, :], lhsT=Yc[:, h, :], rhs=Xc[:, h, :], start=True, stop=True),
                                     reads=[Xc, Yc], writes=[F[3]])
                        S.op("dve", lambda: nc.vector.tensor_tensor(out=b.IYF[:], in0=F[2][:], in1=i4f, op=ALU.add), reads=[F[2], I4f], writes=[b.IYF])
                        if not last:
                            S.op("dve", lambda: nc.vector.tensor_copy(out=Yn[:], in_=F[2][:]), reads=[F[2]], writes=[Yn])
                            S.op("act", lambda: nc.scalar.copy(out=Xn[:], in_=F[3][:]), reads=[F[3]], writes=[Xn])
                        Tn_o, Tt_n, Tn_n = b.TnF[cur], b.TtF[1 - cur], b.TnF[1 - cur]
                        for h in range(4):
                            S.op("pe", lambda h=h: nc.tensor.matmul(F[4][:, h, :], lhsT=Tn_o[:, h, :], rhs=b.IYF[:, h, :], start=True, stop=True),
                                 reads=[Tn_o, b.IYF], writes=[F[4]])
                        if not last:
                            for h in range(4):
                                S.op("pe", lambda h=h: nc.tensor.matmul(F[5][:, h, :], lhsT=b.IYF[:, h, :], rhs=Tn_o[:, h, :], start=True, stop=True),
                                     reads=[Tn_o, b.IYF], writes=[F[5]])
                        S.op("dve", lambda: nc.vector.tensor_copy(out=Tt_n[:], in_=F[4][:]), reads=[F[4]], writes=[Tt_n])
                        if not last:
                            S.op("act", lambda: nc.scalar.copy(out=Tn_n[:], in_=F[5][:]), reads=[F[5]], writes=[Tn_n])
                        cur = 1 - cur
                        Xc, Yc = Xn, Yn
                    DgT = b.TtF[cur]
                    for I_ in range(4):
                        S.op("dve", lambda I_=I_: nc.vector.tensor_tensor(out=b.G[I_][:].rearrange("p a b -> p (a b)"), in0=DgT[:].rearrange("p a b -> p (a b)"),
                                                                          in1=cm[I_][:], op=ALU.mult), reads=[DgT, cm[I_]], writes=[b.G[I_]])
                    for h in range(4):
                        S.op("dve", lambda h=h: nc.vector.tensor_scalar(out=b.Bt[:, h, 0:128], in0=b.vt[:, h * 128:(h + 1) * 128], scalar1=b.gb[:, b0 + h:b0 + h + 1], scalar2=None, op0=ALU.mult),
                             reads=[b.vt, b.gb], writes=[b.Bt])
                        S.op("act", lambda h=h: nc.scalar.activation(out=b.Bt[:, h, 128:256], in_=b.kt[:, h * 128:(h + 1) * 128], func=AF.Copy, scale=sc[:, 16 + h:17 + h]),
                             reads=[b.kt, sc], writes=[b.Bt])
                        S.op("act", lambda h=h: nc.scalar.activation(out=b.kd[:, h, :], in_=b.kt[:, h * 128:(h + 1) * 128], func=AF.Copy, scale=sc[:, 24 + h:25 + h]),
                             reads=[b.kt, sc], writes=[b.kd])

                    def pv(h):
                        return F[2 + h // 2][:].rearrange("p a b -> p (a b)")[:, (h % 2) * 256:(h % 2 + 1) * 256]

                    def px(h):
                        return F[4 + h // 2][:].rearrange("p a b -> p (a b)")[:, (h % 2) * 256:(h % 2 + 1) * 256]

                    def fl(t_, lo, hi):
                        return t_[:, lo:hi, :].rearrange("p a b -> p (a b)")

                    for step_, I_ in enumerate((0, 1, 2, 3) if d == 0 else (3, 2, 1, 0)):
                        if step_ == 0:
                            rhs_t = b.Bt
                        else:
                            for h in range(4):
                                S.op("pe", lambda h=h: nc.tensor.matmul(pv(h), lhsT=b.YA[:, h, :], rhs=b.Xs[:, h, :], start=True, stop=True),
                                     reads=[b.YA, b.Xs], writes=[F[2 + h // 2]])
                            for q_ in range(2):
                                S.op("dve", lambda q_=q_: nc.vector.tensor_tensor(out=fl(b.Vt, 2 * q_, 2 * q_ + 2), in0=F[2 + q_][:].rearrange("p a b -> p (a b)"),
                                                                                  in1=fl(b.Bt, 2 * q_, 2 * q_ + 2), op=ALU.add), reads=[F[2 + q_], b.Bt], writes=[b.Vt])
                            rhs_t = b.Vt
                        for h in range(4):
                            S.op("pe", lambda h=h: nc.tensor.matmul(px(h), lhsT=b.G[I_][:, h, :], rhs=rhs_t[:, h, :], start=True, stop=True),
                                 reads=[b.G[I_], rhs_t], writes=[F[4 + h // 2]])
                        for q_ in range(2):
                            if step_ == 0:
                                S.op("dve", lambda q_=q_: nc.vector.tensor_copy(out=fl(b.Xs, 2 * q_, 2 * q_ + 2), in_=F[4 + q_][:].rearrange("p a b -> p (a b)")),
                                     reads=[F[4 + q_]], writes=[b.Xs])
                            else:
                                S.op("dve", lambda q_=q_: nc.vector.tensor_tensor(out=fl(b.Xs, 2 * q_, 2 * q_ + 2), in0=F[4 + q_][:].rearrange("p a b -> p (a b)"),
                                                                                  in1=fl(b.Xs, 2 * q_, 2 * q_ + 2), op=ALU.add), reads=[F[4 + q_], b.Xs], writes=[b.Xs])
                    for h in range(4):
                        S.op("pe", lambda h=h: nc.tensor.transpose(TPb[:, h, :], b.Xs[:, h, 128:256], ident_bf[:]), reads=[b.Xs, ident_bf], writes=[TPb])
                    S.op("dve", lambda: nc.vector.tensor_scalar(out=b.wTn[:], in0=TPb[:, 0:4, :], scalar1=-1.0, scalar2=None, op0=ALU.mult), reads=[TPb], writes=[b.wTn])
                    for h in range(4):
                        S.op("pe", lambda h=h: nc.tensor.matmul(F[6][:, h, :], lhsT=b.wTn[:, h, :], rhs=Sb_[:, h, :], start=True, stop=True),
                             reads=[b.wTn, Sb_], writes=[F[6]])
                    S.op("dve", lambda: nc.vector.tensor_tensor(out=b.vn[:], in0=F[6][:], in1=b.Xs[:, :, 0:128], op=ALU.add), reads=[F[6], b.Xs], writes=[b.vn])
                    if want_o:
                        for h in range(4):
                            S.op("pe", lambda h=h: nc.tensor.matmul(F[4][:, h, :], lhsT=b.qT[:, h, :], rhs=Sb_[:, h, :], start=True, stop=True),
                                 reads=[b.qT, Sb_], writes=[F[4]])
                            S.op("pe", lambda h=h: nc.tensor.matmul(F[5][:, h, :], lhsT=b.YA[:, 4 + h, :], rhs=b.vn[:, h, :], start=True, stop=True),
                                 reads=[b.YA, b.vn], writes=[F[5]])
                        S.op("act", lambda: nc.scalar.copy(out=b.AVs[:], in_=F[5][:]), reads=[F[5]], writes=[b.AVs])
                        for h in range(4):
                            S.op("dve", lambda h=h: nc.vector.scalar_tensor_tensor(out=b.o[:, h, :], in0=F[4][:, h, :], scalar=sc[:, 12 + h:13 + h], in1=b.AVs[:, h, :],
                                                                                    op0=ALU.mult, op1=ALU.add), reads=[F[4], sc, b.AVs], writes=[b.o])
                        S.dma("pool", lambda: nc.gpsimd.dma_start(out=o_dst[t0:t0 + 128, :], in_=b.o[:].rearrange("p a b -> p (a b)")), reads=[b.o])
                    for h in range(4):
                        S.op("pe", lambda h=h: nc.tensor.matmul(F[1][:, h, :], lhsT=b.kd[:, h, :], rhs=b.vn[:, h, :], start=True, stop=True),
                             reads=[b.kd, b.vn], writes=[F[1]])
                    for h in range(4):
                        S.op("dve", lambda h=h: nc.vector.scalar_tensor_tensor(out=Sd[:, h, :], in0=Sd[:, h, :], scalar=sc[:, 28 + h:29 + h], in1=F[1][:, h, :],
                                                                                op0=ALU.mult, op1=ALU.add), reads=[Sd, sc, F[1]], writes=[Sd])
                    S.op("act", lambda: nc.scalar.copy(out=Sb_[:], in_=Sd[:]), reads=[Sd], writes=[Sb_])

                csrc = (cqT_d, ckT_d, ck_d, cv_d, cgb_d)
                msrc = (qT_d, kT_d, k_d, v_d, gb_d)
                if ulimit is not None:
                    for (n_, d_) in ulimit:
                        unit(n_, d_, csrc, False, None)
                    S.barrier()
                    return True
                for n in range(2):
                    unit(n, 0, csrc, False, None)
                    unit(1 - n, 1, csrc, False, None)
                if dbg:
                    for d in range(2):
                        S.dma("pool", lambda d=d: nc.gpsimd.dma_start(out=s0_d[d], in_=Sst[d][:]), reads=[Sst[d]])
                for s_ in range(nch):
                    unit(s_, 0, msrc, True, of_d)
                    unit(nch - 1 - s_, 1, msrc, True, ob_d)
                    if s_ in (21, 43):
                        S.new_epoch()
                S.new_epoch()
            return False

        if 2 in run:
            if stage2():
                return nc, S

        def stage3():
            st3 = contextlib.ExitStack()
            with st3:
                sb = lambda name, shape, dt: mk(st3, "sb", name, shape, dt)
                ps = lambda name, shape, dt: mk(st3, "ps", name, shape, dt)
                cosA = sb("cosA", [128, 64, 128], BF16)
                sinA = sb("sinA", [128, 64, 128], BF16)
                nsinA = sb("nsinA", [128, 64, 128], BF16)
                cs64 = sb("cs64", [128, 64], BF16)
                S.dma("sp", lambda: nc.sync.dma_start(out=cosA[:], in_=cosA_d), writes=[cosA])
                S.dma("sp", lambda: nc.sync.dma_start(out=sinA[:], in_=sinA_d), writes=[sinA])
                S.dma("sp", lambda: nc.sync.dma_start(out=nsinA[:], in_=nsinA_d), writes=[nsinA])
                S.dma("sp", lambda: nc.sync.dma_start(out=cs64[:], in_=cs64_d), writes=[cs64])
                fm = sb("fm", [128, 4, T], BF16)
                pqa = [sb("pqa%d" % i, [128, 1024], BF16) for i in range(2)]
                zs = [sb("zs3_%d" % i, [128, 2, 512], BF16) for i in range(2)]
                zin = [sb("zin%d" % i, [128, 16, 512], BF16) for i in range(2)]
                P = [ps("P3_%d" % i, [128, 512], F32) for i in range(4)]
                pq_v = pq_d.rearrange("(b a) c -> a b c", a=64)
                for a in range(64):
                    pb = pqa[a % 2]
                    zb = zs[a % 2]
                    S.dma("sp", lambda: nc.sync.dma_start(out=pb[:], in_=pq_v[a]), writes=[pb])
                    pr, pi = P[(a % 2) * 2], P[(a % 2) * 2 + 1]
                    S.op("pe", lambda: nc.tensor.matmul(pr[:], lhsT=cosA[:, a, :], rhs=pb[:, 0:512], start=True, stop=False), reads=[cosA, pb], writes=[pr])
                    S.op("pe", lambda: nc.tensor.matmul(pr[:], lhsT=nsinA[:, a, :], rhs=pb[:, 512:1024], start=False, stop=True), reads=[nsinA, pb], writes=[pr])
                    S.op("pe", lambda: nc.tensor.matmul(pi[:], lhsT=cosA[:, a, :], rhs=pb[:, 512:1024], start=True, stop=False), reads=[cosA, pb], writes=[pi])
                    S.op("pe", lambda: nc.tensor.matmul(pi[:], lhsT=sinA[:, a, :], rhs=pb[:, 0:512], start=False, stop=True), reads=[sinA, pb], writes=[pi])
                    S.op("dve", lambda: nc.vector.tensor_copy(out=zb[:, 0, :], in_=pr[:]), reads=[pr], writes=[zb])
                    S.op("act", lambda: nc.scalar.copy(out=zb[:, 1, :], in_=pi[:]), reads=[pi], writes=[zb])
                    S.dma("pool", lambda: nc.gpsimd.dma_start(out=Zd[:, a, :, :].rearrange("r p c -> p r c"), in_=zb[:]), reads=[zb])
                S.barrier()
                zv = Zd.rearrange("r a b c -> (r a) b c")
                for g in range(8):
                    zi = zin[g % 2]
                    S.dma("sp", lambda: nc.sync.dma_start(out=zi[:], in_=zv[:, g * 16:(g + 1) * 16, :]), writes=[zi])
                    for half in range(2):
                        for ct in range(4):
                            pp = P[(half * 4 + ct) % 4]
                            ppv = pp[:].rearrange("p (b a) -> p b a", a=64)
                            for bl in range(8):
                                S.op("pe", lambda bl=bl: nc.tensor.matmul(ppv[:, bl, :], lhsT=zi[:, half * 8 + bl, ct * 128:(ct + 1) * 128], rhs=cs64[:],
                                                                          start=True, stop=True), reads=[zi, cs64], writes=[pp])
                            b0 = g * 16 + half * 8
                            ov = fm[:, ct, :].rearrange("p (a b) -> p b a", b=128)[:, b0:b0 + 8, :]
                            if ct % 2 == 0:
                                S.op("dve", lambda: nc.vector.tensor_scalar(out=ov, in0=ppv, scalar1=1.0 / 1024.0, scalar2=None, op0=ALU.mult), reads=[pp], writes=[fm])
                            else:
                                S.op("act", lambda: nc.scalar.activation(out=ov, in_=ppv, func=AF.Copy, scale=1.0 / 1024.0), reads=[pp], writes=[fm])
                for ct in range(4):
                    S.dma("pool", lambda ct=ct: nc.gpsimd.dma_start(out=fmT_d[ct], in_=fm[:, ct, :]), reads=[fm])
                S.barrier()
            return False

        if 3 in run:
            if stage3():
                return nc, S

        def stage4():
            st4 = contextlib.ExitStack()
            with st4:
                sb = lambda name, shape, dt: mk(st4, "sb", name, shape, dt)
                ps = lambda name, shape, dt: mk(st4, "ps", name, shape, dt)
                comb = sb("comb", [128, NT4, 32], F32)
                gate2b = sb("gate2b", [128, D], F32)
                fingb = sb("fingb", [128, D], F32)
                S.dma("sp", lambda: nc.sync.dma_start(out=gate2b[:], in_=modrow_d[0:1, 5120:6144].partition_broadcast(128)), writes=[gate2b])
                S.dma("sp", lambda: nc.sync.dma_start(out=fingb[:], in_=fing_d.partition_broadcast(128)), writes=[fingb])
                junk4 = sb("junk4", [128, D], BF16)
                PS = [ps("P4_%d" % i, [128, 512], F32) for i in range(7)]
                PTb = ps("PTb4", [128, 8, 128], BF16)
                sa4 = contextlib.ExitStack()
                sa4.__enter__()
                sA = lambda name, shape, dt: mk(sa4, "sb", name, shape, dt)
                gate1b = sA("gate1b", [128, D], F32); gs2b = sA("gs2b", [128, D], F32); sh2b = sA("sh2b", [128, D], F32)
                n2gb = sA("n2gb", [128, D], F32)
                S.dma("sp", lambda: nc.sync.dma_start(out=gate1b[:], in_=modrow_d[0:1, 2048:3072].partition_broadcast(128)), writes=[gate1b])
                S.dma("sp", lambda: nc.sync.dma_start(out=sh2b[:], in_=modrow_d[0:1, 3072:4096].partition_broadcast(128)), writes=[sh2b])
                S.dma("sp", lambda: nc.sync.dma_start(out=gs2b[:], in_=modrow_d[0:1, 4096:5120].partition_broadcast(128)), writes=[gs2b])
                S.dma("sp", lambda: nc.sync.dma_start(out=n2gb[:], in_=n2g_d.partition_broadcast(128)), writes=[n2gb])
                S.op("dve", lambda: nc.vector.scalar_tensor_tensor(out=gs2b[:], in0=gs2b[:], scalar=1.0, in1=n2gb[:], op0=ALU.add, op1=ALU.mult),
                     reads=[gs2b, n2gb], writes=[gs2b])
                og4 = sA("og4", [128, 512], F32)
                for h in range(4):
                    S.dma("sp", lambda h=h: nc.sync.dma_start(out=og4[:, h * 128:(h + 1) * 128], in_=onorm_d.partition_broadcast(128)), writes=[og4])
                brb = sA("brb", [128, 36], F32)
                S.dma("sp", lambda: nc.sync.dma_start(out=brb[:, 0:4], in_=bgrp_d.partition_broadcast(128)), writes=[brb])
                S.dma("sp", lambda: nc.sync.dma_start(out=brb[:, 4:36], in_=brt_d.partition_broadcast(128)), writes=[brb])
                wrg = sA("wrg", [128, 8, 36], F32)
                with nc.allow_non_contiguous_dma(reason="tiny router weights"):
                    S.dma("sp", lambda: nc.sync.dma_start(out=wrg[:, :, 0:4], in_=wgrp_d.rearrange("(k p) n -> p k n", p=128)), writes=[wrg])
                    S.dma("sp", lambda: nc.sync.dma_start(out=wrg[:, :, 4:36], in_=wrt_d.rearrange("(k p) n -> p k n", p=128)), writes=[wrg])
                w_out = sA("w_out", [128, 8, D], BF16)
                S.dma("pool", lambda: nc.gpsimd.dma_start(out=w_out[:], in_=w_out_d.rearrange("(k p) n -> p k n", p=128)), writes=[w_out])
                mixT = [sA("mixT%d" % i, [128, 8, 512], BF16) for i in range(2)]
                oft = [sA("oft%d" % i, [128, 512], F32) for i in range(2)]
                obt = [sA("obt%d" % i, [128, 512], F32) for i in range(2)]
                zt = [sA("zt%d" % i, [128, 512], BF16) for i in range(2)]
                gz = sA("gz", [128, 512], F32)
                ogb = sA("ogb", [128, 512], BF16)
                s4 = [sA("s4_%d" % i, [128, 48], F32) for i in range(2)]
                xt4 = [sA("xt4_%d" % i, [128, D], F32) for i in range(2)]
                x1t = [sA("x1t%d" % i, [128, D], F32) for i in range(2)]
                h2t = [sA("h2t%d" % i, [128, D], F32) for i in range(2)]
                h2T32 = sA("h2T32", [128, 8, 128], F32)
                h2Tb = [sA("h2Tb%d" % i, [128, 8, 128], BF16) for i in range(2)]
                lg = [sA("lg%d" % i, [128, 36], F32) for i in range(2)]
                lm = sA("lm", [128, 32], F32); lm2 = sA("lm2", [128, 32], F32)
                sel1 = sA("sel1", [128, 32], F32); sel2 = sA("sel2", [128, 32], F32)
                for j in range(NT4 // 4):
                    mT = mixT[j % 2]
                    for ct in range(4):
                        S.dma("sp", lambda ct=ct: nc.sync.dma_start(out=mT[:, ct, :], in_=fmT_d[ct, :, j * 512:(j + 1) * 512]), writes=[mT])
                    for tt in range(4):
                        ti = j * 4 + tt
                        r0 = ti * 128
                        a_, b_, z_, sc_ = oft[ti % 2], obt[ti % 2], zt[ti % 2], s4[ti % 2]
                        S.dma("sp", lambda: nc.sync.dma_start(out=a_[:], in_=of_d[r0:r0 + 128, :]), writes=[a_])
                        S.dma("sp", lambda: nc.sync.dma_start(out=b_[:], in_=ob_d[r0:r0 + 128, :]), writes=[b_])
                        S.dma("sp", lambda: nc.sync.dma_start(out=z_[:], in_=z_d[r0:r0 + 128, :]), writes=[z_])
                        S.op("dve", lambda: nc.vector.tensor_tensor(out=a_[:], in0=a_[:], in1=b_[:], op=ALU.add), reads=[a_, b_], writes=[a_])
                        for h in range(4):
                            S.op("act", lambda h=h: nc.scalar.activation(out=junk4[:, 0:128], in_=a_[:, h * 128:(h + 1) * 128], func=AF.Square, accum_out=sc_[:, h:h + 1]),
                                 reads=[a_], writes=[junk4, sc_])
                        S.op("dve", lambda: nc.vector.tensor_scalar(out=sc_[:, 0:4], in0=sc_[:, 0:4], scalar1=1.0 / 128, scalar2=EPS, op0=ALU.mult, op1=ALU.add), reads=[sc_], writes=[sc_])
                        S.op("act", lambda: nc.scalar.sqrt(out=sc_[:, 0:4], in_=sc_[:, 0:4]), reads=[sc_], writes=[sc_])
                        S.op("dve", lambda: nc.vector.reciprocal(out=sc_[:, 0:4], in_=sc_[:, 0:4]), reads=[sc_], writes=[sc_])
                        S.op("dve", lambda: nc.vector.tensor_tensor(out=gz[:], in0=z_[:], in1=og4[:], op=ALU.mult), reads=[z_, og4], writes=[gz])
                        for h in range(4):
                            S.op("dve", lambda h=h: nc.vector.scalar_tensor_tensor(out=ogb[:, h * 128:(h + 1) * 128], in0=a_[:, h * 128:(h + 1) * 128], scalar=sc_[:, h:h + 1],
                                                                                    in1=gz[:, h * 128:(h + 1) * 128], op0=ALU.mult, op1=ALU.mult), reads=[a_, sc_, gz], writes=[ogb])
                        for h in range(4):
                            S.op("pe", lambda h=h: nc.tensor.transpose(PTb[:, h, :], ogb[:, h * 128:(h + 1) * 128], ident_bf[:]), reads=[ogb, ident_bf], writes=[PTb])
                        S.op("dve", lambda: nc.vector.tensor_copy(out=mT[:, 4:8, tt * 128:(tt + 1) * 128], in_=PTb[:, 0:4, :]), reads=[PTb], writes=[mT])
                    for tt in range(4):
                        ti = j * 4 + tt
                        r0 = ti * 128
                        xb, x1b, h2b, sc_ = xt4[ti % 2], x1t[ti % 2], h2t[ti % 2], s4[ti % 2]
                        S.dma("sp", lambda: nc.sync.dma_start(out=xb[:], in_=x_d[r0:r0 + 128, :]), writes=[xb])
                        for half in range(2):
                            pp = PS[half]
                            for k in range(8):
                                S.op("pe", lambda k=k: nc.tensor.matmul(pp[:], lhsT=mT[:, k, tt * 128:(tt + 1) * 128], rhs=w_out[:, k, half * 512:(half + 1) * 512],
                                                                        start=(k == 0), stop=(k == 7)), reads=[mT, w_out], writes=[pp])
                            hs = slice(half * 512, (half + 1) * 512)
                            S.op("dve", lambda: nc.vector.tensor_tensor(out=x1b[:, hs], in0=pp[:], in1=gate1b[:, hs], op=ALU.mult), reads=[pp, gate1b], writes=[x1b])
                        S.op("dve", lambda: nc.vector.tensor_tensor(out=x1b[:], in0=x1b[:], in1=xb[:], op=ALU.add), reads=[x1b, xb], writes=[x1b])
                        S.dma("pool", lambda: nc.gpsimd.dma_start(out=x1_d[r0:r0 + 128, :], in_=x1b[:]), reads=[x1b])
                        S.op("act", lambda: nc.scalar.activation(out=junk4[:], in_=x1b[:], func=AF.Square, accum_out=sc_[:, 8:9]), reads=[x1b], writes=[junk4, sc_])
                        S.op("dve", lambda: nc.vector.tensor_scalar(out=sc_[:, 8:9], in0=sc_[:, 8:9], scalar1=1.0 / D, scalar2=EPS, op0=ALU.mult, op1=ALU.add), reads=[sc_], writes=[sc_])
                        S.op("act", lambda: nc.scalar.sqrt(out=sc_[:, 8:9], in_=sc_[:, 8:9]), reads=[sc_], writes=[sc_])
                        S.op("dve", lambda: nc.vector.reciprocal(out=sc_[:, 8:9], in_=sc_[:, 8:9]), reads=[sc_], writes=[sc_])
                        S.op("dve", lambda: nc.vector.scalar_tensor_tensor(out=h2b[:], in0=x1b[:], scalar=sc_[:, 8:9], in1=gs2b[:], op0=ALU.mult, op1=ALU.mult),
                             reads=[x1b, sc_, gs2b], writes=[h2b])
                        S.op("dve", lambda: nc.vector.tensor_tensor(out=h2b[:], in0=h2b[:], in1=sh2b[:], op=ALU.add), reads=[h2b, sh2b], writes=[h2b])
                        for k in range(8):
                            pp = PS[2 + k // 4]
                            S.op("pe", lambda k=k, pp=pp: nc.tensor.transpose(pp[:, (k % 4) * 128:(k % 4 + 1) * 128], h2b[:, k * 128:(k + 1) * 128], ident_f[:]),
                                 reads=[h2b, ident_f], writes=[pp])
                        hb = h2Tb[ti % 2]
                        for q_ in range(2):
                            pp = PS[2 + q_]
                            S.op("dve", lambda: nc.vector.tensor_copy(out=h2T32[:, q_ * 4:(q_ + 1) * 4, :].rearrange("p a b -> p (a b)"), in_=pp[:]), reads=[pp], writes=[h2T32])
                            S.op("act", lambda: nc.scalar.copy(out=hb[:, q_ * 4:(q_ + 1) * 4, :].rearrange("p a b -> p (a b)"), in_=pp[:]), reads=[pp], writes=[hb])
                        S.dma("pool", lambda: nc.gpsimd.dma_start(out=h2T_d[:, :, r0:r0 + 128].rearrange("k p t -> p k t"), in_=hb[:]), reads=[hb])
                        lt = lg[ti % 2]
                        for k in range(8):
                            S.op("pe", lambda k=k: nc.tensor.matmul(PS[4][:, 0:36], lhsT=h2T32[:, k, :], rhs=wrg[:, k, :], start=(k == 0), stop=(k == 7)),
                                 reads=[h2T32, wrg], writes=[PS[4]])
                        S.op("dve", lambda: nc.vector.tensor_tensor(out=lt[:], in0=PS[4][:, 0:36], in1=brb[:], op=ALU.add), reads=[PS[4], brb], writes=[lt])
                        S.op("dve", lambda: nc.vector.tensor_reduce(out=sc_[:, 16:17], in_=lt[:, 0:4], axis=AX.X, op=ALU.max), reads=[lt], writes=[sc_])
                        S.op("dve", lambda: nc.vector.tensor_scalar(out=sc_[:, 17:18], in0=sc_[:, 16:17], scalar1=-1.0, scalar2=None, op0=ALU.mult), reads=[sc_], writes=[sc_])
                        S.op("act", lambda: nc.scalar.activation(out=sc_[:, 36:40], in_=lt[:, 0:4], func=AF.Exp, bias=sc_[:, 17:18], scale=1.0, accum_out=sc_[:, 18:19]),
                             reads=[lt, sc_], writes=[sc_])
                        S.op("dve", lambda: nc.vector.reciprocal(out=sc_[:, 19:20], in_=sc_[:, 18:19]), reads=[sc_], writes=[sc_])
                        S.op("dve", lambda: nc.vector.tensor_scalar(out=sc_[:, 20:24], in0=lt[:, 0:4], scalar1=sc_[:, 16:17], scalar2=None, op0=ALU.is_equal), reads=[lt, sc_], writes=[sc_])
                        S.op("dve", lambda: nc.vector.tensor_scalar(out=sc_[:, 24:28], in0=sc_[:, 20:24], scalar1=-1.0, scalar2=1e30, op0=ALU.add, op1=ALU.mult), reads=[sc_], writes=[sc_])
                        for g in range(4):
                            S.op("dve", lambda g=g: nc.vector.tensor_scalar(out=lm[:, g * 8:(g + 1) * 8], in0=lt[:, 4 + g * 8:12 + g * 8], scalar1=sc_[:, 20 + g:21 + g],
                                                                             scalar2=sc_[:, 24 + g:25 + g], op0=ALU.mult, op1=ALU.add), reads=[lt, sc_], writes=[lm])
                        S.op("dve", lambda: nc.vector.tensor_reduce(out=sc_[:, 28:29], in_=lm[:], axis=AX.X, op=ALU.max), reads=[lm], writes=[sc_])
                        S.op("dve", lambda: nc.vector.tensor_scalar(out=sel1[:], in0=lm[:], scalar1=sc_[:, 28:29], scalar2=None, op0=ALU.is_equal), reads=[lm, sc_], writes=[sel1])
                        S.op("dve", lambda: nc.vector.scalar_tensor_tensor(out=lm2[:], in0=sel1[:], scalar=-1e30, in1=lm[:], op0=ALU.mult, op1=ALU.add), reads=[sel1, lm], writes=[lm2])
                        S.op("dve", lambda: nc.vector.tensor_reduce(out=sc_[:, 29:30], in_=lm2[:], axis=AX.X, op=ALU.max), reads=[lm2], writes=[sc_])
                        S.op("dve", lambda: nc.vector.tensor_scalar(out=sel2[:], in0=lm2[:], scalar1=sc_[:, 29:30], scalar2=None, op0=ALU.is_equal), reads=[lm2, sc_], writes=[sel2])
                        S.op("dve", lambda: nc.vector.tensor_tensor(out=sc_[:, 30:31], in0=sc_[:, 28:29], in1=sc_[:, 29:30], op=ALU.subtract), reads=[sc_], writes=[sc_])
                        S.op("act", lambda: nc.scalar.activation(out=sc_[:, 31:32], in_=sc_[:, 30:31], func=AF.Sigmoid), reads=[sc_], writes=[sc_])
                        S.op("dve", lambda: nc.vector.tensor_tensor(out=sc_[:, 31:32], in0=sc_[:, 31:32], in1=sc_[:, 19:20], op=ALU.mult), reads=[sc_], writes=[sc_])
                        S.op("dve", lambda: nc.vector.tensor_tensor(out=sc_[:, 32:33], in0=sc_[:, 19:20], in1=sc_[:, 31:32], op=ALU.subtract), reads=[sc_], writes=[sc_])
                        S.op("dve", lambda: nc.vector.tensor_scalar(out=comb[:, ti, :], in0=sel1[:], scalar1=sc_[:, 31:32], scalar2=None, op0=ALU.mult), reads=[sel1, sc_], writes=[comb])
                        S.op("dve", lambda: nc.vector.scalar_tensor_tensor(out=comb[:, ti, :], in0=sel2[:], scalar=sc_[:, 32:33], in1=comb[:, ti, :], op0=ALU.mult, op1=ALU.add),
                             reads=[sel2, sc_, comb], writes=[comb])
                if dbg:
                    S.dma("pool", lambda: nc.gpsimd.dma_start(out=comb_d[0:NT4 * 128, :].rearrange("(t p) e -> p t e", p=128), in_=comb[:]), reads=[comb])
                S.new_epoch()
                sa4.__exit__(None, None, None)
                if stages <= 3.5:
                    return True
                NG = max(1, NT4 // 16)
                TPG = NT4 // NG
                acc = sb("acc", [128, TPG, D], F32)
                wg = [sb("wg%d" % i, [128, 8, 512], BF16) for i in range(2)]
                wu = [sb("wu%d" % i, [128, 8, 512], BF16) for i in range(2)]
                wd = [sb("wd%d" % i, [128, 4, D], BF16) for i in range(2)]
                hblk = [sb("hblk%d" % i, [128, 8, 512], BF16) for i in range(2)]
                sg = [sb("sg%d" % i, [128, 512], F32) for i in range(2)]
                hid = [sb("hid%d" % i, [128, 4, 512], BF16) for i in range(2)]
                x1f = [sb("x1f%d" % i, [128, D], F32) for i in range(2)]
                sf = [sb("sf%d" % i, [128, 4], F32) for i in range(2)]
                cnt4 = {"h": 0}
                for grp in range(NG):
                    for e in range(NEXP):
                        g_, u_, d_ = wg[e % 2], wu[e % 2], wd[e % 2]
                        S.dma("pool", lambda: nc.gpsimd.dma_start(out=g_[:], in_=wgate_d[e].rearrange("(k p) n -> p k n", p=128)), writes=[g_])
                        S.dma("pool", lambda: nc.gpsimd.dma_start(out=u_[:], in_=wup_d[e].rearrange("(k p) n -> p k n", p=128)), writes=[u_])
                        S.dma("pool", lambda: nc.gpsimd.dma_start(out=d_[:], in_=wdown_d[e].rearrange("(k p) n -> p k n", p=128)), writes=[d_])
                        for jb in range(TPG // 4):
                            t0 = (grp * TPG + jb * 4) * 128
                            hb = hblk[cnt4["h"] % 2]
                            hd = hid[cnt4["h"] % 2]
                            cnt4["h"] += 1
                            S.dma("sp", lambda: nc.sync.dma_start(out=hb[:], in_=h2T_d[:, :, t0:t0 + 512].rearrange("k p t -> p k t")), writes=[hb])
                            for hc in range(4):
                                pg_, pu_ = PS[(hc % 2) * 2], PS[(hc % 2) * 2 + 1]
                                for k in range(8):
                                    S.op("pe", lambda k=k: nc.tensor.matmul(pg_[:], lhsT=g_[:, k, hc * 128:(hc + 1) * 128], rhs=hb[:, k, :], start=(k == 0), stop=(k == 7)),
                                         reads=[g_, hb], writes=[pg_])
                                for k in range(8):
                                    S.op("pe", lambda k=k: nc.tensor.matmul(pu_[:], lhsT=u_[:, k, hc * 128:(hc + 1) * 128], rhs=hb[:, k, :], start=(k == 0), stop=(k == 7)),
                                         reads=[u_, hb], writes=[pu_])
                                sgb = sg[hc % 2]
                                S.op("act", lambda: nc.scalar.activation(out=sgb[:], in_=pg_[:], func=AF.Silu), reads=[pg_], writes=[sgb])
                                S.op("dve", lambda: nc.vector.tensor_tensor(out=hd[:, hc, :], in0=pu_[:], in1=sgb[:], op=ALU.mult), reads=[pu_, sgb], writes=[hd])
                            for tt in range(4):
                                tl = jb * 4 + tt
                                tg = grp * TPG + tl
                                for half in range(2):
                                    py = PS[4 + (tt * 2 + half) % 3]
                                    for hc in range(4):
                                        S.op("pe", lambda hc=hc: nc.tensor.matmul(py[:], lhsT=hd[:, hc, tt * 128:(tt + 1) * 128], rhs=d_[:, hc, half * 512:(half + 1) * 512],
                                                                                  start=(hc == 0), stop=(hc == 3)), reads=[hd, d_], writes=[py])
                                    hs = slice(half * 512, (half + 1) * 512)
                                    if e == 0:
                                        S.op("dve", lambda: nc.vector.tensor_scalar(out=acc[:, tl, hs], in0=py[:], scalar1=comb[:, tg, e:e + 1], scalar2=None, op0=ALU.mult),
                                             reads=[py, comb], writes=[acc])
                                    else:
                                        S.op("dve", lambda: nc.vector.scalar_tensor_tensor(out=acc[:, tl, hs], in0=py[:], scalar=comb[:, tg, e:e + 1], in1=acc[:, tl, hs],
                                                                                            op0=ALU.mult, op1=ALU.add), reads=[py, comb, acc], writes=[acc])
                    for tl in range(TPG):
                        tg = grp * TPG + tl
                        r0 = tg * 128
                        xb = x1f[tl % 2]
                        sc_ = sf[tl % 2]
                        S.dma("sp", lambda: nc.sync.dma_start(out=xb[:], in_=x1_d[r0:r0 + 128, :]), writes=[xb])
                        S.op("dve", lambda: nc.vector.tensor_tensor(out=acc[:, tl, :], in0=acc[:, tl, :], in1=gate2b[:], op=ALU.mult), reads=[acc, gate2b], writes=[acc])
                        S.op("dve", lambda: nc.vector.tensor_tensor(out=xb[:], in0=xb[:], in1=acc[:, tl, :], op=ALU.add), reads=[xb, acc], writes=[xb])
                        S.op("act", lambda: nc.scalar.activation(out=junk4[:], in_=xb[:], func=AF.Square, accum_out=sc_[:, 0:1]), reads=[xb], writes=[junk4, sc_])
                        S.op("dve", lambda: nc.vector.tensor_scalar(out=sc_[:, 0:1], in0=sc_[:, 0:1], scalar1=1.0 / D, scalar2=EPS, op0=ALU.mult, op1=ALU.add), reads=[sc_], writes=[sc_])
                        S.op("act", lambda: nc.scalar.sqrt(out=sc_[:, 0:1], in_=sc_[:, 0:1]), reads=[sc_], writes=[sc_])
                        S.op("dve", lambda: nc.vector.reciprocal(out=sc_[:, 0:1], in_=sc_[:, 0:1]), reads=[sc_], writes=[sc_])
                        S.op("dve", lambda: nc.vector.scalar_tensor_tensor(out=xb[:], in0=xb[:], scalar=sc_[:, 0:1], in1=fingb[:], op0=ALU.mult, op1=ALU.mult),
                             reads=[xb, sc_, fingb], writes=[xb])
                        S.dma("pool", lambda: nc.gpsimd.dma_start(out=out_d[r0:r0 + 128, :], in_=xb[:]), reads=[xb])
                    if grp == 1 and NG > 2:
                        S.new_epoch()
                S.barrier()
            return False

        if 4 in run:
            if stage4():
                return nc, S
        if stages <= 1:
            return nc, S

    return nc, S


def make_inputs(core, inp):
    b = core
    m = {}
    m["x"] = np.ascontiguousarray(inp["x"][b])
    m["ctx"] = np.ascontiguousarray(inp["ctx"][b])
    cT = np.zeros((128, 16), np.float32)
    cT[:, 0:8] = inp["c"][b].reshape(8, 128).T
    cT[:, 8:16] = inp["c_ctx"].reshape(8, 128).T
    m["cT"] = cT
    m["w_mod"] = np.ascontiguousarray(inp["w_mod"][0])
    m["b_mod"] = np.ascontiguousarray(inp["b_mod"][0].reshape(1, -1))
    m["n1gT"] = np.ascontiguousarray(inp["norm1_g"][0].reshape(8, 128).T)
    m["w_in"] = np.ascontiguousarray(inp["w_in"][0])
    cw = inp["conv_w"][0].reshape(9, 12, 128)
    m["convw"] = np.ascontiguousarray(cw.transpose(2, 1, 0))
    m["a_log"] = np.ascontiguousarray(inp["a_log"][0].reshape(1, 8))
    m["dt_bias"] = np.ascontiguousarray(inp["dt_bias"][0].reshape(1, 8))
    m["n2g"] = np.ascontiguousarray(inp["norm2_g"][0].reshape(1, -1))
    m["fing"] = np.ascontiguousarray(inp["final_g"].reshape(1, -1))
    m["onorm"] = np.ascontiguousarray(inp["onorm_g"][0].reshape(1, -1))
    m["b_group"] = np.ascontiguousarray(inp["b_group"][0].reshape(1, -1))
    m["b_router"] = np.ascontiguousarray(inp["b_router"][0].reshape(1, -1))
    m["w_group"] = np.ascontiguousarray(inp["w_group"][0])
    m["w_router"] = np.ascontiguousarray(inp["w_router"][0])
    m["w_out"] = np.ascontiguousarray(inp["w_out"][0])
    m["w_gate"] = np.ascontiguousarray(inp["w_gate"][0])
    m["w_up"] = np.ascontiguousarray(inp["w_up"][0])
    m["w_down"] = np.ascontiguousarray(inp["w_down"][0])
    m.update(host_consts())
    return m


def kernel(**inp):
    inp = {k: np.asarray(v) for k, v in inp.items()}
    nc, S = build()
    in_maps = [make_inputs(c, inp) for c in range(8)]
    res = run_bass_kernel_spmd(nc, in_maps, core_ids=list(range(8)))
    return np.stack([r["out"] for r in res.results], axis=0).astype(np.float32)
```

```python
import contextlib
import numpy as np
import ml_dtypes
import concourse.bass as bass
import concourse.mybir as mybir
from concourse.bass_utils import run_bass_kernel_spmd

F32 = mybir.dt.float32
BF16 = mybir.dt.bfloat16
I32 = mybir.dt.int32
AF = mybir.ActivationFunctionType
ALU = mybir.AluOpType
AX = mybir.AxisListType

T = 8192
D = 1024
TC = 256
NB = 16
EPS = 1e-6


class TB:
    def __init__(self, t, name=""):
        self.t = t
        self.name = name
        self.w = None
        self.r = []
        self.psum = False

    def __getitem__(self, k):
        return self.t[k]


class Sync:
    ENG = ("pe", "dve", "act", "pool", "sp")

    def __init__(self, nc, n_dma_sems=64):
        self.nc = nc
        self.e = {"pe": nc.tensor, "dve": nc.vector, "act": nc.scalar, "pool": nc.gpsimd, "sp": nc.sync}
        self.sem = {k: nc.alloc_semaphore(name="c_" + k) for k in self.ENG}
        self.cnt = {k: 0 for k in self.ENG}
        self.seen = {k: {} for k in self.ENG}
        self.dsems = [nc.alloc_semaphore(name="d_%d" % i) for i in range(n_dma_sems)]
        self.dcnt = [0] * n_dma_sems
        self.dnext = 0
        self.n_hw = n_dma_sems - 8
        self.pnext = 0
        self.n_ins = 0
        self.epoch = 0
        self.limit = None
        self.log = []

    def _wait(self, eng, tok):
        if tok is None:
            return
        kind, key, val = tok[0], tok[1], tok[2]
        if kind == "c":
            if tok[3] < self.epoch:
                return
            if key == "pe" and eng == "pe":
                return
            sem = self.sem[key]
            sk = "c" + key
        else:
            sem = self.dsems[key]
            sk = "d%d" % key
        if self.seen[eng].get(sk, 0) >= val:
            return
        self.e[eng].wait_ge(sem, val)
        self.seen[eng][sk] = val

    def _deps(self, eng, reads, writes):
        for b in reads:
            self._wait(eng, b.w)
            if b.psum:
                for t in b.r:
                    self._wait(eng, t)
        for b in writes:
            self._wait(eng, b.w)
            for t in b.r:
                self._wait(eng, t)

    def _commit(self, tok, reads, writes):
        for b in reads:
            b.r.append(tok)
            if len(b.r) > 48:
                b.r = b.r[-48:]
        for b in writes:
            b.w = tok
            b.r = []

    def op(self, eng, fn, reads=(), writes=()):
        if self.limit is not None and self.n_ins >= self.limit:
            return None
        self._deps(eng, reads, writes)
        self.log.append((self.n_ins, eng, fn.__code__.co_firstlineno))
        ins = fn()
        ins.then_inc(self.sem[eng], 1)
        self.cnt[eng] += 1
        self.n_ins += 1
        tok = ("c", eng, self.cnt[eng], self.epoch)
        self._commit(tok, reads, writes)
        return tok

    def dma(self, eng, fn, reads=(), writes=()):
        if self.limit is not None and self.n_ins >= self.limit:
            return None
        if eng == "pool":
            i = self.n_hw + self.pnext
            self.pnext = (self.pnext + 1) % 8
        else:
            i = self.dnext
            self.dnext = (self.dnext + 1) % self.n_hw
        if self.dcnt[i] > 0:
            self._wait(eng, ("d", i, self.dcnt[i]))
        self._deps(eng, reads, writes)
        self.log.append((self.n_ins, eng + "-dma", fn.__code__.co_firstlineno))
        ins = fn()
        self.dcnt[i] += 16
        ins.then_inc(self.dsems[i], 16)
        self.n_ins += 1
        tok = ("d", i, self.dcnt[i])
        self._commit(tok, reads, writes)
        return tok

    def barrier(self):
        for eng in self.ENG:
            for k in self.ENG:
                if self.cnt[k] > 0 and k != eng:
                    self._wait(eng, ("c", k, self.cnt[k], self.epoch))
            for i in range(len(self.dsems)):
                if self.dcnt[i] > 0:
                    self._wait(eng, ("d", i, self.dcnt[i]))

    def new_epoch(self):
        self.barrier()
        self.epoch += 1
        self.sem = {k: self.nc.alloc_semaphore(name="c%d_%s" % (self.epoch, k)) for k in self.ENG}
        self.cnt = {k: 0 for k in self.ENG}
        for eng in self.ENG:
            self.seen[eng] = {k: v for k, v in self.seen[eng].items() if not k.startswith("c")}


def host_consts():
    c = {}
    c["ident_bf"] = np.eye(128, dtype=np.float32).astype(ml_dtypes.bfloat16)
    c["ident_f"] = np.eye(128, dtype=np.float32)
    n = np.arange(128)
    ang = 2 * np.pi * np.outer(n, n) / 128.0
    c["cs128"] = np.concatenate([np.cos(ang), np.sin(ang)], axis=1).astype(np.float32).astype(ml_dtypes.bfloat16)
    c["ones_bf"] = np.ones((128, 128), np.float32).astype(ml_dtypes.bfloat16)
    c["ones_f"] = np.ones((128, 128), np.float32)
    c["zf"] = np.zeros((128, 512), np.float32)
    c["zb"] = np.zeros((128, 7680), np.float32).astype(ml_dtypes.bfloat16)
    ii = np.arange(128)[:, None]
    jj = np.arange(128)[None, :]
    c["mS0"] = np.tile((ii > jj).astype(np.float32), (1, 4)); c["mI0"] = np.tile((ii >= jj).astype(np.float32), (1, 4))
    c["mS1"] = np.tile((ii < jj).astype(np.float32), (1, 4)); c["mI1"] = np.tile((ii <= jj).astype(np.float32), (1, 4))
    c["Lm0"] = (ii <= jj).astype(np.float32)
    c["Lm1"] = (ii >= jj).astype(np.float32)
    c["Um0"] = (ii > jj).astype(np.float32)
    c["Um1"] = (ii < jj).astype(np.float32)
    bb = np.arange(128)[:, None, None]; aa = np.arange(64)[None, :, None]; bp = np.arange(128)[None, None, :]
    th = 2 * np.pi * ((bp * (64 * bb + aa)) % 8192) / 8192.0
    c["cosA"] = np.cos(th).astype(np.float32).astype(ml_dtypes.bfloat16)
    c["sinA"] = np.sin(th).astype(np.float32).astype(ml_dtypes.bfloat16)
    c["nsinA"] = (-np.sin(th)).astype(np.float32).astype(ml_dtypes.bfloat16)
    a1 = np.arange(64)[:, None]; a2 = np.arange(64)[None, :]
    ps_ = 2 * np.pi * ((a1 * a2) % 64) / 64.0
    c["cs64"] = np.concatenate([np.cos(ps_), -np.sin(ps_)], axis=0).astype(np.float32).astype(ml_dtypes.bfloat16)
    c["I4f"] = np.tile(np.eye(128, dtype=np.float32), (1, 4))
    c["bdm"] = np.tile(((ii // 32) == (jj // 32)).astype(np.float32), (1, 4))
    for i_ in range(4):
        c["cm%d" % i_] = np.tile(((jj // 32) == i_).astype(np.float32) * np.ones((128, 1), np.float32), (1, 4))
    c["I4"] = np.tile(np.eye(128, dtype=np.float32), (1, 4)).astype(ml_dtypes.bfloat16)
    return c


def build(stages=4, dbg=False, nblk=NB, run=(1, 2, 3, 4), feed=(), nch=64, NT4=64, NEXP=32, ulimit=None, limit=None):
    nc = bass.Bass("TRN2", target_bir_lowering=False)
    S = Sync(nc)
    S.limit = limit
    E = S.e

    def din(name, shape, dt=F32):
        return nc.dram_tensor(name, list(shape), dt, kind="ExternalInput").ap()

    def dscr(name, shape, dt, out=False):
        if name in feed:
            return nc.dram_tensor(name, list(shape), dt, kind="ExternalInput").ap()
        return nc.dram_tensor(name, list(shape), dt, kind=("ExternalOutput" if (out or (dbg and (dbg is True or name in dbg))) else "Internal")).ap()

    x_d = din("x", [T, D])
    ctx_d = din("ctx", [TC, D])
    cT_d = din("cT", [128, 16])
    w_mod_d = din("w_mod", [D, 6 * D])
    b_mod_d = din("b_mod", [1, 6 * D])
    n1gT_d = din("n1gT", [128, 8])
    w_in_d = din("w_in", [D, 2576])
    convw_d = din("convw", [128, 12, 9])
    alog_d = din("a_log", [1, 8])
    dtb_d = din("dt_bias", [1, 8])
    ident_bf_d = din("ident_bf", [128, 128], BF16)
    ident_f_d = din("ident_f", [128, 128])
    cs128_d = din("cs128", [128, 256], BF16)
    ones_bf_d = din("ones_bf", [128, 128], BF16)
    ones_f_d = din("ones_f", [128, 128])
    zf_d = din("zf", [128, 512])
    zb_d = din("zb", [128, 7680], BF16)

    qT_d = dscr("qT_s", [4, 128, T], BF16)
    kT_d = dscr("kT_s", [4, 128, T], BF16)
    k_d = dscr("k_s", [T, 512], BF16)
    v_d = dscr("v_s", [T, 512], BF16)
    z_d = dscr("z_s", [T, 512], BF16)
    pq_d = dscr("pq_s", [T, 1024], BF16)
    gb_d = dscr("gb_s", [T, 16], F32)
    cqT_d = dscr("cqT_s", [4, 128, TC], BF16)
    ckT_d = dscr("ckT_s", [4, 128, TC], BF16)
    ck_d = dscr("ck_s", [TC, 512], BF16)
    cv_d = dscr("cv_s", [TC, 512], BF16)
    cgb_d = dscr("cgb_s", [TC, 16], F32)
    modrow_d = dscr("modrow_s", [2, 6 * D], F32)

    of_d = dscr("of_s", [T, 512], F32)
    ob_d = dscr("ob_s", [T, 512], F32)
    s0_d = [dscr("s0_%d" % d, [128, 4, 128], F32) for d in range(2)]
    mS_d = [din("mS%d" % d, [128, 512]) for d in range(2)]
    mI_d = [din("mI%d" % d, [128, 512]) for d in range(2)]
    Lm_d = [din("Lm%d" % d, [128, 128]) for d in range(2)]
    Um_d = [din("Um%d" % d, [128, 128]) for d in range(2)]
    I4_d = din("I4", [128, 512], BF16)
    I4f_d = din("I4f", [128, 512])
    bdm_d = din("bdm", [128, 512])
    cm_d = [din("cm%d" % i, [128, 512]) for i in range(4)]
    Zd = dscr("Z_s", [2, 64, 128, 512], BF16)
    fmT_d = dscr("fmT_s", [4, 128, T], BF16)
    cosA_d = din("cosA", [128, 64, 128], BF16)
    sinA_d = din("sinA", [128, 64, 128], BF16)
    nsinA_d = din("nsinA", [128, 64, 128], BF16)
    cs64_d = din("cs64", [128, 64], BF16)
    x1_d = dscr("x1_s", [T, D], F32)
    h2T_d = dscr("h2T_s", [8, 128, T], BF16)
    comb_d = dscr("comb_s", [T, 32], F32)
    n2g_d = din("n2g", [1, D])
    fing_d = din("fing", [1, D])
    onorm_d = din("onorm", [1, 128])
    bgrp_d = din("b_group", [1, 4])
    brt_d = din("b_router", [1, 32])
    wgrp_d = din("w_group", [D, 4])
    wrt_d = din("w_router", [D, 32])
    w_out_d = din("w_out", [D, D])
    wgate_d = din("w_gate", [32, D, 512])
    wup_d = din("w_up", [32, D, 512])
    wdown_d = din("w_down", [32, 512, D])
    out_d = nc.dram_tensor("out", [T, D], F32, kind="ExternalOutput").ap()

    glob = contextlib.ExitStack()

    def mk(stack, space, name, shape, dt):
        if space == "sb":
            t = stack.enter_context(nc.sbuf_tensor("t_" + name, list(shape), dt))
        else:
            t = stack.enter_context(nc.psum_tensor("t_" + name, list(shape), dt))
            tb = TB(t, name)
            tb.psum = True
            return tb
        return TB(t, name)

    with glob:
        ident_bf = mk(glob, "sb", "ident_bf", [128, 128], BF16)
        ident_f = mk(glob, "sb", "ident_f", [128, 128], F32)
        ones_bf = mk(glob, "sb", "ones_bf", [128, 128], BF16)
        ones_f = mk(glob, "sb", "ones_f", [128, 128], F32)
        modT = mk(glob, "sb", "modT", [128, 48, 2], F32)
        for tb, d in ((ident_bf, ident_bf_d), (ident_f, ident_f_d), (ones_bf, ones_bf_d), (ones_f, ones_f_d)):
            S.dma("sp", lambda tb=tb, d=d: nc.sync.dma_start(out=tb[:], in_=d), writes=[tb])

        def stage1():
            st = contextlib.ExitStack()
            with st:
                sb = lambda name, shape, dt: mk(st, "sb", name, shape, dt)
                ps = lambda name, shape, dt: mk(st, "ps", name, shape, dt)
                n1g = sb("n1g", [128, 8], F32)
                gs1 = sb("gs1", [128, 8, 2], F32)
                w_in = sb("w_in", [128, 8, 2064], BF16)
                cs128 = sb("cs128", [128, 256], BF16)
                wpq = sb("wpq", [128, 8, 1024], BF16)
                convw = sb("convw", [128, 12, 9], F32)
                alog = sb("alog", [128, 8], F32)
                dtb = sb("dtb", [128, 8], F32)
                nea = sb("nea", [128, 8], F32)
                sa = contextlib.ExitStack()
                sa.__enter__()
                sbA = lambda name, shape, dt: mk(sa, "sb", name, shape, dt)
                cT = sbA("cT", [128, 16], F32)
                sc = sbA("sc", [128, 16], F32)
                S.dma("sp", lambda: nc.sync.dma_start(out=cT[:], in_=cT_d), writes=[cT])
                S.op("act", lambda: nc.scalar.activation(out=sc[:], in_=cT[:], func=AF.Silu), reads=[cT], writes=[sc])
                scl = sbA("scl", [128, 8, 2], F32)
                S.op("dve", lambda: nc.vector.tensor_copy(out=scl[:, :, 0], in_=sc[:, 0:8]), reads=[sc], writes=[scl])
                S.op("dve", lambda: nc.vector.tensor_copy(out=scl[:, :, 1], in_=sc[:, 8:16]), reads=[sc], writes=[scl])
                wm = [sbA("wm%d" % i, [128, 8, 512], F32) for i in range(2)]
                bmod = sbA("bmod", [2, 6 * D], F32)
                for r in range(2):
                    S.dma("sp", lambda r=r: nc.sync.dma_start(out=bmod[r:r + 1, :], in_=b_mod_d), writes=[bmod])
                modrow = sbA("modrow", [2, 6 * D], F32)
                pm = [ps("pm%d" % i, [128, 512], F32) for i in range(2)]
                for g in range(12):
                    w = wm[g % 2]
                    S.dma("sp", lambda w=w, g=g: nc.sync.dma_start(
                        out=w[:], in_=w_mod_d[:, g * 512:(g + 1) * 512].rearrange("(k p) n -> p k n", p=128)), writes=[w])
                    p = pm[g % 2]
                    for k in range(8):
                        S.op("pe", lambda p=p, w=w, k=k: nc.tensor.matmul(p[0:2, :], lhsT=scl[:, k, :], rhs=w[:, k, :],
                                                                           start=(k == 0), stop=(k == 7)),
                             reads=[scl, w], writes=[p])
                    S.op("dve", lambda p=p, g=g: nc.vector.tensor_tensor(out=modrow[:, g * 512:(g + 1) * 512], in0=p[0:2, :],
                                                                          in1=bmod[:, g * 512:(g + 1) * 512], op=ALU.add),
                         reads=[p, bmod], writes=[modrow])
                pGb = ps("pGb", [128, 512], F32)
                pmt = TB(pGb.t, "pmt")
                pmtv = pGb[:, 0:96].rearrange("p (a b) -> p a b", b=2)
                for blk in range(48):
                    S.op("pe", lambda blk=blk: nc.tensor.transpose(pmtv[:, blk, :], modrow[0:2, blk * 128:(blk + 1) * 128],
                                                                   ident_f[0:2, 0:2]), reads=[modrow, ident_f], writes=[pGb])
                S.op("dve", lambda: nc.vector.tensor_copy(out=modT[:], in_=pmtv), reads=[pGb], writes=[modT])
                S.dma("pool", lambda: nc.gpsimd.dma_start(out=modrow_d, in_=modrow[:]), reads=[modrow])
                S.dma("sp", lambda: nc.sync.dma_start(out=n1g[:], in_=n1gT_d), writes=[n1g])
                for j in range(2):
                    S.op("dve", lambda j=j: nc.vector.scalar_tensor_tensor(out=gs1[:, :, j], in0=modT[:, 8:16, j], scalar=1.0,
                                                                            in1=n1g[:], op0=ALU.add, op1=ALU.mult),
                         reads=[modT, n1g], writes=[gs1])

                S.dma("pool", lambda: nc.gpsimd.dma_start(out=w_in[:], in_=w_in_d[:, 512:2576].rearrange("(k p) n -> p k n", p=128)),
                      writes=[w_in])
                wf = sbA("wf", [128, 8, 512], BF16)
                S.dma("pool", lambda: nc.gpsimd.dma_start(out=wf[:], in_=w_in_d[:, 0:512].rearrange("(k p) n -> p k n", p=128)),
                      writes=[wf])
                S.dma("sp", lambda: nc.sync.dma_start(out=cs128[:], in_=cs128_d), writes=[cs128])
                wfT = sbA("wfT", [128, 4, 1024], BF16)
                ptr = [ps("ptr%d" % i, [128, 8, 128], BF16) for i in range(1)]
                for g in range(4):
                    for k in range(8):
                        S.op("pe", lambda g=g, k=k: nc.tensor.transpose(ptr[0][:, k, :], wf[:, k, g * 128:(g + 1) * 128], ident_bf[:]),
                             reads=[wf, ident_bf], writes=[ptr[0]])
                    S.op("dve", lambda g=g: nc.vector.tensor_copy(out=wfT[:, g, :], in_=ptr[0][:].rearrange("p a b -> p (a b)")),
                         reads=[ptr[0]], writes=[wfT])
                for k in range(8):
                    p = pm[k % 2]
                    for g in range(4):
                        S.op("pe", lambda p=p, g=g, k=k: nc.tensor.matmul(p[:, (g % 2) * 256:(g % 2) * 256 + 256],
                                                                           lhsT=wfT[:, g, k * 128:(k + 1) * 128], rhs=cs128[:], start=True, stop=True),
                             reads=[wfT, cs128], writes=[p])
                        S.op("act", lambda p=p, g=g, k=k: nc.scalar.copy(out=wpq[:, k, g * 128:(g + 1) * 128],
                                                                          in_=p[:, (g % 2) * 256:(g % 2) * 256 + 128]),
                             reads=[p], writes=[wpq])
                        S.op("act", lambda p=p, g=g, k=k: nc.scalar.copy(out=wpq[:, k, 512 + g * 128:512 + (g + 1) * 128],
                                                                          in_=p[:, (g % 2) * 256 + 128:(g % 2) * 256 + 256]),
                             reads=[p], writes=[wpq])

                S.dma("sp", lambda: nc.sync.dma_start(out=convw[:], in_=convw_d), writes=[convw])
                S.dma("sp", lambda: nc.sync.dma_start(out=alog[:], in_=alog_d.partition_broadcast(128)), writes=[alog])
                S.dma("sp", lambda: nc.sync.dma_start(out=dtb[:], in_=dtb_d.partition_broadcast(128)), writes=[dtb])
                S.op("act", lambda: nc.scalar.activation(out=nea[:], in_=alog[:], func=AF.Exp), reads=[alog], writes=[nea])
                S.op("dve", lambda: nc.vector.tensor_scalar(out=nea[:], in0=nea[:], scalar1=-1.0, scalar2=None, op0=ALU.mult),
                     reads=[nea], writes=[nea])

                S.barrier()
                sa.__exit__(None, None, None)
                if stages <= 0.3:
                    return True
                xt = [sb("xt%d" % i, [128, D], F32) for i in range(2)]
                xn = [sb("xn%d" % i, [128, D], BF16) for i in range(2)]
                junk = sb("junk", [128, D], BF16)
                ssq = [sb("ssq%d" % i, [128, 1], F32) for i in range(4)]
                hT = [sb("hT%d" % i, [128, 8, 512], BF16) for i in range(2)]
                pre = [sb("pre%d" % i, [128, 12, 640], BF16) for i in range(3)]
                cacc = [sb("cacc%d" % i, [128, 512], F32) for i in range(4)]
                cvT = sb("cvT", [128, 12, 512], BF16)
                sq = [sb("sq%d" % i, [128, 512], BF16) for i in range(2)]
                rs = [sb("rs%d" % i, [128, 512], F32) for i in range(2)]
                nT = [sb("nT%d" % i, [128, 8, 512], BF16) for i in range(1)]
                ktok = [sb("ktok%d" % i, [128, 512], BF16) for i in range(2)]
                vtok = [sb("vtok%d" % i, [128, 512], BF16) for i in range(2)]
                pqs = [sb("pqs%d" % i, [128, 1024], BF16) for i in range(2)]
                zs = [sb("zs%d" % i, [128, 512], BF16) for i in range(2)]
                gts = [sb("gts%d" % i, [128, 16], F32) for i in range(2)]
                gbs = [sb("gbs%d" % i, [128, 16], F32) for i in range(2)]
                pT = ps("pT", [128, 8, 128], BF16)
                pA = pm
                pB = [ps("pB%d" % i, [128, 512], F32) for i in range(2)]
                pZ = ps("pZ", [128, 512], F32)
                pG = pGb
                pX = ptr[0]

                cnt = {"x": 0, "n": 0, "s": 0, "c": 0}

                def norm_transpose(src_d, row0, jmod, hTb, col0):
                    xb = xt[cnt["x"] % 2]
                    xnb = xn[cnt["n"] % 2]
                    sq1 = ssq[cnt["s"] % 4]
                    cnt["x"] += 1; cnt["n"] += 1; cnt["s"] += 1
                    S.dma("sp", lambda: nc.sync.dma_start(out=xb[:], in_=src_d[row0:row0 + 128, :]), writes=[xb])
                    S.op("act", lambda: nc.scalar.activation(out=junk[:], in_=xb[:], func=AF.Square, accum_out=sq1[:]),
                         reads=[xb], writes=[junk, sq1])
                    S.op("dve", lambda: nc.vector.tensor_scalar(out=sq1[:], in0=sq1[:], scalar1=1.0 / D, scalar2=EPS, op0=ALU.mult, op1=ALU.add),
                         reads=[sq1], writes=[sq1])
                    S.op("act", lambda: nc.scalar.sqrt(out=sq1[:], in_=sq1[:]), reads=[sq1], writes=[sq1])
                    S.op("dve", lambda: nc.vector.reciprocal(out=sq1[:], in_=sq1[:]), reads=[sq1], writes=[sq1])
                    S.op("act", lambda: nc.scalar.activation(out=xnb[:], in_=xb[:], func=AF.Copy, scale=sq1[:]),
                         reads=[xb, sq1], writes=[xnb])
                    for k in range(8):
                        S.op("pe", lambda k=k: nc.tensor.transpose(pT[:, k, :], xnb[:, k * 128:(k + 1) * 128], ident_bf[:]),
                             reads=[xnb, ident_bf], writes=[pT])
                    for k in range(8):
                        eng = "dve" if k % 2 == 0 else "pool"
                        if eng == "pool":
                            S.op("act", lambda k=k: nc.scalar.activation(out=hTb[:, k, col0:col0 + 128], in_=pT[:, k, :], func=AF.Identity,
                                                                          scale=gs1[:, k, jmod:jmod + 1], bias=modT[:, k, jmod:jmod + 1]),
                                 reads=[pT, gs1, modT], writes=[hTb])
                        else:
                            S.op("dve", lambda k=k: nc.vector.tensor_scalar(out=hTb[:, k, col0:col0 + 128], in0=pT[:, k, :],
                                                                             scalar1=gs1[:, k, jmod:jmod + 1], scalar2=modT[:, k, jmod:jmod + 1],
                                                                             op0=ALU.mult, op1=ALU.add),
                                 reads=[pT, gs1, modT], writes=[hTb])

                def gates_math(gt, gbo):
                    S.op("act", lambda: nc.scalar.activation(out=gbo[:, 0:8], in_=gt[:, 0:8], func=AF.Sigmoid), reads=[gt], writes=[gbo])
                    S.op("dve", lambda: nc.vector.tensor_tensor(out=gt[:, 8:16], in0=gt[:, 8:16], in1=dtb[:], op=ALU.add),
                         reads=[gt, dtb], writes=[gt])
                    S.op("act", lambda: nc.scalar.activation(out=gt[:, 8:16], in_=gt[:, 8:16], func=AF.Exp), reads=[gt], writes=[gt])
                    S.op("act", lambda: nc.scalar.activation(out=gt[:, 8:16], in_=gt[:, 8:16], func=AF.Ln, bias=1.0, scale=1.0),
                         reads=[gt], writes=[gt])
                    S.op("dve", lambda: nc.vector.tensor_tensor(out=gbo[:, 8:16], in0=gt[:, 8:16], in1=nea[:], op=ALU.mult),
                         reads=[gt, nea], writes=[gbo])

                def features(cv, ntok, qTd, kTd, kd, vd, tok0):
                    nTb = nT[0]
                    cnt["c"] += 1
                    for i in range(8):
                        sqb = sq[i % 2]
                        rsb = rs[i % 2]
                        pa = pA[i % 2]
                        S.op("act", lambda: nc.scalar.activation(out=sqb[:, 0:ntok], in_=cv[:, i, 0:ntok], func=AF.Square),
                             reads=[cv], writes=[sqb])
                        S.op("pe", lambda: nc.tensor.matmul(pa[:, 0:ntok], lhsT=ones_bf[:], rhs=sqb[:, 0:ntok], start=True, stop=True),
                             reads=[ones_bf, sqb], writes=[pa])
                        S.op("dve", lambda: nc.vector.tensor_scalar(out=rsb[:, 0:ntok], in0=pa[:, 0:ntok], scalar1=EPS, scalar2=None,
                                                                     op0=ALU.add), reads=[pa], writes=[rsb])
                        S.op("act", lambda: nc.scalar.sqrt(out=rsb[:, 0:ntok], in_=rsb[:, 0:ntok]), reads=[rsb], writes=[rsb])
                        S.op("dve", lambda: nc.vector.reciprocal(out=rsb[:, 0:ntok], in_=rsb[:, 0:ntok]), reads=[rsb], writes=[rsb])
                        S.op("dve", lambda: nc.vector.tensor_tensor(out=nTb[:, i, 0:ntok], in0=cv[:, i, 0:ntok], in1=rsb[:, 0:ntok], op=ALU.mult),
                             reads=[cv, rsb], writes=[nTb])
                    for h in range(4):
                        S.dma("pool", lambda h=h: nc.gpsimd.dma_start(out=qTd[h, :, tok0:tok0 + ntok], in_=nTb[:, h, 0:ntok]), reads=[nTb])
                        S.dma("pool", lambda h=h: nc.gpsimd.dma_start(out=kTd[h, :, tok0:tok0 + ntok], in_=nTb[:, 4 + h, 0:ntok]), reads=[nTb])
                    if stages <= 0.57:
                        return
                    for tt in range(ntok // 128):
                        kb = ktok[tt % 2]
                        vb = vtok[tt % 2]
                        for h in range(4):
                            S.op("pe", lambda h=h: nc.tensor.transpose(pX[:, h, :], nTb[:, 4 + h, tt * 128:(tt + 1) * 128], ident_bf[:]),
                                 reads=[nTb, ident_bf], writes=[pX])
                            S.op("pe", lambda h=h: nc.tensor.transpose(pX[:, 4 + h, :], cv[:, 8 + h, tt * 128:(tt + 1) * 128], ident_bf[:]),
                                 reads=[cv, ident_bf], writes=[pX])
                        S.op("dve", lambda: nc.vector.tensor_copy(out=kb[:], in_=pX[:, 0:4, :].rearrange("p a b -> p (a b)")),
                             reads=[pX], writes=[kb])
                        S.op("dve", lambda: nc.vector.tensor_copy(out=vb[:], in_=pX[:, 4:8, :].rearrange("p a b -> p (a b)")),
                             reads=[pX], writes=[vb])
                        r0 = tok0 + tt * 128
                        S.dma("pool", lambda: nc.gpsimd.dma_start(out=kd[r0:r0 + 128, :], in_=kb[:]), reads=[kb])
                        S.dma("pool", lambda: nc.gpsimd.dma_start(out=vd[r0:r0 + 128, :], in_=vb[:]), reads=[vb])

                hTc = hT[0]
                for tt in range(2):
                    norm_transpose(ctx_d, tt * 128, 1, hTc, tt * 128)
                if stages <= 0.4:
                    S.barrier()
                    return True
                prc = pre[0]
                for ct in range(12):
                    pa = pA[ct % 2]
                    for k in range(8):
                        S.op("pe", lambda k=k: nc.tensor.matmul(pa[:, 0:256], lhsT=w_in[:, k, ct * 128:(ct + 1) * 128], rhs=hTc[:, k, 0:256],
                                                                start=(k == 0), stop=(k == 7)), reads=[w_in, hTc], writes=[pa])
                    S.op("act", lambda: nc.scalar.copy(out=prc[:, ct, 0:256], in_=pa[:, 0:256]), reads=[pa], writes=[prc])
                for tt in range(2):
                    gt = gts[tt % 2]
                    gbo = gbs[tt % 2]
                    for k in range(8):
                        S.op("pe", lambda k=k: nc.tensor.matmul(pG[:, 0:16], lhsT=hTc[:, k, tt * 128:(tt + 1) * 128], rhs=w_in[:, k, 2048:2064],
                                                                start=(k == 0), stop=(k == 7)), reads=[hTc, w_in], writes=[pG])
                    S.op("dve", lambda: nc.vector.tensor_copy(out=gt[:], in_=pG[:, 0:16]), reads=[pG], writes=[gt])
                    gates_math(gt, gbo)
                    S.dma("pool", lambda: nc.gpsimd.dma_start(out=cgb_d[tt * 128:(tt + 1) * 128, :], in_=gbo[:]), reads=[gbo])
                if stages <= 0.5:
                    S.barrier()
                    return True
                for ct in range(12):
                    ca = cacc[ct % 4]
                    eng = "dve"
                    V = E[eng]
                    S.op(eng, lambda: V.tensor_scalar(out=ca[:, 0:256], in0=prc[:, ct, 0:256], scalar1=convw[:, ct, 4:5], scalar2=None, op0=ALU.mult),
                         reads=[prc, convw], writes=[ca])
                    S.op(eng, lambda: V.scalar_tensor_tensor(out=ca[:, 1:256], in0=prc[:, ct, 0:255], scalar=convw[:, ct, 3:4], in1=ca[:, 1:256],
                                                             op0=ALU.mult, op1=ALU.add), reads=[prc, convw, ca], writes=[ca])
                    S.op(eng, lambda: V.scalar_tensor_tensor(out=ca[:, 0:255], in0=prc[:, ct, 1:256], scalar=convw[:, ct, 5:6], in1=ca[:, 0:255],
                                                             op0=ALU.mult, op1=ALU.add), reads=[prc, convw, ca], writes=[ca])
                    S.op("act", lambda: nc.scalar.activation(out=cvT[:, ct, 0:256], in_=ca[:, 0:256], func=AF.Silu), reads=[ca], writes=[cvT])
                if stages <= 0.55:
                    S.barrier()
                    return True
                features(cvT, 256, cqT_d, ckT_d, ck_d, cv_d, 0)
                if stages <= 0.6:
                    S.barrier()
                    return True

                for pb in pre:
                    S.dma("sp", lambda pb=pb: nc.sync.dma_start(out=pb[:].rearrange("p a b -> p (a b)"), in_=zb_d), writes=[pb])

                def inproj(j):
                    hTb = hT[j % 2]
                    for tt in range(4):
                        norm_transpose(x_d, j * 512 + tt * 128, 0, hTb, tt * 128)
                    pj = pre[j % 3]
                    for ct in range(12):
                        pa = pA[ct % 2]
                        for k in range(8):
                            S.op("pe", lambda k=k: nc.tensor.matmul(pa[:], lhsT=w_in[:, k, ct * 128:(ct + 1) * 128], rhs=hTb[:, k, :],
                                                                    start=(k == 0), stop=(k == 7)), reads=[w_in, hTb], writes=[pa])
                        S.op("act", lambda: nc.scalar.copy(out=pj[:, ct, 64:576], in_=pa[:]), reads=[pa], writes=[pj])
                        if j > 0:
                            pp = pre[(j - 1) % 3]
                            S.op("dve", lambda: nc.vector.tensor_copy(out=pp[:, ct, 576:640], in_=pa[:, 0:64]), reads=[pa], writes=[pp])
                        if j < NB - 1:
                            pn = pre[(j + 1) % 3]
                            S.op("dve", lambda: nc.vector.tensor_copy(out=pn[:, ct, 0:64], in_=pa[:, 448:512]), reads=[pa], writes=[pn])
                    if j == NB - 1:
                        S.dma("sp", lambda: nc.sync.dma_start(out=pj[:, :, 576:640], in_=zb_d[:, 0:768].rearrange("p (a b) -> p a b", b=64)), writes=[pj])
                    for tt in range(4):
                        r0 = j * 512 + tt * 128
                        pqb = pqs[tt % 2]
                        zb = zs[tt % 2]
                        gt = gts[tt % 2]
                        gbo = gbs[tt % 2]
                        for half in range(2):
                            for k in range(8):
                                S.op("pe", lambda k=k: nc.tensor.matmul(pB[half][:], lhsT=hTb[:, k, tt * 128:(tt + 1) * 128],
                                                                        rhs=wpq[:, k, half * 512:(half + 1) * 512], start=(k == 0), stop=(k == 7)),
                                     reads=[hTb, wpq], writes=[pB[half]])
                        S.op("dve", lambda: nc.vector.tensor_copy(out=pqb[:, 0:512], in_=pB[0][:]), reads=[pB[0]], writes=[pqb])
                        S.op("act", lambda: nc.scalar.copy(out=pqb[:, 512:1024], in_=pB[1][:]), reads=[pB[1]], writes=[pqb])
                        S.dma("pool", lambda: nc.gpsimd.dma_start(out=pq_d[r0:r0 + 128, :], in_=pqb[:]), reads=[pqb])
                        for k in range(8):
                            S.op("pe", lambda k=k: nc.tensor.matmul(pZ[:], lhsT=hTb[:, k, tt * 128:(tt + 1) * 128], rhs=w_in[:, k, 1536:2048],
                                                                    start=(k == 0), stop=(k == 7)), reads=[hTb, w_in], writes=[pZ])
                        S.op("act", lambda: nc.scalar.activation(out=zb[:], in_=pZ[:], func=AF.Silu), reads=[pZ], writes=[zb])
                        S.dma("pool", lambda: nc.gpsimd.dma_start(out=z_d[r0:r0 + 128, :], in_=zb[:]), reads=[zb])
                        for k in range(8):
                            S.op("pe", lambda k=k: nc.tensor.matmul(pG[:, 0:16], lhsT=hTb[:, k, tt * 128:(tt + 1) * 128], rhs=w_in[:, k, 2048:2064],
                                                                    start=(k == 0), stop=(k == 7)), reads=[hTb, w_in], writes=[pG])
                        S.op("dve", lambda: nc.vector.tensor_copy(out=gt[:], in_=pG[:, 0:16]), reads=[pG], writes=[gt])
                        gates_math(gt, gbo)
                        S.dma("pool", lambda: nc.gpsimd.dma_start(out=gb_d[r0:r0 + 128, :], in_=gbo[:]), reads=[gbo])

                def conv(j):
                    pj = pre[j % 3]
                    pv = pj[:].rearrange("p c (r w) -> p c r w", w=64)
                    for ct in range(12):
                        ca = cacc[ct % 4]
                        cav = ca[:].rearrange("p (r w) -> p r w", w=64)
                        eng = "dve"
                        V = E[eng]
                        S.op(eng, lambda: V.tensor_scalar(out=cav, in0=pv[:, ct, 1:9, :], scalar1=convw[:, ct, 4:5], scalar2=None, op0=ALU.mult),
                             reads=[pj, convw], writes=[ca])
                        for dr in (-1, 0, 1):
                            for dc in (-1, 0, 1):
                                if dr == 0 and dc == 0:
                                    continue
                                tap = (dr + 1) * 3 + (dc + 1)
                                c0 = max(0, -dc)
                                c1 = 64 - max(0, dc)
                                S.op(eng, lambda: V.scalar_tensor_tensor(
                                    out=cav[:, :, c0:c1], in0=pv[:, ct, 1 + dr:9 + dr, c0 + dc:c1 + dc], scalar=convw[:, ct, tap:tap + 1],
                                    in1=cav[:, :, c0:c1], op0=ALU.mult, op1=ALU.add), reads=[pj, convw, ca], writes=[ca])
                        S.op("act", lambda: nc.scalar.activation(out=cvT[:, ct, :], in_=ca[:], func=AF.Silu), reads=[ca], writes=[cvT])
                    features(cvT, 512, qT_d, kT_d, k_d, v_d, j * 512)

                for j in range(nblk + 1):
                    if j < nblk:
                        inproj(j)
                    if j >= 1:
                        conv(j - 1)
                S.new_epoch()
            return False

        if 1 in run:
            if stage1():
                return nc, S

        def stage2():
            st2 = contextlib.ExitStack()
            with st2:
                sb = lambda name, shape, dt: mk(st2, "sb", name, shape, dt)
                ps = lambda name, shape, dt: mk(st2, "ps", name, shape, dt)
                QS = 128.0 ** -0.5
                mS = [sb("mS%d" % d, [128, 512], F32) for d in range(2)]
                mI = [sb("mI%d" % d, [128, 512], F32) for d in range(2)]
                Lm = [sb("Lm%d" % d, [128, 128], F32) for d in range(2)]
                Um = [sb("Um%d" % d, [128, 128], F32) for d in range(2)]
                I4 = sb("I4", [128, 512], BF16)
                for d in range(2):
                    for tb, dd in ((mS[d], mS_d[d]), (mI[d], mI_d[d]), (Lm[d], Lm_d[d]), (Um[d], Um_d[d])):
                        S.dma("sp", lambda tb=tb, dd=dd: nc.sync.dma_start(out=tb[:], in_=dd), writes=[tb])
                S.dma("sp", lambda: nc.sync.dma_start(out=I4[:], in_=I4_d), writes=[I4])
                I4f = sb("I4f", [128, 512], F32)
                bdm = sb("bdm", [128, 512], F32)
                cm = [sb("cm%d" % i, [128, 512], F32) for i in range(4)]
                S.dma("sp", lambda: nc.sync.dma_start(out=I4f[:], in_=I4f_d), writes=[I4f])
                S.dma("sp", lambda: nc.sync.dma_start(out=bdm[:], in_=bdm_d), writes=[bdm])
                for i in range(4):
                    S.dma("sp", lambda i=i: nc.sync.dma_start(out=cm[i][:], in_=cm_d[i]), writes=[cm[i]])
                Sst = [sb("Sst%d" % d, [128, 4, 128], F32) for d in range(2)]
                Sbf = [sb("Sbf%d" % d, [128, 4, 128], BF16) for d in range(2)]
                for d in range(2):
                    S.dma("sp", lambda d=d: nc.sync.dma_start(out=Sst[d][:].rearrange("p a b -> p (a b)"), in_=zf_d), writes=[Sst[d]])
                    S.dma("sp", lambda d=d: nc.sync.dma_start(out=Sbf[d][:].rearrange("p a b -> p (a b)"), in_=zb_d[:, 0:512]), writes=[Sbf[d]])

                class UB:
                    pass
                ubs = []
                for u in range(2):
                    b = UB()
                    n_ = lambda nm: "%s_%d" % (nm, u)
                    b.qT = sb(n_("qT"), [128, 4, 128], BF16); b.kT = sb(n_("kT"), [128, 4, 128], BF16)
                    b.kt = sb(n_("kt"), [128, 512], BF16); b.vt = sb(n_("vt"), [128, 512], BF16)
                    b.gb = sb(n_("gb"), [128, 16], F32)
                    b.Lg = sb(n_("Lg"), [128, 4, 128], F32)
                    b.E1 = sb(n_("E1"), [128, 512], F32); b.E1s = sb(n_("E1s"), [128, 512], F32); b.E1i = sb(n_("E1i"), [128, 512], F32)
                    b.sc = sb(n_("sc"), [128, 40], F32)
                    b.XA = sb(n_("XA"), [128, 8, 128], BF16); b.YA = sb(n_("YA"), [128, 8, 128], BF16)
                    b.XF = [sb(n_("XF%d" % i), [128, 4, 128], F32) for i in range(2)]
                    b.YF = [sb(n_("YF%d" % i), [128, 4, 128], F32) for i in range(2)]
                    b.IYF = sb(n_("IYF"), [128, 4, 128], F32)
                    b.TnF = [sb(n_("TnF%d" % i), [128, 4, 128], F32) for i in range(2)]
                    b.TtF = [sb(n_("TtF%d" % i), [128, 4, 128], F32) for i in range(2)]
                    b.E1sB = sb(n_("E1sB"), [128, 512], F32)
                    b.G = [sb(n_("G%d" % i), [128, 4, 128], BF16) for i in range(4)]
                    b.Bt = sb(n_("Bt"), [128, 4, 256], BF16); b.Xs = sb(n_("Xs"), [128, 4, 256], BF16); b.Vt = sb(n_("Vt"), [128, 4, 256], BF16)
                    b.kd = sb(n_("kd"), [128, 4, 128], BF16)
                    b.wTn = sb(n_("wTn"), [128, 4, 128], BF16); b.vn = sb(n_("vn"), [128, 4, 128], BF16)
                    b.AVs = sb(n_("AVs"), [128, 4, 128], F32); b.o = sb(n_("o"), [128, 4, 128], F32)
                    ubs.append(b)
                F = [ps("F%d" % i, [128, 4, 128], F32) for i in range(7)]
                TPb = ps("TPb", [128, 8, 128], BF16)
                cnt2 = {"u": 0}

                def unit(n, d, srcs, want_o, o_dst):
                    qTd, kTd, kd_, vd_, gbd = srcs
                    b = ubs[cnt2["u"] % 2]
                    cnt2["u"] += 1
                    t0 = n * 128
                    Sd, Sb_ = Sst[d], Sbf[d]
                    S.dma("sp", lambda: nc.sync.dma_start(out=b.qT[:], in_=qTd[:, :, t0:t0 + 128].rearrange("h p t -> p h t")), writes=[b.qT])
                    S.dma("sp", lambda: nc.sync.dma_start(out=b.kT[:], in_=kTd[:, :, t0:t0 + 128].rearrange("h p t -> p h t")), writes=[b.kT])
                    S.dma("sp", lambda: nc.sync.dma_start(out=b.kt[:], in_=kd_[t0:t0 + 128, :]), writes=[b.kt])
                    S.dma("sp", lambda: nc.sync.dma_start(out=b.vt[:], in_=vd_[t0:t0 + 128, :]), writes=[b.vt])
                    S.dma("sp", lambda: nc.sync.dma_start(out=b.gb[:], in_=gbd[t0:t0 + 128, :]), writes=[b.gb])
                    g0 = 8 + 4 * d
                    b0 = 4 * d
                    sc = b.sc
                    for h in range(4):
                        S.op("dve", lambda h=h: nc.vector.tensor_scalar(out=b.Lg[:, h, :], in0=Lm[d][:], scalar1=b.gb[:, g0 + h:g0 + h + 1], scalar2=None, op0=ALU.mult),
                             reads=[Lm[d], b.gb], writes=[b.Lg])
                    for h in range(4):
                        S.op("pe", lambda h=h: nc.tensor.matmul(F[0][:, h, :], lhsT=b.Lg[:, h, :], rhs=Um[d][:], start=True, stop=True),
                             reads=[b.Lg, Um[d]], writes=[F[0]])
                    S.op("pe", lambda: nc.tensor.matmul(F[1][:, 0, 0:4], lhsT=Lm[d][:], rhs=b.gb[:, g0:g0 + 4], start=True, stop=True),
                         reads=[Lm[d], b.gb], writes=[F[1]])
                    S.op("pe", lambda: nc.tensor.matmul(F[1][:, 0, 4:8], lhsT=ones_f[:], rhs=b.gb[:, g0:g0 + 4], start=True, stop=True),
                         reads=[ones_f, b.gb], writes=[F[1]])
                    S.op("act", lambda: nc.scalar.activation(out=b.E1[:], in_=F[0][:].rearrange("p a b -> p (a b)"), func=AF.Exp), reads=[F[0]], writes=[b.E1])
                    S.op("dve", lambda: nc.vector.tensor_copy(out=sc[:, 0:8], in_=F[1][:, 0, 0:8]), reads=[F[1]], writes=[sc])
                    S.op("act", lambda: nc.scalar.activation(out=sc[:, 8:12], in_=sc[:, 0:4], func=AF.Exp), reads=[sc], writes=[sc])
                    S.op("act", lambda: nc.scalar.activation(out=sc[:, 28:32], in_=sc[:, 4:8], func=AF.Exp), reads=[sc], writes=[sc])
                    S.op("dve", lambda: nc.vector.tensor_scalar(out=sc[:, 12:16], in0=sc[:, 8:12], scalar1=QS, scalar2=None, op0=ALU.mult), reads=[sc], writes=[sc])
                    S.op("dve", lambda: nc.vector.tensor_tensor(out=sc[:, 16:20], in0=sc[:, 8:12], in1=b.gb[:, b0:b0 + 4], op=ALU.mult), reads=[sc, b.gb], writes=[sc])
                    S.op("dve", lambda: nc.vector.tensor_scalar(out=sc[:, 20:24], in0=b.gb[:, b0:b0 + 4], scalar1=-1.0, scalar2=None, op0=ALU.mult), reads=[b.gb], writes=[sc])
                    S.op("dve", lambda: nc.vector.tensor_tensor(out=sc[:, 32:36], in0=sc[:, 4:8], in1=sc[:, 0:4], op=ALU.subtract), reads=[sc], writes=[sc])
                    S.op("act", lambda: nc.scalar.activation(out=sc[:, 24:28], in_=sc[:, 32:36], func=AF.Exp), reads=[sc], writes=[sc])
                    S.op("dve", lambda: nc.vector.tensor_tensor(out=b.E1s[:], in0=b.E1[:], in1=mS[d][:], op=ALU.mult), reads=[b.E1, mS[d]], writes=[b.E1s])
                    S.op("dve", lambda: nc.vector.tensor_tensor(out=b.E1i[:], in0=b.E1[:], in1=mI[d][:], op=ALU.mult), reads=[b.E1, mI[d]], writes=[b.E1i])
                    for h in range(4):
                        S.op("pe", lambda h=h: nc.tensor.matmul(F[2][:, h, :], lhsT=b.kT[:, h, :], rhs=b.kT[:, h, :], start=True, stop=True),
                             reads=[b.kT], writes=[F[2]])
                        S.op("pe", lambda h=h: nc.tensor.matmul(F[3][:, h, :], lhsT=b.qT[:, h, :], rhs=b.kT[:, h, :], start=True, stop=True),
                             reads=[b.qT, b.kT], writes=[F[3]])
                    for h in range(4):
                        S.op("dve", lambda h=h: nc.vector.scalar_tensor_tensor(out=b.XA[:, h, :], in0=F[2][:, h, :], scalar=sc[:, 20 + h:21 + h],
                                                                                in1=b.E1s[:, h * 128:(h + 1) * 128], op0=ALU.mult, op1=ALU.mult),
                             reads=[F[2], sc, b.E1s], writes=[b.XA])
                    S.op("dve", lambda: nc.vector.scalar_tensor_tensor(out=b.XA[:, 4:8, :].rearrange("p a b -> p (a b)"), in0=F[3][:].rearrange("p a b -> p (a b)"),
                                                                        scalar=QS, in1=b.E1i[:], op0=ALU.mult, op1=ALU.mult),
                         reads=[F[3], b.E1i], writes=[b.XA])
                    for i in range(8):
                        S.op("pe", lambda i=i: nc.tensor.transpose(TPb[:, i, :], b.XA[:, i, :], ident_bf[:]), reads=[b.XA, ident_bf], writes=[TPb])
                    S.op("dve", lambda: nc.vector.tensor_copy(out=b.YA[:], in_=TPb[:]), reads=[TPb], writes=[b.YA])
                    i4f = I4f[:].rearrange("p (a b) -> p a b", b=128)
                    S.op("dve", lambda: nc.vector.tensor_tensor(out=b.E1sB[:], in0=b.E1s[:], in1=bdm[:], op=ALU.mult), reads=[b.E1s, bdm], writes=[b.E1sB])
                    for h in range(4):
                        S.op("dve", lambda h=h: nc.vector.scalar_tensor_tensor(out=b.XF[0][:, h, :], in0=F[2][:, h, :], scalar=sc[:, 20 + h:21 + h],
                                                                                in1=b.E1sB[:, h * 128:(h + 1) * 128], op0=ALU.mult, op1=ALU.mult),
                             reads=[F[2], sc, b.E1sB], writes=[b.XF[0]])
                    for h in range(4):
                        S.op("pe", lambda h=h: nc.tensor.transpose(F[4][:, h, :], b.XF[0][:, h, :], ident_f[:]), reads=[b.XF[0], ident_f], writes=[F[4]])
                    S.op("dve", lambda: nc.vector.tensor_copy(out=b.YF[0][:], in_=F[4][:]), reads=[F[4]], writes=[b.YF[0]])
                    S.op("dve", lambda: nc.vector.tensor_tensor(out=b.TtF[0][:], in0=b.YF[0][:], in1=i4f, op=ALU.add), reads=[b.YF[0], I4f], writes=[b.TtF[0]])
                    S.op("dve", lambda: nc.vector.tensor_tensor(out=b.TnF[0][:], in0=b.XF[0][:], in1=i4f, op=ALU.add), reads=[b.XF[0], I4f], writes=[b.TnF[0]])
                    Xc, Yc = b.XF[0], b.YF[0]
                    cur = 0
                    for lev in range(1, 5):
                        last = (lev == 4)
                        Xn, Yn = b.XF[lev % 2], b.YF[lev % 2]
                        for h in range(4):
                            S.op("pe", lambda h=h: nc.tensor.matmul(F[2][:, h, :], lhsT=Xc[:, h, :], rhs=Yc[:, h, :], start=True, stop=True),
                                 reads=[Xc, Yc], writes=[F[2]])
                        if not last:
                            for h in range(4):
                                S.op("pe", lambda h=h: nc.tensor.matmul(F[3][:, h, :], lhsT=Yc[:, h, :], rhs=Xc[:, h, :], start=True, stop=True),
                                     reads=[Xc, Yc], writes=[F[3]])
                        S.op("dve", lambda: nc.vector.tensor_tensor(out=b.IYF[:], in0=F[2][:], in1=i4f, op=ALU.add), reads=[F[2], I4f], writes=[b.IYF])
                        if not last:
                            S.op("act", lambda: nc.scalar.copy(out=Yn[:], in_=F[2][:]), reads=[F[2]], writes=[Yn])
                            S.op("act", lambda: nc.scalar.copy(out=Xn[:], in_=F[3][:]), reads=[F[3]], writes=[Xn])
                        Tn_o, Tt_n, Tn_n = b.TnF[cur], b.TtF[1 - cur], b.TnF[1 - cur]
                        for h in range(4):
                            S.op("pe", lambda h=h: nc.tensor.matmul(F[4][:, h, :], lhsT=Tn_o[:, h, :], rhs=b.IYF[:, h, :], start=True, stop=True),
                                 reads=[Tn_o, b.IYF], writes=[F[4]])
                        if not last:
                            for h in range(4):
                                S.op("pe", lambda h=h: nc.tensor.matmul(F[5][:, h, :], lhsT=b.IYF[:, h, :], rhs=Tn_o[:, h, :], start=True, stop=True),
                                     reads=[Tn_o, b.IYF], writes=[F[5]])
                        S.op("dve", lambda: nc.vector.tensor_copy(out=Tt_n[:], in_=F[4][:]), reads=[F[4]], writes=[Tt_n])
                        if not last:
                            S.op("act", lambda: nc.scalar.copy(out=Tn_n[:], in_=F[5][:]), reads=[F[5]], writes=[Tn_n])
                        cur = 1 - cur
                        Xc, Yc = Xn, Yn
                    DgT = b.TtF[cur]
                    for I_ in range(4):
                        S.op("dve", lambda I_=I_: nc.vector.tensor_tensor(out=b.G[I_][:].rearrange("p a b -> p (a b)"), in0=DgT[:].rearrange("p a b -> p (a b)"),
                                                                          in1=cm[I_][:], op=ALU.mult), reads=[DgT, cm[I_]], writes=[b.G[I_]])
                    for h in range(4):
                        S.op("dve", lambda h=h: nc.vector.tensor_scalar(out=b.Bt[:, h, 0:128], in0=b.vt[:, h * 128:(h + 1) * 128], scalar1=b.gb[:, b0 + h:b0 + h + 1], scalar2=None, op0=ALU.mult),
                             reads=[b.vt, b.gb], writes=[b.Bt])
                        S.op("act", lambda h=h: nc.scalar.activation(out=b.Bt[:, h, 128:256], in_=b.kt[:, h * 128:(h + 1) * 128], func=AF.Copy, scale=sc[:, 16 + h:17 + h]),
                             reads=[b.kt, sc], writes=[b.Bt])
                        S.op("act", lambda h=h: nc.scalar.activation(out=b.kd[:, h, :], in_=b.kt[:, h * 128:(h + 1) * 128], func=AF.Copy, scale=sc[:, 24 + h:25 + h]),
                             reads=[b.kt, sc], writes=[b.kd])

                    def pv(h):
                        return F[2 + h // 2][:].rearrange("p a b -> p (a b)")[:, (h % 2) * 256:(h % 2 + 1) * 256]

                    def px(h):
                        return F[4 + h // 2][:].rearrange("p a b -> p (a b)")[:, (h % 2) * 256:(h % 2 + 1) * 256]

                    def fl(t_, lo, hi):
                        return t_[:, lo:hi, :].rearrange("p a b -> p (a b)")

                    for step_, I_ in enumerate((0, 1, 2, 3) if d == 0 else (3, 2, 1, 0)):
                        if step_ == 0:
                            rhs_t = b.Bt
                        else:
                            for h in range(4):
                                S.op("pe", lambda h=h: nc.tensor.matmul(pv(h), lhsT=b.YA[:, h, :], rhs=b.Xs[:, h, :], start=True, stop=True),
                                     reads=[b.YA, b.Xs], writes=[F[2 + h // 2]])
                            for q_ in range(2):
                                S.op("dve", lambda q_=q_: nc.vector.tensor_tensor(out=fl(b.Vt, 2 * q_, 2 * q_ + 2), in0=F[2 + q_][:].rearrange("p a b -> p (a b)"),
                                                                                  in1=fl(b.Bt, 2 * q_, 2 * q_ + 2), op=ALU.add), reads=[F[2 + q_], b.Bt], writes=[b.Vt])
                            rhs_t = b.Vt
                        for h in range(4):
                            S.op("pe", lambda h=h: nc.tensor.matmul(px(h), lhsT=b.G[I_][:, h, :], rhs=rhs_t[:, h, :], start=True, stop=True),
                                 reads=[b.G[I_], rhs_t], writes=[F[4 + h // 2]])
                        for q_ in range(2):
                            if step_ == 0:
                                S.op("dve", lambda q_=q_: nc.vector.tensor_copy(out=fl(b.Xs, 2 * q_, 2 * q_ + 2), in_=F[4 + q_][:].rearrange("p a b -> p (a b)")),
                                     reads=[F[4 + q_]], writes=[b.Xs])
                            else:
                                S.op("dve", lambda q_=q_: nc.vector.tensor_tensor(out=fl(b.Xs, 2 * q_, 2 * q_ + 2), in0=F[4 + q_][:].rearrange("p a b -> p (a b)"),
                                                                                  in1=fl(b.Xs, 2 * q_, 2 * q_ + 2), op=ALU.add), reads=[F[4 + q_], b.Xs], writes=[b.Xs])
                    for h in range(4):
                        S.op("pe", lambda h=h: nc.tensor.transpose(TPb[:, h, :], b.Xs[:, h, 128:256], ident_bf[:]), reads=[b.Xs, ident_bf], writes=[TPb])
                    S.op("dve", lambda: nc.vector.tensor_scalar(out=b.wTn[:], in0=TPb[:, 0:4, :], scalar1=-1.0, scalar2=None, op0=ALU.mult), reads=[TPb], writes=[b.wTn])
                    for h in range(4):
                        S.op("pe", lambda h=h: nc.tensor.matmul(F[6][:, h, :], lhsT=b.wTn[:, h, :], rhs=Sb_[:, h, :], start=True, stop=True),
                             reads=[b.wTn, Sb_], writes=[F[6]])
                    S.op("dve", lambda: nc.vector.tensor_tensor(out=b.vn[:], in0=F[6][:], in1=b.Xs[:, :, 0:128], op=ALU.add), reads=[F[6], b.Xs], writes=[b.vn])
                    if want_o:
                        for h in range(4):
                            S.op("pe", lambda h=h: nc.tensor.matmul(F[4][:, h, :], lhsT=b.qT[:, h, :], rhs=Sb_[:, h, :], start=True, stop=True),
                                 reads=[b.qT, Sb_], writes=[F[4]])
                            S.op("pe", lambda h=h: nc.tensor.matmul(F[5][:, h, :], lhsT=b.YA[:, 4 + h, :], rhs=b.vn[:, h, :], start=True, stop=True),
                                 reads=[b.YA, b.vn], writes=[F[5]])
                        S.op("act", lambda: nc.scalar.copy(out=b.AVs[:], in_=F[5][:]), reads=[F[5]], writes=[b.AVs])
                        for h in range(4):
                            S.op("dve", lambda h=h: nc.vector.scalar_tensor_tensor(out=b.o[:, h, :], in0=F[4][:, h, :], scalar=sc[:, 12 + h:13 + h], in1=b.AVs[:, h, :],
                                                                                    op0=ALU.mult, op1=ALU.add), reads=[F[4], sc, b.AVs], writes=[b.o])
                        S.dma("pool", lambda: nc.gpsimd.dma_start(out=o_dst[t0:t0 + 128, :], in_=b.o[:].rearrange("p a b -> p (a b)")), reads=[b.o])
                    for h in range(4):
                        S.op("pe", lambda h=h: nc.tensor.matmul(F[1][:, h, :], lhsT=b.kd[:, h, :], rhs=b.vn[:, h, :], start=True, stop=True),
                             reads=[b.kd, b.vn], writes=[F[1]])
                    for h in range(4):
                        S.op("dve", lambda h=h: nc.vector.scalar_tensor_tensor(out=Sd[:, h, :], in0=Sd[:, h, :], scalar=sc[:, 28 + h:29 + h], in1=F[1][:, h, :],
                                                                                op0=ALU.mult, op1=ALU.add), reads=[Sd, sc, F[1]], writes=[Sd])
                    S.op("act", lambda: nc.scalar.copy(out=Sb_[:], in_=Sd[:]), reads=[Sd], writes=[Sb_])

                csrc = (cqT_d, ckT_d, ck_d, cv_d, cgb_d)
                msrc = (qT_d, kT_d, k_d, v_d, gb_d)
                if ulimit is not None:
                    for (n_, d_) in ulimit:
                        unit(n_, d_, csrc, False, None)
                    S.barrier()
                    return True
                for n in range(2):
                    unit(n, 0, csrc, False, None)
                    unit(1 - n, 1, csrc, False, None)
                if dbg:
                    for d in range(2):
                        S.dma("pool", lambda d=d: nc.gpsimd.dma_start(out=s0_d[d], in_=Sst[d][:]), reads=[Sst[d]])
                for s_ in range(nch):
                    unit(s_, 0, msrc, True, of_d)
                    unit(nch - 1 - s_, 1, msrc, True, ob_d)
                    if s_ in (21, 43):
                        S.new_epoch()
                S.new_epoch()
            return False

        if 2 in run:
            if stage2():
                return nc, S

        def stage3():
            st3 = contextlib.ExitStack()
            with st3:
                sb = lambda name, shape, dt: mk(st3, "sb", name, shape, dt)
                ps = lambda name, shape, dt: mk(st3, "ps", name, shape, dt)
                cosA = sb("cosA", [128, 64, 128], BF16)
                sinA = sb("sinA", [128, 64, 128], BF16)
                nsinA = sb("nsinA", [128, 64, 128], BF16)
                cs64 = sb("cs64", [128, 64], BF16)
                S.dma("sp", lambda: nc.sync.dma_start(out=cosA[:], in_=cosA_d), writes=[cosA])
                S.dma("sp", lambda: nc.sync.dma_start(out=sinA[:], in_=sinA_d), writes=[sinA])
                S.dma("sp", lambda: nc.sync.dma_start(out=nsinA[:], in_=nsinA_d), writes=[nsinA])
                S.dma("sp", lambda: nc.sync.dma_start(out=cs64[:], in_=cs64_d), writes=[cs64])
                fm = sb("fm", [128, 4, T], BF16)
                pqa = [sb("pqa%d" % i, [128, 1024], BF16) for i in range(2)]
                zs = [sb("zs3_%d" % i, [128, 2, 512], BF16) for i in range(2)]
                zin = [sb("zin%d" % i, [128, 16, 512], BF16) for i in range(2)]
                P = [ps("P3_%d" % i, [128, 512], F32) for i in range(4)]
                pq_v = pq_d.rearrange("(b a) c -> a b c", a=64)
                for a in range(64):
                    pb = pqa[a % 2]
                    zb = zs[a % 2]
                    S.dma("sp", lambda: nc.sync.dma_start(out=pb[:], in_=pq_v[a]), writes=[pb])
                    pr, pi = P[(a % 2) * 2], P[(a % 2) * 2 + 1]
                    S.op("pe", lambda: nc.tensor.matmul(pr[:], lhsT=cosA[:, a, :], rhs=pb[:, 0:512], start=True, stop=False), reads=[cosA, pb], writes=[pr])
                    S.op("pe", lambda: nc.tensor.matmul(pr[:], lhsT=nsinA[:, a, :], rhs=pb[:, 512:1024], start=False, stop=True), reads=[nsinA, pb], writes=[pr])
                    S.op("pe", lambda: nc.tensor.matmul(pi[:], lhsT=cosA[:, a, :], rhs=pb[:, 512:1024], start=True, stop=False), reads=[cosA, pb], writes=[pi])
                    S.op("pe", lambda: nc.tensor.matmul(pi[:], lhsT=sinA[:, a, :], rhs=pb[:, 0:512], start=False, stop=True), reads=[sinA, pb], writes=[pi])
                    S.op("dve", lambda: nc.vector.tensor_copy(out=zb[:, 0, :], in_=pr[:]), reads=[pr], writes=[zb])
                    S.op("act", lambda: nc.scalar.copy(out=zb[:, 1, :], in_=pi[:]), reads=[pi], writes=[zb])
                    S.dma("pool", lambda: nc.gpsimd.dma_start(out=Zd[:, a, :, :].rearrange("r p c -> p r c"), in_=zb[:]), reads=[zb])
                S.barrier()
                zv = Zd.rearrange("r a b c -> (r a) b c")
                for g in range(8):
                    zi = zin[g % 2]
                    S.dma("sp", lambda: nc.sync.dma_start(out=zi[:], in_=zv[:, g * 16:(g + 1) * 16, :]), writes=[zi])
                    for half in range(2):
                        for ct in range(4):
                            pp = P[(half * 4 + ct) % 4]
                            ppv = pp[:].rearrange("p (b a) -> p b a", a=64)
                            for bl in range(8):
                                S.op("pe", lambda bl=bl: nc.tensor.matmul(ppv[:, bl, :], lhsT=zi[:, half * 8 + bl, ct * 128:(ct + 1) * 128], rhs=cs64[:],
                                                                          start=True, stop=True), reads=[zi, cs64], writes=[pp])
                            b0 = g * 16 + half * 8
                            ov = fm[:, ct, :].rearrange("p (a b) -> p b a", b=128)[:, b0:b0 + 8, :]
                            if ct % 2 == 0:
                                S.op("dve", lambda: nc.vector.tensor_scalar(out=ov, in0=ppv, scalar1=1.0 / 1024.0, scalar2=None, op0=ALU.mult), reads=[pp], writes=[fm])
                            else:
                                S.op("act", lambda: nc.scalar.activation(out=ov, in_=ppv, func=AF.Copy, scale=1.0 / 1024.0), reads=[pp], writes=[fm])
                for ct in range(4):
                    S.dma("pool", lambda ct=ct: nc.gpsimd.dma_start(out=fmT_d[ct], in_=fm[:, ct, :]), reads=[fm])
                S.barrier()
            return False

        if 3 in run:
            if stage3():
                return nc, S

        def stage4():
            st4 = contextlib.ExitStack()
            with st4:
                sb = lambda name, shape, dt: mk(st4, "sb", name, shape, dt)
                ps = lambda name, shape, dt: mk(st4, "ps", name, shape, dt)
                comb = sb("comb", [128, NT4, 32], F32)
                gate2b = sb("gate2b", [128, D], F32)
                fingb = sb("fingb", [128, D], F32)
                S.dma("sp", lambda: nc.sync.dma_start(out=gate2b[:], in_=modrow_d[0:1, 5120:6144].partition_broadcast(128)), writes=[gate2b])
                S.dma("sp", lambda: nc.sync.dma_start(out=fingb[:], in_=fing_d.partition_broadcast(128)), writes=[fingb])
                junk4 = sb("junk4", [128, D], BF16)
                PS = [ps("P4_%d" % i, [128, 512], F32) for i in range(7)]
                PTb = ps("PTb4", [128, 8, 128], BF16)
                sa4 = contextlib.ExitStack()
                sa4.__enter__()
                sA = lambda name, shape, dt: mk(sa4, "sb", name, shape, dt)
                gate1b = sA("gate1b", [128, D], F32); gs2b = sA("gs2b", [128, D], F32); sh2b = sA("sh2b", [128, D], F32)
                n2gb = sA("n2gb", [128, D], F32)
                S.dma("sp", lambda: nc.sync.dma_start(out=gate1b[:], in_=modrow_d[0:1, 2048:3072].partition_broadcast(128)), writes=[gate1b])
                S.dma("sp", lambda: nc.sync.dma_start(out=sh2b[:], in_=modrow_d[0:1, 3072:4096].partition_broadcast(128)), writes=[sh2b])
                S.dma("sp", lambda: nc.sync.dma_start(out=gs2b[:], in_=modrow_d[0:1, 4096:5120].partition_broadcast(128)), writes=[gs2b])
                S.dma("sp", lambda: nc.sync.dma_start(out=n2gb[:], in_=n2g_d.partition_broadcast(128)), writes=[n2gb])
                S.op("dve", lambda: nc.vector.scalar_tensor_tensor(out=gs2b[:], in0=gs2b[:], scalar=1.0, in1=n2gb[:], op0=ALU.add, op1=ALU.mult),
                     reads=[gs2b, n2gb], writes=[gs2b])
                og4 = sA("og4", [128, 512], F32)
                for h in range(4):
                    S.dma("sp", lambda h=h: nc.sync.dma_start(out=og4[:, h * 128:(h + 1) * 128], in_=onorm_d.partition_broadcast(128)), writes=[og4])
                brb = sA("brb", [128, 36], F32)
                S.dma("sp", lambda: nc.sync.dma_start(out=brb[:, 0:4], in_=bgrp_d.partition_broadcast(128)), writes=[brb])
                S.dma("sp", lambda: nc.sync.dma_start(out=brb[:, 4:36], in_=brt_d.partition_broadcast(128)), writes=[brb])
                wrg = sA("wrg", [128, 8, 36], F32)
                with nc.allow_non_contiguous_dma(reason="tiny router weights"):
                    S.dma("sp", lambda: nc.sync.dma_start(out=wrg[:, :, 0:4], in_=wgrp_d.rearrange("(k p) n -> p k n", p=128)), writes=[wrg])
                    S.dma("sp", lambda: nc.sync.dma_start(out=wrg[:, :, 4:36], in_=wrt_d.rearrange("(k p) n -> p k n", p=128)), writes=[wrg])
                w_out = sA("w_out", [128, 8, D], BF16)
                S.dma("pool", lambda: nc.gpsimd.dma_start(out=w_out[:], in_=w_out_d.rearrange("(k p) n -> p k n", p=128)), writes=[w_out])
                mixT = [sA("mixT%d" % i, [128, 8, 512], BF16) for i in range(2)]
                oft = [sA("oft%d" % i, [128, 512], F32) for i in range(2)]
                obt = [sA("obt%d" % i, [128, 512], F32) for i in range(2)]
                zt = [sA("zt%d" % i, [128, 512], BF16) for i in range(2)]
                gz = sA("gz", [128, 512], F32)
                ogb = sA("ogb", [128, 512], BF16)
                s4 = [sA("s4_%d" % i, [128, 48], F32) for i in range(2)]
                xt4 = [sA("xt4_%d" % i, [128, D], F32) for i in range(2)]
                x1t = [sA("x1t%d" % i, [128, D], F32) for i in range(2)]
                h2t = [sA("h2t%d" % i, [128, D], F32) for i in range(2)]
                h2T32 = sA("h2T32", [128, 8, 128], F32)
                h2Tb = [sA("h2Tb%d" % i, [128, 8, 128], BF16) for i in range(2)]
                lg = [sA("lg%d" % i, [128, 36], F32) for i in range(2)]
                lm = sA("lm", [128, 32], F32); lm2 = sA("lm2", [128, 32], F32)
                sel1 = sA("sel1", [128, 32], F32); sel2 = sA("sel2", [128, 32], F32)
                for j in range(NT4 // 4):
                    mT = mixT[j % 2]
                    for ct in range(4):
                        S.dma("sp", lambda ct=ct: nc.sync.dma_start(out=mT[:, ct, :], in_=fmT_d[ct, :, j * 512:(j + 1) * 512]), writes=[mT])
                    for tt in range(4):
                        ti = j * 4 + tt
                        r0 = ti * 128
                        a_, b_, z_, sc_ = oft[ti % 2], obt[ti % 2], zt[ti % 2], s4[ti % 2]
                        S.dma("sp", lambda: nc.sync.dma_start(out=a_[:], in_=of_d[r0:r0 + 128, :]), writes=[a_])
                        S.dma("sp", lambda: nc.sync.dma_start(out=b_[:], in_=ob_d[r0:r0 + 128, :]), writes=[b_])
                        S.dma("sp", lambda: nc.sync.dma_start(out=z_[:], in_=z_d[r0:r0 + 128, :]), writes=[z_])
                        S.op("dve", lambda: nc.vector.tensor_tensor(out=a_[:], in0=a_[:], in1=b_[:], op=ALU.add), reads=[a_, b_], writes=[a_])
                        for h in range(4):
                            S.op("act", lambda h=h: nc.scalar.activation(out=junk4[:, 0:128], in_=a_[:, h * 128:(h + 1) * 128], func=AF.Square, accum_out=sc_[:, h:h + 1]),
                                 reads=[a_], writes=[junk4, sc_])
                        S.op("dve", lambda: nc.vector.tensor_scalar(out=sc_[:, 0:4], in0=sc_[:, 0:4], scalar1=1.0 / 128, scalar2=EPS, op0=ALU.mult, op1=ALU.add), reads=[sc_], writes=[sc_])
                        S.op("act", lambda: nc.scalar.sqrt(out=sc_[:, 0:4], in_=sc_[:, 0:4]), reads=[sc_], writes=[sc_])
                        S.op("dve", lambda: nc.vector.reciprocal(out=sc_[:, 0:4], in_=sc_[:, 0:4]), reads=[sc_], writes=[sc_])
                        S.op("dve", lambda: nc.vector.tensor_tensor(out=gz[:], in0=z_[:], in1=og4[:], op=ALU.mult), reads=[z_, og4], writes=[gz])
                        for h in range(4):
                            S.op("dve", lambda h=h: nc.vector.scalar_tensor_tensor(out=ogb[:, h * 128:(h + 1) * 128], in0=a_[:, h * 128:(h + 1) * 128], scalar=sc_[:, h:h + 1],
                                                                                    in1=gz[:, h * 128:(h + 1) * 128], op0=ALU.mult, op1=ALU.mult), reads=[a_, sc_, gz], writes=[ogb])
                        for h in range(4):
                            S.op("pe", lambda h=h: nc.tensor.transpose(PTb[:, h, :], ogb[:, h * 128:(h + 1) * 128], ident_bf[:]), reads=[ogb, ident_bf], writes=[PTb])
                        S.op("dve", lambda: nc.vector.tensor_copy(out=mT[:, 4:8, tt * 128:(tt + 1) * 128], in_=PTb[:, 0:4, :]), reads=[PTb], writes=[mT])
                    for tt in range(4):
                        ti = j * 4 + tt
                        r0 = ti * 128
                        xb, x1b, h2b, sc_ = xt4[ti % 2], x1t[ti % 2], h2t[ti % 2], s4[ti % 2]
                        S.dma("sp", lambda: nc.sync.dma_start(out=xb[:], in_=x_d[r0:r0 + 128, :]), writes=[xb])
                        for half in range(2):
                            pp = PS[half]
                            for k in range(8):
                                S.op("pe", lambda k=k: nc.tensor.matmul(pp[:], lhsT=mT[:, k, tt * 128:(tt + 1) * 128], rhs=w_out[:, k, half * 512:(half + 1) * 512],
                                                                        start=(k == 0), stop=(k == 7)), reads=[mT, w_out], writes=[pp])
                            hs = slice(half * 512, (half + 1) * 512)
                            S.op("dve", lambda: nc.vector.tensor_tensor(out=x1b[:, hs], in0=pp[:], in1=gate1b[:, hs], op=ALU.mult), reads=[pp, gate1b], writes=[x1b])
                        S.op("dve", lambda: nc.vector.tensor_tensor(out=x1b[:], in0=x1b[:], in1=xb[:], op=ALU.add), reads=[x1b, xb], writes=[x1b])
                        S.dma("pool", lambda: nc.gpsimd.dma_start(out=x1_d[r0:r0 + 128, :], in_=x1b[:]), reads=[x1b])
                        S.op("act", lambda: nc.scalar.activation(out=junk4[:], in_=x1b[:], func=AF.Square, accum_out=sc_[:, 8:9]), reads=[x1b], writes=[junk4, sc_])
                        S.op("dve", lambda: nc.vector.tensor_scalar(out=sc_[:, 8:9], in0=sc_[:, 8:9], scalar1=1.0 / D, scalar2=EPS, op0=ALU.mult, op1=ALU.add), reads=[sc_], writes=[sc_])
                        S.op("act", lambda: nc.scalar.sqrt(out=sc_[:, 8:9], in_=sc_[:, 8:9]), reads=[sc_], writes=[sc_])
                        S.op("dve", lambda: nc.vector.reciprocal(out=sc_[:, 8:9], in_=sc_[:, 8:9]), reads=[sc_], writes=[sc_])
                        S.op("dve", lambda: nc.vector.scalar_tensor_tensor(out=h2b[:], in0=x1b[:], scalar=sc_[:, 8:9], in1=gs2b[:], op0=ALU.mult, op1=ALU.mult),
                             reads=[x1b, sc_, gs2b], writes=[h2b])
                        S.op("dve", lambda: nc.vector.tensor_tensor(out=h2b[:], in0=h2b[:], in1=sh2b[:], op=ALU.add), reads=[h2b, sh2b], writes=[h2b])
                        for k in range(8):
                            pp = PS[2 + k // 4]
                            S.op("pe", lambda k=k, pp=pp: nc.tensor.transpose(pp[:, (k % 4) * 128:(k % 4 + 1) * 128], h2b[:, k * 128:(k + 1) * 128], ident_f[:]),
                                 reads=[h2b, ident_f], writes=[pp])
                        hb = h2Tb[ti % 2]
                        for q_ in range(2):
                            pp = PS[2 + q_]
                            S.op("dve", lambda: nc.vector.tensor_copy(out=h2T32[:, q_ * 4:(q_ + 1) * 4, :].rearrange("p a b -> p (a b)"), in_=pp[:]), reads=[pp], writes=[h2T32])
                            S.op("act", lambda: nc.scalar.copy(out=hb[:, q_ * 4:(q_ + 1) * 4, :].rearrange("p a b -> p (a b)"), in_=pp[:]), reads=[pp], writes=[hb])
                        S.dma("pool", lambda: nc.gpsimd.dma_start(out=h2T_d[:, :, r0:r0 + 128].rearrange("k p t -> p k t"), in_=hb[:]), reads=[hb])
                        lt = lg[ti % 2]
                        for k in range(8):
                            S.op("pe", lambda k=k: nc.tensor.matmul(PS[4][:, 0:36], lhsT=h2T32[:, k, :], rhs=wrg[:, k, :], start=(k == 0), stop=(k == 7)),
                                 reads=[h2T32, wrg], writes=[PS[4]])
                        S.op("dve", lambda: nc.vector.tensor_tensor(out=lt[:], in0=PS[4][:, 0:36], in1=brb[:], op=ALU.add), reads=[PS[4], brb], writes=[lt])
                        S.op("dve", lambda: nc.vector.tensor_reduce(out=sc_[:, 16:17], in_=lt[:, 0:4], axis=AX.X, op=ALU.max), reads=[lt], writes=[sc_])
                        S.op("dve", lambda: nc.vector.tensor_scalar(out=sc_[:, 17:18], in0=sc_[:, 16:17], scalar1=-1.0, scalar2=None, op0=ALU.mult), reads=[sc_], writes=[sc_])
                        S.op("act", lambda: nc.scalar.activation(out=sc_[:, 36:40], in_=lt[:, 0:4], func=AF.Exp, bias=sc_[:, 17:18], scale=1.0, accum_out=sc_[:, 18:19]),
                             reads=[lt, sc_], writes=[sc_])
                        S.op("dve", lambda: nc.vector.reciprocal(out=sc_[:, 19:20], in_=sc_[:, 18:19]), reads=[sc_], writes=[sc_])
                        S.op("dve", lambda: nc.vector.tensor_scalar(out=sc_[:, 20:24], in0=lt[:, 0:4], scalar1=sc_[:, 16:17], scalar2=None, op0=ALU.is_equal), reads=[lt, sc_], writes=[sc_])
                        S.op("dve", lambda: nc.vector.tensor_scalar(out=sc_[:, 24:28], in0=sc_[:, 20:24], scalar1=-1.0, scalar2=1e30, op0=ALU.add, op1=ALU.mult), reads=[sc_], writes=[sc_])
                        for g in range(4):
                            S.op("dve", lambda g=g: nc.vector.tensor_scalar(out=lm[:, g * 8:(g + 1) * 8], in0=lt[:, 4 + g * 8:12 + g * 8], scalar1=sc_[:, 20 + g:21 + g],
                                                                             scalar2=sc_[:, 24 + g:25 + g], op0=ALU.mult, op1=ALU.add), reads=[lt, sc_], writes=[lm])
                        S.op("dve", lambda: nc.vector.tensor_reduce(out=sc_[:, 28:29], in_=lm[:], axis=AX.X, op=ALU.max), reads=[lm], writes=[sc_])
                        S.op("dve", lambda: nc.vector.tensor_scalar(out=sel1[:], in0=lm[:], scalar1=sc_[:, 28:29], scalar2=None, op0=ALU.is_equal), reads=[lm, sc_], writes=[sel1])
                        S.op("dve", lambda: nc.vector.scalar_tensor_tensor(out=lm2[:], in0=sel1[:], scalar=-1e30, in1=lm[:], op0=ALU.mult, op1=ALU.add), reads=[sel1, lm], writes=[lm2])
                        S.op("dve", lambda: nc.vector.tensor_reduce(out=sc_[:, 29:30], in_=lm2[:], axis=AX.X, op=ALU.max), reads=[lm2], writes=[sc_])
                        S.op("dve", lambda: nc.vector.tensor_scalar(out=sel2[:], in0=lm2[:], scalar1=sc_[:, 29:30], scalar2=None, op0=ALU.is_equal), reads=[lm2, sc_], writes=[sel2])
                        S.op("dve", lambda: nc.vector.tensor_tensor(out=sc_[:, 30:31], in0=sc_[:, 28:29], in1=sc_[:, 29:30], op=ALU.subtract), reads=[sc_], writes=[sc_])
                        S.op("act", lambda: nc.scalar.activation(out=sc_[:, 31:32], in_=sc_[:, 30:31], func=AF.Sigmoid), reads=[sc_], writes=[sc_])
                        S.op("dve", lambda: nc.vector.tensor_tensor(out=sc_[:, 31:32], in0=sc_[:, 31:32], in1=sc_[:, 19:20], op=ALU.mult), reads=[sc_], writes=[sc_])
                        S.op("dve", lambda: nc.vector.tensor_tensor(out=sc_[:, 32:33], in0=sc_[:, 19:20], in1=sc_[:, 31:32], op=ALU.subtract), reads=[sc_], writes=[sc_])
                        S.op("dve", lambda: nc.vector.tensor_scalar(out=comb[:, ti, :], in0=sel1[:], scalar1=sc_[:, 31:32], scalar2=None, op0=ALU.mult), reads=[sel1, sc_], writes=[comb])
                        S.op("dve", lambda: nc.vector.scalar_tensor_tensor(out=comb[:, ti, :], in0=sel2[:], scalar=sc_[:, 32:33], in1=comb[:, ti, :], op0=ALU.mult, op1=ALU.add),
                             reads=[sel2, sc_, comb], writes=[comb])
                if dbg:
                    S.dma("pool", lambda: nc.gpsimd.dma_start(out=comb_d[0:NT4 * 128, :].rearrange("(t p) e -> p t e", p=128), in_=comb[:]), reads=[comb])
                S.new_epoch()
                sa4.__exit__(None, None, None)
                if stages <= 3.5:
                    return True
                NG = max(1, NT4 // 16)
                TPG = NT4 // NG
                acc = sb("acc", [128, TPG, D], F32)
                wg = [sb("wg%d" % i, [128, 8, 512], BF16) for i in range(2)]
                wu = [sb("wu%d" % i, [128, 8, 512], BF16) for i in range(2)]
                wd = [sb("wd%d" % i, [128, 4, D], BF16) for i in range(2)]
                hblk = [sb("hblk%d" % i, [128, 8, 512], BF16) for i in range(2)]
                sg = [sb("sg%d" % i, [128, 512], F32) for i in range(2)]
                hid = [sb("hid%d" % i, [128, 4, 512], BF16) for i in range(2)]
                x1f = [sb("x1f%d" % i, [128, D], F32) for i in range(2)]
                sf = [sb("sf%d" % i, [128, 4], F32) for i in range(2)]
                cnt4 = {"h": 0}
                pend = [None]
                for grp in range(NG):
                    for e in range(NEXP):
                        g_, u_, d_ = wg[e % 2], wu[e % 2], wd[e % 2]
                        S.dma("pool", lambda: nc.gpsimd.dma_start(out=g_[:], in_=wgate_d[e].rearrange("(k p) n -> p k n", p=128)), writes=[g_])
                        S.dma("pool", lambda: nc.gpsimd.dma_start(out=u_[:], in_=wup_d[e].rearrange("(k p) n -> p k n", p=128)), writes=[u_])
                        S.dma("pool", lambda: nc.gpsimd.dma_start(out=d_[:], in_=wdown_d[e].rearrange("(k p) n -> p k n", p=128)), writes=[d_])
                        for jb in range(TPG // 4):
                            t0 = (grp * TPG + jb * 4) * 128
                            hb = hblk[cnt4["h"] % 2]
                            hd = hid[cnt4["h"] % 2]
                            cnt4["h"] += 1
                            S.dma("sp", lambda: nc.sync.dma_start(out=hb[:], in_=h2T_d[:, :, t0:t0 + 512].rearrange("k p t -> p k t")), writes=[hb])
                            for hc in range(4):
                                pg_, pu_ = PS[(hc % 2) * 2], PS[(hc % 2) * 2 + 1]
                                for k in range(8):
                                    S.op("pe", lambda k=k: nc.tensor.matmul(pg_[:], lhsT=g_[:, k, hc * 128:(hc + 1) * 128], rhs=hb[:, k, :], start=(k == 0), stop=(k == 7)),
                                         reads=[g_, hb], writes=[pg_])
                                for k in range(8):
                                    S.op("pe", lambda k=k: nc.tensor.matmul(pu_[:], lhsT=u_[:, k, hc * 128:(hc + 1) * 128], rhs=hb[:, k, :], start=(k == 0), stop=(k == 7)),
                                         reads=[u_, hb], writes=[pu_])
                                sgb = sg[hc % 2]
                                S.op("act", lambda: nc.scalar.activation(out=sgb[:], in_=pg_[:], func=AF.Silu), reads=[pg_], writes=[sgb])
                                S.op("dve", lambda: nc.vector.tensor_tensor(out=hd[:, hc, :], in0=pu_[:], in1=sgb[:], op=ALU.mult), reads=[pu_, sgb], writes=[hd])
                            def down(e=e, jb=jb, hd=hd, d_=d_, grp=grp):
                                for tt in range(4):
                                    tl = jb * 4 + tt
                                    tg = grp * TPG + tl
                                    for half in range(2):
                                        py = PS[4 + (tt * 2 + half) % 3]
                                        for hc in range(4):
                                            S.op("pe", lambda hc=hc: nc.tensor.matmul(py[:], lhsT=hd[:, hc, tt * 128:(tt + 1) * 128], rhs=d_[:, hc, half * 512:(half + 1) * 512],
                                                                                      start=(hc == 0), stop=(hc == 3)), reads=[hd, d_], writes=[py])
                                        hs = slice(half * 512, (half + 1) * 512)
                                        if e == 0:
                                            S.op("dve", lambda: nc.vector.tensor_scalar(out=acc[:, tl, hs], in0=py[:], scalar1=comb[:, tg, e:e + 1], scalar2=None, op0=ALU.mult),
                                                 reads=[py, comb], writes=[acc])
                                        else:
                                            S.op("dve", lambda: nc.vector.scalar_tensor_tensor(out=acc[:, tl, hs], in0=py[:], scalar=comb[:, tg, e:e + 1], in1=acc[:, tl, hs],
                                                                                                op0=ALU.mult, op1=ALU.add), reads=[py, comb, acc], writes=[acc])
                            if pend[0] is not None:
                                pend[0]()
                            pend[0] = down
                    if pend[0] is not None:
                        pend[0]()
                        pend[0] = None
                    for tl in range(TPG):
                        tg = grp * TPG + tl
                        r0 = tg * 128
                        xb = x1f[tl % 2]
                        sc_ = sf[tl % 2]
                        S.dma("sp", lambda: nc.sync.dma_start(out=xb[:], in_=x1_d[r0:r0 + 128, :]), writes=[xb])
                        S.op("dve", lambda: nc.vector.tensor_tensor(out=acc[:, tl, :], in0=acc[:, tl, :], in1=gate2b[:], op=ALU.mult), reads=[acc, gate2b], writes=[acc])
                        S.op("dve", lambda: nc.vector.tensor_tensor(out=xb[:], in0=xb[:], in1=acc[:, tl, :], op=ALU.add), reads=[xb, acc], writes=[xb])
                        S.op("act", lambda: nc.scalar.activation(out=junk4[:], in_=xb[:], func=AF.Square, accum_out=sc_[:, 0:1]), reads=[xb], writes=[junk4, sc_])
                        S.op("dve", lambda: nc.vector.tensor_scalar(out=sc_[:, 0:1], in0=sc_[:, 0:1], scalar1=1.0 / D, scalar2=EPS, op0=ALU.mult, op1=ALU.add), reads=[sc_], writes=[sc_])
                        S.op("act", lambda: nc.scalar.sqrt(out=sc_[:, 0:1], in_=sc_[:, 0:1]), reads=[sc_], writes=[sc_])
                        S.op("dve", lambda: nc.vector.reciprocal(out=sc_[:, 0:1], in_=sc_[:, 0:1]), reads=[sc_], writes=[sc_])
                        S.op("dve", lambda: nc.vector.scalar_tensor_tensor(out=xb[:], in0=xb[:], scalar=sc_[:, 0:1], in1=fingb[:], op0=ALU.mult, op1=ALU.mult),
                             reads=[xb, sc_, fingb], writes=[xb])
                        S.dma("pool", lambda: nc.gpsimd.dma_start(out=out_d[r0:r0 + 128, :], in_=xb[:]), reads=[xb])
                    if grp == 1 and NG > 2:
                        S.new_epoch()
                S.barrier()
            return False

        if 4 in run:
            if stage4():
                return nc, S
        if stages <= 1:
            return nc, S

    return nc, S


def make_inputs(core, inp):
    b = core
    m = {}
    m["x"] = np.ascontiguousarray(inp["x"][b])
    m["ctx"] = np.ascontiguousarray(inp["ctx"][b])
    cT = np.zeros((128, 16), np.float32)
    cT[:, 0:8] = inp["c"][b].reshape(8, 128).T
    cT[:, 8:16] = inp["c_ctx"].reshape(8, 128).T
    m["cT"] = cT
    m["w_mod"] = np.ascontiguousarray(inp["w_mod"][0])
    m["b_mod"] = np.ascontiguousarray(inp["b_mod"][0].reshape(1, -1))
    m["n1gT"] = np.ascontiguousarray(inp["norm1_g"][0].reshape(8, 128).T)
    m["w_in"] = np.ascontiguousarray(inp["w_in"][0])
    cw = inp["conv_w"][0].reshape(9, 12, 128)
    m["convw"] = np.ascontiguousarray(cw.transpose(2, 1, 0))
    m["a_log"] = np.ascontiguousarray(inp["a_log"][0].reshape(1, 8))
    m["dt_bias"] = np.ascontiguousarray(inp["dt_bias"][0].reshape(1, 8))
    m["n2g"] = np.ascontiguousarray(inp["norm2_g"][0].reshape(1, -1))
    m["fing"] = np.ascontiguousarray(inp["final_g"].reshape(1, -1))
    m["onorm"] = np.ascontiguousarray(inp["onorm_g"][0].reshape(1, -1))
    m["b_group"] = np.ascontiguousarray(inp["b_group"][0].reshape(1, -1))
    m["b_router"] = np.ascontiguousarray(inp["b_router"][0].reshape(1, -1))
    m["w_group"] = np.ascontiguousarray(inp["w_group"][0])
    m["w_router"] = np.ascontiguousarray(inp["w_router"][0])
    m["w_out"] = np.ascontiguousarray(inp["w_out"][0])
    m["w_gate"] = np.ascontiguousarray(inp["w_gate"][0])
    m["w_up"] = np.ascontiguousarray(inp["w_up"][0])
    m["w_down"] = np.ascontiguousarray(inp["w_down"][0])
    m.update(host_consts())
    return m


def kernel(**inp):
    inp = {k: np.asarray(v) for k, v in inp.items()}
    nc, S = build()
    in_maps = [make_inputs(c, inp) for c in range(8)]
    res = run_bass_kernel_spmd(nc, in_maps, core_ids=list(range(8)))
    return np.stack([r["out"] for r in res.results], axis=0).astype(np.float32)
```
